# Optimizing a Trainium2 kernel written in Bass

```python
import math
import jax
import jax.numpy as jnp
from jax import lax
import numpy as np


D_MODEL = 4096
BATCH = 4
SEQ = 2048
DEPTH = 1

CHUNK = 64
EPS = 1e-6
N_MOD = 6
A_HEADS = 16
A_HEAD_DIM = 128
A_Q_RANK = 1024
A_KV_RANK = 512
A_WIDTH = A_HEADS * A_HEAD_DIM
IDX_HEADS = 64
IDX_DIM = 128
TOPK_MAX = 256
Q_BLOCK = 128
T5_BUCKETS = 32
T5_MAX_DIST = 128
R_HEADS = 8
R_QK_DIM = 128
R_V_DIM = 256
R_QK_WIDTH = R_HEADS * R_QK_DIM
R_WIDTH = R_HEADS * R_V_DIM
ROT_BASE = 10000.0
P_HEADS = 8
P_QUERY_DIM = 256
P_NKEYS = 128
P_TOPK = 16
P_EXPERTS = P_NKEYS * P_NKEYS
P_TOKEN_BLOCK = 128
IN_SPLITS = (A_Q_RANK, A_KV_RANK, IDX_DIM, IDX_HEADS, R_QK_WIDTH, R_QK_WIDTH, R_WIDTH, R_WIDTH)
IN_WIDTH = A_Q_RANK + A_KV_RANK + IDX_DIM + IDX_HEADS + 2 * R_QK_WIDTH + 2 * R_WIDTH
N_BRANCH = 2

kernel_name = 'hybrid_dsa_retention_peer_block'


def rms_norm(x, g):
    xf = x.astype(jnp.float32)
    y = xf * lax.rsqrt(jnp.mean(xf * xf, axis=-1, keepdims=True) + EPS)
    return (y * g).astype(x.dtype)


def layer_norm(x, g, b):
    xf = x.astype(jnp.float32)
    mu = jnp.mean(xf, axis=-1, keepdims=True)
    var = jnp.mean(jnp.square(xf - mu), axis=-1, keepdims=True)
    y = (xf - mu) * lax.rsqrt(var + EPS)
    return (y * g + b).astype(x.dtype)


def modulate(h, shift, scale):
    return h * (1.0 + scale[:, None, :]) + shift[:, None, :]


def t5_bucket(rel):
    half = T5_BUCKETS // 2
    exact = half // 2
    n = jnp.abs(rel)
    log_ratio = jnp.log(jnp.maximum(n, 1).astype(jnp.float32) / exact) / math.log(T5_MAX_DIST / exact)
    large = jnp.minimum(exact + (log_ratio * (half - exact)).astype(jnp.int32), half - 1)
    return jnp.where(rel > 0, half, 0) + jnp.where(n < exact, n, large)


def dsa_branch(c_q, c_kv, k_i, w_i, g_cq, g_ckv, w_uq, w_uk, w_uv, w_qi, g_ki, b_ki, t5_bias, top_k):
    B, S, _ = c_q.shape
    c_q = rms_norm(c_q, g_cq)
    c_kv = rms_norm(c_kv, g_ckv)
    q = jnp.einsum('bsr,rhd->bshd', c_q, w_uq)
    q_lat = jnp.einsum('bshd,chd->bshc', q, w_uk)
    q_idx = jnp.einsum('bsr,rhd->bshd', c_q, w_qi)
    k_idx = layer_norm(k_i, g_ki, b_ki)
    w_idx = w_i * (IDX_HEADS ** -0.5 * IDX_DIM ** -0.5)
    kchunk = jnp.arange(S, dtype=jnp.int32) // CHUNK

    def block(i):
        q0 = i * Q_BLOCK
        qi = lax.dynamic_slice_in_dim(q_idx, q0, Q_BLOCK, axis=1)
        wi = lax.dynamic_slice_in_dim(w_idx, q0, Q_BLOCK, axis=1)
        ql = lax.dynamic_slice_in_dim(q_lat, q0, Q_BLOCK, axis=1)
        qpos = q0 + jnp.arange(Q_BLOCK, dtype=jnp.int32)
        qchunk = qpos // CHUNK
        rel = jax.nn.relu(jnp.einsum('bthd,bsd->bths', qi, k_idx))
        score = jnp.einsum('bths,bth->bts', rel, wi).astype(jnp.float32)
        adm = kchunk[None, :] <= qchunk[:, None]
        score = jnp.where(adm[None], score, -jnp.inf)
        _, sel = lax.top_k(score, top_k)
        valid = (sel // CHUNK) <= qchunk[None, :, None]
        kv_sel = jax.vmap(lambda kv, ix: kv[ix])(c_kv, sel)
        logits = jnp.einsum('bthc,btkc->bthk', ql, kv_sel).astype(jnp.float32) * (A_HEAD_DIM ** -0.5)
        bias = t5_bias[t5_bucket(sel - qpos[None, :, None])]
        logits = logits + jnp.moveaxis(bias, -1, 2).astype(jnp.float32)
        logits = jnp.where(valid[:, :, None, :], logits, -jnp.inf)
        p = jax.nn.softmax(logits, axis=-1).astype(kv_sel.dtype)
        o_lat = jnp.einsum('bthk,btkc->bthc', p, kv_sel)
        o = jnp.einsum('bthc,chd->bthd', o_lat, w_uv)
        return o.reshape(B, Q_BLOCK, A_WIDTH)

    out = lax.map(block, jnp.arange(S // Q_BLOCK))
    return jnp.moveaxis(out, 0, 1).reshape(B, S, A_WIDTH)


def retention_branch(r_q, r_k, r_v, r_g, g_ret):
    B, S, _ = r_q.shape
    f32 = jnp.float32
    NC = S // CHUNK
    half = R_QK_DIM // 2
    q = r_q.astype(f32).reshape(B, S, R_HEADS, R_QK_DIM)
    k = r_k.astype(f32).reshape(B, S, R_HEADS, R_QK_DIM) * (R_QK_DIM ** -0.5)
    v = r_v.astype(f32).reshape(B, S, R_HEADS, R_V_DIM)
    inv = 1.0 / (ROT_BASE ** jnp.linspace(0.0, 1.0, half, dtype=f32))
    ang = jnp.arange(S, dtype=f32)[:, None] * inv[None, :]
    cos = jnp.cos(ang)[None, :, None, :]
    sin = jnp.sin(ang)[None, :, None, :]

    def rot(t):
        t1, t2 = t[..., :half], t[..., half:]
        return jnp.concatenate([t1 * cos - t2 * sin, t1 * sin + t2 * cos], axis=-1)

    q = rot(q).reshape(B, NC, CHUNK, R_HEADS, R_QK_DIM)
    k = rot(k).reshape(B, NC, CHUNK, R_HEADS, R_QK_DIM)
    v = v.reshape(B, NC, CHUNK, R_HEADS, R_V_DIM)
    log_g = jnp.log(1.0 - jnp.power(2.0, -5.0 - jnp.arange(R_HEADS, dtype=f32)))
    j = jnp.arange(CHUNK, dtype=f32)
    diff = j[:, None] - j[None, :]
    dmask = jnp.where(diff[None] >= 0, jnp.exp(jnp.maximum(diff, 0.0)[None] * log_g[:, None, None]), 0.0)
    att = jnp.einsum('bnjhk,bnlhk->bnhjl', q, k) * dmask[None, None]
    y_intra = jnp.einsum('bnhjl,bnlhv->bnjhv', att, v)
    k_dec = k * jnp.exp((CHUNK - 1.0 - j)[:, None] * log_g[None, :])[None, None, :, :, None]
    kv = jnp.einsum('bnlhk,bnlhv->nbhkv', k_dec, v)
    chunk_dec = jnp.exp(CHUNK * log_g)[None, :, None, None]

    def step(state, kv_n):
        return state * chunk_dec + kv_n, state

    _, s_prev = lax.scan(step, jnp.zeros((B, R_HEADS, R_QK_DIM, R_V_DIM), f32), kv)
    q_dec = q * jnp.exp((j + 1.0)[:, None] * log_g[None, :])[None, None, :, :, None]
    y_cross = jnp.einsum('bnjhk,nbhkv->bnjhv', q_dec, s_prev)
    y = (y_intra + y_cross).reshape(B, S, R_HEADS, R_V_DIM)
    y = y * lax.rsqrt(jnp.mean(y * y, axis=-1, keepdims=True) + EPS) * g_ret
    y = jax.nn.silu(r_g.astype(f32)) * y.reshape(B, S, R_WIDTH)
    return y.astype(r_q.dtype)


def mixer_block(h, w_in, g_cq, g_ckv, w_uq, w_uk, w_uv, w_qi, g_ki, b_ki, t5_bias, g_ret,
                w_up, w_gate, b_gate, w_out, top_k):
    proj = jnp.einsum('bsd,de->bse', h, w_in)
    offs = np.cumsum(IN_SPLITS)[:-1].tolist()
    c_q, c_kv, k_i, w_i, r_q, r_k, r_v, r_g = jnp.split(proj, offs, axis=-1)
    y_a = dsa_branch(c_q, c_kv, k_i, w_i, g_cq, g_ckv, w_uq, w_uk, w_uv, w_qi, g_ki, b_ki, t5_bias, top_k)
    y_r = retention_branch(r_q, r_k, r_v, r_g, g_ret)
    p_a = jnp.einsum('bse,ed->bsd', y_a, w_up[:A_WIDTH])
    p_r = jnp.einsum('bse,ed->bsd', y_r, w_up[A_WIDTH:])
    gates = jax.nn.sigmoid(jnp.einsum('bsd,de->bse', h, w_gate) + b_gate)
    g_a, g_r = jnp.split(gates, N_BRANCH, axis=-1)
    merged = g_a * p_a + g_r * p_r
    return jnp.einsum('bsd,de->bse', merged, w_out)


def peer_ffn(h, w_pq, sub_keys, u_exp, v_exp):
    B, S, D = h.shape
    hb = h.reshape(B * S // P_TOKEN_BLOCK, P_TOKEN_BLOCK, D)
    hq = P_QUERY_DIM // 2

    def block(t):
        q = jnp.einsum('td,de->te', t, w_pq).reshape(P_TOKEN_BLOCK, P_HEADS, P_QUERY_DIM)
        s1 = jnp.einsum('thd,kd->thk', q[..., :hq], sub_keys[0]).astype(jnp.float32)
        s2 = jnp.einsum('thd,kd->thk', q[..., hq:], sub_keys[1]).astype(jnp.float32)
        v1, i1 = lax.top_k(s1, P_TOPK)
        v2, i2 = lax.top_k(s2, P_TOPK)
        cand = (v1[..., :, None] + v2[..., None, :]).reshape(P_TOKEN_BLOCK, P_HEADS, P_TOPK * P_TOPK)
        cid = (i1[..., :, None] * P_NKEYS + i2[..., None, :]).reshape(P_TOKEN_BLOCK, P_HEADS, P_TOPK * P_TOPK)
        top, pidx = lax.top_k(cand, P_TOPK)
        eid = jnp.take_along_axis(cid, pidx, axis=-1)
        gate = jax.nn.softmax(top, axis=-1)
        u = u_exp[eid]
        act = jax.nn.gelu(jnp.einsum('thed,td->the', u, t).astype(jnp.float32), approximate=False)
        coef = (gate * act).astype(t.dtype)
        return jnp.einsum('the,thed->td', coef, v_exp[eid])

    return lax.map(block, hb).reshape(B, S, D)


def setup_inputs(seed: int = 0) -> dict:
    key = jax.random.key(seed)
    ks = jax.random.split(key, 32)
    f32 = jnp.float32
    L = DEPTH
    D = D_MODEL

    def nrm(k, shape, scale):
        return jax.random.normal(k, shape, f32) * scale

    return {
        'x': nrm(ks[0], (BATCH, SEQ, D), 1.0),
        'c': nrm(ks[1], (BATCH, D), 1.0),
        'w_ada': nrm(ks[2], (L, D, N_MOD * D), 0.5 * D ** -0.5),
        'b_ada': nrm(ks[3], (L, N_MOD * D), 0.02),
        'g_mix': 1.0 + nrm(ks[4], (L, D), 0.02),
        'w_in': nrm(ks[5], (L, D, IN_WIDTH), D ** -0.5),
        'g_cq': 1.0 + nrm(ks[6], (L, A_Q_RANK), 0.02),
        'g_ckv': 1.0 + nrm(ks[7], (L, A_KV_RANK), 0.02),
        'w_uq': nrm(ks[8], (L, A_Q_RANK, A_HEADS, A_HEAD_DIM), A_Q_RANK ** -0.5),
        'w_uk': nrm(ks[9], (L, A_KV_RANK, A_HEADS, A_HEAD_DIM), A_HEAD_DIM ** -0.5),
        'w_uv': nrm(ks[10], (L, A_KV_RANK, A_HEADS, A_HEAD_DIM), A_KV_RANK ** -0.5),
        'w_qi': nrm(ks[11], (L, A_Q_RANK, IDX_HEADS, IDX_DIM), A_Q_RANK ** -0.5),
        'g_ki': 1.0 + nrm(ks[12], (L, IDX_DIM), 0.02),
        'b_ki': nrm(ks[13], (L, IDX_DIM), 0.02),
        't5_bias': nrm(ks[14], (T5_BUCKETS, A_HEADS), 0.3),
        'g_ret': 1.0 + nrm(ks[15], (L, R_HEADS, R_V_DIM), 0.02),
        'w_up': nrm(ks[16], (L, A_WIDTH + R_WIDTH, D), A_WIDTH ** -0.5),
        'w_gate': nrm(ks[17], (L, D, N_BRANCH * D), D ** -0.5),
        'b_gate': nrm(ks[18], (L, N_BRANCH * D), 0.02),
        'w_out': nrm(ks[19], (L, D, D), D ** -0.5),
        'g_ffn': 1.0 + nrm(ks[20], (L, D), 0.02),
        'w_pq': nrm(ks[21], (L, D, P_HEADS * P_QUERY_DIM), D ** -0.5),
        'sub_keys': nrm(ks[22], (L, 2, P_NKEYS, P_QUERY_DIM // 2), (P_QUERY_DIM // 2) ** -0.5),
        'u_exp': nrm(ks[23], (L, P_EXPERTS, D), D ** -0.5),
        'v_exp': nrm(ks[24], (L, P_EXPERTS, D), P_HEADS ** -0.5),
        'g_final': 1.0 + nrm(ks[25], (D,), 0.02),
    }


def reference(x, c, w_ada, b_ada, g_mix, w_in, g_cq, g_ckv, w_uq, w_uk, w_uv, w_qi, g_ki, b_ki,
              t5_bias, g_ret, w_up, w_gate, b_gate, w_out, g_ffn, w_pq, sub_keys, u_exp, v_exp, g_final):
    S = x.shape[1]
    top_k = min(TOPK_MAX, S // 4)
    c_act = jax.nn.silu(c)
    for l in range(DEPTH):
        mod = jnp.einsum('bd,de->be', c_act, w_ada[l]) + b_ada[l]
        sh_m, sc_m, gt_m, sh_f, sc_f, gt_f = jnp.split(mod, N_MOD, axis=-1)
        h = modulate(rms_norm(x, g_mix[l]), sh_m, sc_m)
        y = mixer_block(h, w_in[l], g_cq[l], g_ckv[l], w_uq[l], w_uk[l], w_uv[l], w_qi[l], g_ki[l],
                        b_ki[l], t5_bias, g_ret[l], w_up[l], w_gate[l], b_gate[l], w_out[l], top_k)
        x = x + gt_m[:, None, :] * y
        h = modulate(rms_norm(x, g_ffn[l]), sh_f, sc_f)
        x = x + gt_f[:, None, :] * peer_ffn(h, w_pq[l], sub_keys[l], u_exp[l], v_exp[l])
    return rms_norm(x, g_final)
```

```python
import math
import numpy as np
import concourse.bass as bass
import concourse.mybir as mybir
from concourse.bass_utils import run_bass_kernel_spmd

F32 = mybir.dt.float32
BF16 = mybir.dt.bfloat16
ALU = mybir.AluOpType
AF = mybir.ActivationFunctionType
AX = mybir.AxisListType

EPOCH = 12000
N_DMA_SEMS = 40
EPS = 1e-6
NEG = -1.0e30


class KB:
    ENGS = ("pe", "act", "dve", "pool", "sp")

    def __init__(self, nc):
        self.nc = nc
        self.stack = []
        self.streams = {e: [] for e in self.ENGS}
        self.cnt = {e: 0 for e in self.ENGS}
        self.cur_sem = {}
        self.known = {e: {} for e in self.ENGS}
        self.res = {}
        self.sem_objs = {}
        self.n_sems = 0
        for e in self.ENGS:
            self.cur_sem[e] = self._new_sem(f"s_{e}")
        self.dma_sems = [self._new_sem(f"s_dma{i}") for i in range(N_DMA_SEMS)]
        self.dma_cnt = [0] * N_DMA_SEMS
        self.dma_rr = 0
        self.n_inst = 0

    def _new_sem(self, name):
        cm = self.nc.semaphore(f"{name}_{self.n_sems}")
        s = cm.__enter__()
        self.stack.append(cm)
        sid = self.n_sems
        self.n_sems += 1
        self.sem_objs[sid] = s
        return sid

    def sbuf(self, name, shape, dtype):
        self.n_alloc = getattr(self, "n_alloc", 0) + 1
        cm = self.nc.sbuf_tensor(f"sb{self.n_alloc}_{name}", list(shape), dtype)
        t = cm.__enter__()
        self.stack.append(cm)
        return t

    def psum(self, name, shape, dtype):
        cm = self.nc.psum_tensor(name, list(shape), dtype)
        t = cm.__enter__()
        self.stack.append(cm)
        return t

    def mark(self):
        return len(self.stack)

    def free_to(self, mark):
        while len(self.stack) > mark:
            cm = self.stack.pop()
            cm.__exit__(None, None, None)

    def _need(self, eng, reads, writes, pe_accum=False):
        need = {}

        def add(ev):
            if ev is None:
                return
            sid, val = ev
            if need.get(sid, 0) < val:
                need[sid] = val

        for r in reads:
            st = self.res.get(r)
            if st is not None:
                add(st[0])
        for w in writes:
            st = self.res.get(w)
            if st is not None:
                if not (pe_accum and st[0] is not None and st[0][0] == self.cur_sem[eng]):
                    add(st[0])
                for sid, val in st[1].items():
                    add((sid, val))
        waits = []
        kn = self.known[eng]
        own = self.cur_sem[eng]
        for sid, val in need.items():
            if kn.get(sid, 0) >= val:
                continue
            if sid == own and eng in ("pe", "sp"):
                continue
            kn[sid] = val
            waits.append((sid, val))
        return waits

    def _commit(self, ev, reads, writes):
        for w in writes:
            self.res[w] = [ev, {}]
        for r in reads:
            st = self.res.setdefault(r, [None, {}])
            sid, val = ev
            if st[1].get(sid, 0) < val:
                st[1][sid] = val

    def op(self, eng, fn, reads=(), writes=(), pe_accum=False):
        if self.cnt[eng] >= EPOCH:
            self.cur_sem[eng] = self._new_sem(f"s_{eng}")
            self.cnt[eng] = 0
        excl = [r for r in reads if r.startswith("pb")]
        if excl:
            reads = [r for r in reads if not r.startswith("pb")]
            writes = list(writes) + excl
        waits = self._need(eng, reads, writes, pe_accum)
        self.cnt[eng] += 1
        ev = (self.cur_sem[eng], self.cnt[eng])
        self.streams[eng].append((waits, fn, (ev[0], 1)))
        self._commit(ev, reads, writes)
        self.n_inst += 1
        return ev

    def dma(self, q, fn, reads=(), writes=()):
        k = self.dma_rr
        self.dma_rr = (self.dma_rr + 1) % N_DMA_SEMS
        sid = self.dma_sems[k]
        waits = self._need(q, reads, writes)
        prev = self.dma_cnt[k]
        if prev > 0 and self.known[q].get(sid, 0) < prev:
            self.known[q][sid] = prev
            waits.append((sid, prev))
        self.dma_cnt[k] += 16
        ev = (sid, self.dma_cnt[k])
        self.streams[q].append((waits, fn, (sid, 16)))
        self._commit(ev, reads, writes)
        self.n_inst += 1
        return ev

    def barrier(self):
        evs = []
        for e in self.ENGS:
            if self.cnt[e] > 0:
                evs.append((self.cur_sem[e], self.cnt[e]))
        for k in range(N_DMA_SEMS):
            if self.dma_cnt[k] > 0:
                evs.append((self.dma_sems[k], self.dma_cnt[k]))
        for e in self.ENGS:
            waits = []
            for sid, val in evs:
                if sid == self.cur_sem[e]:
                    continue
                if self.known[e].get(sid, 0) < val:
                    self.known[e][sid] = val
                    waits.append((sid, val))
            if waits:
                self.streams[e].append((waits, None, None))
        self.res = {}

    def emit(self):
        nc = self.nc
        so = self.sem_objs
        streams = self.streams
        with nc.Block() as block:
            def run(engobj, items):
                for waits, fn, inc in items:
                    for sid, val in waits:
                        engobj.wait_ge(so[sid], val)
                    if fn is not None:
                        ins = fn(engobj)
                        ins.then_inc(so[inc[0]], inc[1])

            @block.tensor
            def _(e):
                run(e, streams["pe"])

            @block.scalar
            def _(e):
                run(e, streams["act"])

            @block.vector
            def _(e):
                run(e, streams["dve"])

            @block.gpsimd
            def _(e):
                run(e, streams["pool"])

            @block.sync
            def _(e):
                run(e, streams["sp"])

    def close(self):
        self.free_to(0)


class Ring:
    def __init__(self, kb, name, n, shape, dtype):
        self.tiles = [kb.sbuf(f"{name}{i}", shape, dtype) for i in range(n)]
        self.keys = [f"{name}{i}" for i in range(n)]
        self.i = 0

    def get(self):
        t, k = self.tiles[self.i], self.keys[self.i]
        self.i = (self.i + 1) % len(self.tiles)
        return t, k


class PRing:
    def __init__(self, aps, keys):
        self.aps, self.keys, self.i = aps, keys, 0

    def get(self):
        t, k = self.aps[self.i], self.keys[self.i]
        self.i = (self.i + 1) % len(self.aps)
        return t, k


O_CQ, O_CKV, O_KI, O_WI, O_RQ, O_RK, O_RV, O_RG = 0, 1024, 1536, 1664, 1728, 2752, 3776, 5824
LOGG = [math.log(1.0 - 2.0 ** (-5.0 - h)) for h in range(8)]


_USED = {}


def build(stages=99, dbg=()):
    nc = bass.Bass("TRN2", target_bir_lowering=False)
    kb = KB(nc)

    used = []
    _USED["names"] = used

    def din(name, shape, dt=F32):
        used.append(name)
        return nc.dram_tensor(name, list(shape), dt, kind="ExternalInput").ap()

    def dscr(name, shape, dt):
        kind = "ExternalOutput" if name in dbg else "Internal"
        return nc.dram_tensor(name, list(shape), dt, kind=kind).ap()

    xkT = din("xkT", [4096, 2048]); xqT = din("xqT", [4096, 1024]); cT = din("cT", [128, 32])
    w_ada = din("w_ada", [4096, 24576]); b_ada = din("b_ada", [1, 24576])
    g_mix = din("g_mix", [128, 32]); g_ffn = din("g_ffn", [128, 32]); g_final = din("g_final", [128, 32])
    w_in = din("w_in", [4096, 7872])
    g_cq = din("g_cq", [1024]); g_ckv = din("g_ckv", [512]); g_ki = din("g_ki", [128]); b_ki = din("b_ki", [128])
    g_ret = din("g_ret", [2048])
    if stages >= 4:
        w_uq = din("w_uq", [1024, 2048]); w_ukT = din("w_ukT", [128, 16, 512]); w_uv = din("w_uv", [512, 2048])
        w_qi = din("w_qi", [1024, 8192])
        t5b = din("t5b", [32, 16]); t15 = din("t15", [16, 1]); oh_rel = din("oh_rel", [32, 512])
    if stages >= 6:
        w_up = din("w_up", [4096, 4096]); w_gate = din("w_gate", [4096, 8192]); b_gate = din("b_gate", [128, 64])
        w_out = din("w_out", [4096, 4096])
    if stages >= 7:
        w_pq = din("w_pq", [4096, 2048]); keysT = din("keysT", [2, 128, 128])
        uT = din("uT", [4096, 16384]); v_exp = din("v_exp", [16384, 4096])
    ident_in = din("ident", [128, 128]); anti_in = din("anti", [128, 128])
    cosk = din("cosk", [2048, 128]); sink = din("sink", [2048, 128]); kdec_tab = din("kdec_tab", [2048, 8])
    cosq = din("cosq", [1024, 128]); sinq = din("sinq", [1024, 128]); qdec_tab = din("qdec_tab", [1024, 8])
    mret_in = din("mret", [128, 2, 8, 128]); adm01_in = din("adm01", [128, 256]); negb_in = din("negb", [128, 256])
    outT = nc.dram_tensor("outT", [4096, 1024], F32, kind="ExternalOutput").ap()

    mod_d = dscr("mod_d", [1, 24576], F32)
    hk_d = dscr("hk_d", [128, 32, 2048], BF16); hq_d = dscr("hq_d", [128, 32, 1024], BF16)
    ckv_tm_d = dscr("ckv_tm_d", [2048, 512], BF16); ckvT_d = dscr("ckvT_d", [128, 4, 2048], BF16)
    kidxT_d = dscr("kidxT_d", [128, 2048], BF16)
    kT_d = dscr("kT_d", [128, 8, 2048], BF16); kdec_d = dscr("kdec_d", [2048, 1024], BF16); v_d = dscr("v_d", [2048, 2048], BF16)
    cqT_d = dscr("cqT_d", [128, 8, 1024], BF16); wi_d = dscr("wi_d", [1024, 64], F32)
    qT_d = dscr("qT_d", [128, 8, 1024], BF16); qdT_d = dscr("qdT_d", [128, 8, 1024], BF16); rg_d = dscr("rg_d", [1024, 2048], BF16)
    yaT_d = dscr("yaT_d", [128, 16, 1024], BF16); yrT_d = dscr("yrT_d", [128, 16, 1024], BF16)
    ef_d = dscr("ef_d", [16, 512], F32)
    gate_d = dscr("gate_d", [128, 64, 1024], BF16); mrg_d = dscr("mrg_d", [128, 32, 1024], BF16)
    x1T_d = dscr("x1T_d", [128, 32, 1024], F32); h2T_d = dscr("h2T_d", [128, 32, 1024], BF16)
    ge_d = dscr("ge_d", [1024, 16384], BF16); coefT_d = dscr("coefT_d", [16384, 1024], BF16)
    x2T_d = dscr("x2T_d", [128, 32, 1024], F32)

    def dma1(q, out, in_, reads, writes):
        kb.dma(q, lambda e: e.dma_start(out=out, in_=in_), reads, writes)

    def dma(q, out, in_, reads, writes):
        sh = out.shape
        if len(sh) == 3 and sh[0] * sh[1] > 1024 and len(in_.shape) == 3 and in_.shape[1] == sh[1]:
            step = max(1, 1024 // sh[0])
            for a in range(0, sh[1], step):
                dma1(q, out[:, a:a + step, :], in_[:, a:a + step, :], reads, writes)
        else:
            dma1(q, out, in_, reads, writes)

    def mm(out, lhsT, rhs, start, stop, reads, writes):
        kb.op("pe", lambda e: e.matmul(out, lhsT, rhs, start=start, stop=stop), reads, writes, pe_accum=not start)

    def tr(out, in_, reads, writes):
        kb.op("pe", lambda e: e.transpose(out, in_, ident[:]), list(reads) + ["ident"], writes)

    def act(out, in_, func, reads, writes, bias=None, scale=None):
        kw = {}
        if bias is not None:
            kw["bias"] = bias
        if scale is not None:
            kw["scale"] = scale
        kb.op("act", lambda e: e.activation(out=out, in_=in_, func=func, **kw), reads, writes)

    def tt(eng, out, in0, in1, op, reads, writes):
        kb.op(eng, lambda e: e.tensor_tensor(out=out, in0=in0, in1=in1, op=op), reads, writes)

    def ts(eng, out, in0, s1, op0, reads, writes, s2=None, op1=None):
        if op1 is None:
            kb.op(eng, lambda e: e.tensor_scalar(out=out, in0=in0, scalar1=s1, scalar2=None, op0=op0), reads, writes)
        else:
            kb.op(eng, lambda e: e.tensor_scalar(out=out, in0=in0, scalar1=s1, scalar2=s2, op0=op0, op1=op1), reads, writes)

    def stt(eng, out, in0, scalar, in1, op0, op1, reads, writes):
        kb.op(eng, lambda e: e.scalar_tensor_tensor(out=out, in0=in0, scalar=scalar, in1=in1, op0=op0, op1=op1), reads, writes)

    def cp(eng, out, in_, reads, writes):
        if eng == "act":
            kb.op("act", lambda e: e.copy(out, in_), reads, writes)
        else:
            kb.op(eng, lambda e: e.tensor_copy(out, in_), reads, writes)

    def recip(out, in_, reads, writes):
        kb.op("dve", lambda e: e.reciprocal(out=out, in_=in_), reads, writes)

    def rsum(out, in_, reads, writes):
        kb.op("dve", lambda e: e.reduce_sum(out=out, in_=in_, axis=AX.X), reads, writes)

    def vmax(out, in_, reads, writes):
        kb.op("dve", lambda e: e.max(out=out, in_=in_), reads, writes)

    def mrep(out, rep, vals, reads, writes):
        kb.op("dve", lambda e: e.match_replace(out=out, in_to_replace=rep, in_values=vals, imm_value=NEG), reads, writes)

    def amul(out, in_, c, reads, writes):
        kb.op("act", lambda e: e.mul(out, in_, c), reads, writes)

    def memset(eng, ap, val, writes):
        kb.op(eng, lambda e: e.memset(ap, val), (), writes)

    ident = kb.sbuf("ident", [128, 128], BF16)
    anti = kb.sbuf("anti", [128, 128], BF16)
    ones = kb.sbuf("ones", [128, 128], BF16)
    eps_t = kb.sbuf("eps_t", [128, 1], F32)
    modT = kb.sbuf("modT", [128, 192], F32)
    A1 = kb.sbuf("A1", [128, 32], F32); A2 = kb.sbuf("A2", [128, 32], F32)
    pbig = kb.psum("pbig", [128, 2048], F32)
    pb4 = kb.psum("pb4", [128, 512], F32); pb5 = kb.psum("pb5", [128, 512], F32); pb6 = kb.psum("pb6", [128, 512], F32)
    pb7 = kb.psum("pb7", [128, 512], F32)
    pst = pb7[:].bitcast(BF16)
    tslots = PRing([pst[:, 0:1024]], ["pb7"])
    big = [pbig[:, i * 512:(i + 1) * 512] for i in range(4)]
    bigk = [f"pbig{i}" for i in range(4)]

    dma("pool", ident[:], ident_in, [], ["ident"])
    dma("pool", anti[:], anti_in, [], ["anti"])
    memset("dve", ones[:], 1.0, ["ones"])
    memset("dve", eps_t[:], EPS, ["eps"])

    def transposes(dst, src, n, rk, wk, eng="act"):
        slot, sk = tslots.get()
        for i in range(n):
            tr(slot[:, i * 128:(i + 1) * 128], src[:, i * 128:(i + 1) * 128], [rk], [sk])
        cp(eng, dst, slot[:, 0:n * 128].rearrange("p (n c) -> p n c", n=n), [sk], [wk])

    m0 = kb.mark()
    c32 = kb.sbuf("c32", [128, 32], F32); cact = kb.sbuf("cact", [128, 32], BF16)
    dma("sp", c32[:], cT, [], ["c32"])
    act(cact[:], c32[:], AF.Silu, ["c32"], ["cact"])
    w_ada_v = w_ada.rearrange("(k p) e -> p k e", p=128)
    wring = Ring(kb, "wada", 2, [128, 32, 512], BF16)
    brow = Ring(kb, "brow", 2, [1, 512], F32); mrow = Ring(kb, "mrow", 2, [1, 512], F32)
    p0 = PRing([pb4[:], pb5[:]], ["pb4", "pb5"])
    p0b = PRing([pb6[:]], ["pb6"])
    one32 = kb.sbuf("one32", [1, 1], F32)
    memset("dve", one32[:], 1.0, ["one32"])
    wring32 = Ring(kb, "wada32", 2, [128, 32, 512], F32)
    cact32 = kb.sbuf("cact32", [128, 32], F32)
    act(cact32[:], c32[:], AF.Silu, ["c32"], ["cact32"])
    for g in range(48):
        ps, pk = p0.get()
        if g % 2 == 0:
            wt, wk = wring.get()
            dma("pool", wt[:], w_ada_v[:, :, g * 512:(g + 1) * 512], [], [wk])
            for k in range(32):
                mm(ps[0:1, :], cact[:, k:k + 1], wt[:, k, :], k == 0, k == 31, [wk, "cact"], [pk])
        else:
            wt, wk = wring32.get()
            dma("sp", wt[:], w_ada_v[:, :, g * 512:(g + 1) * 512], [], [wk])
            for k in range(32):
                mm(ps[0:1, :], cact32[:, k:k + 1], wt[:, k, :], k == 0, k == 31, [wk, "cact32"], [pk])
        bt, bk = brow.get(); mt, mk = mrow.get()
        dma("sp", bt[:], b_ada[0:1, g * 512:(g + 1) * 512], [], [bk])
        tt("dve", mt[:], ps[0:1, :], bt[:], ALU.add, [pk, bk], [mk])
        ps2, pk2 = p0b.get()
        for i in range(4):
            mm(ps2[:, i:i + 1], mt[0:1, i * 128:(i + 1) * 128], one32[0:1, 0:1], True, True, [mk, "one32"], [pk2])
        cp("act", modT[:, g * 4:(g + 1) * 4], ps2[:, 0:4], [pk2], ["modT"])
    gm = kb.sbuf("gm", [128, 32], F32); gf = kb.sbuf("gf", [128, 32], F32)
    dma("sp", gm[:], g_mix, [], ["gm"]); dma("sp", gf[:], g_ffn, [], ["gf"])
    stt("dve", A1[:], modT[:, 32:64], 1.0, gm[:], ALU.add, ALU.mult, ["modT", "gm"], ["A1"])
    stt("dve", A2[:], modT[:, 128:160], 1.0, gf[:], ALU.add, ALU.mult, ["modT", "gf"], ["A2"])
    kb.barrier(); kb.free_to(m0)
    if stages <= 0:
        return finish(nc, kb, outT)

    def phase_norm(xT_v, ntok, A, Bv, dst_v, dkey, gvec=None):
        m = kb.mark()
        xr = Ring(kb, "xr", 2, [128, 32, 256], F32); sqr = Ring(kb, "sqr", 2, [128, 32, 256], BF16)
        hr = Ring(kb, "hr", 2, [128, 32, 256], BF16 if dst_v.dtype == BF16 else F32)
        rr = Ring(kb, "rr", 2, [128, 256], F32)
        pr = PRing([pb4[:], pb5[:]], ["pb4", "pb5"])
        for ti in range(ntok // 256):
            xt, kx = xr.get(); sq, ksq = sqr.get(); ht, kh = hr.get(); rt, krt = rr.get(); ps, pk = pr.get()
            dma("sp", xt[:], xT_v[:, :, ti * 256:(ti + 1) * 256], [], [kx])
            act(sq[:], xt[:], AF.Square, [kx], [ksq])
            for k in range(32):
                mm(ps[:, 0:256], ones[:], sq[:, k, :], k == 0, k == 31, [ksq, "ones"], [pk])
            act(rt[:], ps[:, 0:256], AF.Sqrt, [pk, "eps"], [krt], bias=eps_t[:, 0:1], scale=1.0 / 4096)
            recip(rt[:], rt[:], [krt], [krt])
            tt("dve", xt[:], xt[:], rt[:].unsqueeze(1).to_broadcast([128, 32, 256]), ALU.mult, [kx, krt], [kx])
            if Bv is not None:
                tt("pool", xt[:], xt[:], A.unsqueeze(2).to_broadcast([128, 32, 256]), ALU.mult, [kx, "A1", "A2", "modT"], [kx])
                tt("dve", ht[:], xt[:], Bv.unsqueeze(2).to_broadcast([128, 32, 256]), ALU.add, [kx, "modT"], [kh])
            else:
                tt("pool", ht[:], xt[:], A.unsqueeze(2).to_broadcast([128, 32, 256]), ALU.mult, [kx, "gfin"], [kh])
            dma("sp", dst_v[:, :, ti * 256:(ti + 1) * 256], ht[:], [kh], [dkey])
        kb.barrier(); kb.free_to(m)

    xk_v = xkT.rearrange("(k p) t -> p k t", p=128)
    xq_v = xqT.rearrange("(k p) t -> p k t", p=128)
    phase_norm(xk_v, 2048, A1[:], modT[:, 0:32], hk_d, "hk_d")
    phase_norm(xq_v, 1024, A1[:], modT[:, 0:32], hq_d, "hq_d")
    if stages <= 1:
        return finish(nc, kb, outT)

    w_in_v = w_in.rearrange("(k p) e -> p k e", p=128)

    def bc_load(name, src, n):
        t = kb.sbuf(name, [128, n], F32)
        dma("sp", t[:], src.partition_broadcast(128), [], [name])
        return t

    m2 = kb.mark()
    gckv_t = bc_load("gckv_t", g_ckv, 512); gki_t = bc_load("gki_t", g_ki, 128); bki_t = bc_load("bki_t", b_ki, 128)
    hT = kb.sbuf("hT", [128, 32, 1024], BF16)
    wr = Ring(kb, "wr", 2, [128, 32, 512], BF16)
    cos_t = kb.sbuf("cos_t", [128, 8, 128], F32); sin_t = kb.sbuf("sin_t", [128, 8, 128], F32); dec_t = kb.sbuf("dec_t", [128, 8, 8], F32)
    t512 = Ring(kb, "t512", 2, [128, 512], F32); u512 = Ring(kb, "u512", 2, [128, 512], F32)
    b512 = Ring(kb, "b512", 3, [128, 512], BF16); b512b = Ring(kb, "b512b", 2, [128, 512], BF16)
    trb = Ring(kb, "trb", 3, [128, 4, 128], BF16)
    sm = Ring(kb, "sm", 4, [128, 2], F32)
    pr2 = PRing(big + [pb4[:], pb5[:], pb6[:]], bigk + ["pb4", "pb5", "pb6"])

    def gemm_tm(hTt, col0, ncols, consumer):
        wt, wk = wr.get()
        dma("pool", wt[:, :, 0:ncols], w_in_v[:, :, col0:col0 + ncols], [], [wk])
        pend = None
        for tti in range(8):
            ps, pk = pr2.get()
            for k in range(32):
                mm(ps[:, 0:ncols], hTt[:, k, tti * 128:(tti + 1) * 128], wt[:, k, 0:ncols], k == 0, k == 31, [wk, "hT"], [pk])
            if pend is not None:
                pend()
            pend = consumer(tti, ps, pk)
        if pend is not None:
            pend()

    def rms_rows(ps_ap, pk, n, gtab, gk, outbf, ok, extra_reads=()):
        t, tk = t512.get(); s, sk = sm.get()
        act(t[:, 0:n], ps_ap, AF.Square, [pk] + list(extra_reads), [tk])
        rsum(s[:, 0:1], t[:, 0:n], [tk], [sk])
        act(s[:, 0:1], s[:, 0:1], AF.Sqrt, [sk, "eps"], [sk], bias=eps_t[:, 0:1], scale=1.0 / n)
        recip(s[:, 0:1], s[:, 0:1], [sk], [sk])
        stt("dve", outbf, ps_ap, s[:, 0:1], gtab, ALU.mult, ALU.mult, [pk, sk, gk] + list(extra_reads), [ok])

    def rotary(psv_ap, pk, cosr, sinr, tabk, ra, rak, rb, rbk):
        tt("dve", ra, psv_ap, cosr.unsqueeze(1).to_broadcast([128, 4, 128]), ALU.mult, [pk, tabk], [rak])
        tt("dve", rb[:, :, 0:64], psv_ap[:, :, 64:128], sinr[:, 0:64].unsqueeze(1).to_broadcast([128, 4, 64]), ALU.mult, [pk, tabk], [rbk])
        tt("dve", rb[:, :, 64:128], psv_ap[:, :, 0:64], sinr[:, 64:128].unsqueeze(1).to_broadcast([128, 4, 64]), ALU.mult, [pk, tabk], [rbk])
        tt("pool", ra, ra, rb, ALU.add, [rak, rbk], [rak])

    for half in range(2):
        dma("sp", hT[:], hk_d[:, :, half * 1024:(half + 1) * 1024], ["hk_d"], ["hT"])
        dma("sp", cos_t[:], cosk[half * 1024:(half + 1) * 1024, :].rearrange("(t p) c -> p t c", p=128), [], ["ktab"])
        dma("sp", sin_t[:], sink[half * 1024:(half + 1) * 1024, :].rearrange("(t p) c -> p t c", p=128), [], ["ktab"])
        dma("sp", dec_t[:], kdec_tab[half * 1024:(half + 1) * 1024, :].rearrange("(t p) c -> p t c", p=128), [], ["ktab"])

        def c_ckv(tti, ps, pk):
            T = half * 8 + tti
            cn, ck = b512.get()
            rms_rows(ps[:, 0:512], pk, 512, gckv_t[:], "gckv_t", cn[:], ck)
            dma("sp", ckv_tm_d[T * 128:(T + 1) * 128, :], cn[:], [ck], ["ckv_tm_d"])

            def tail():
                ct, ctk = trb.get()
                transposes(ct[:], cn[:], 4, ck, ctk)
                dma("sp", ckvT_d[:, :, T * 128:(T + 1) * 128], ct[:], [ctk], ["ckvT_d"])
            return tail

        def c_ki(tti, ps, pk):
            T = half * 8 + tti
            s, sk = sm.get(); xc, xk_ = t512.get(); sq, sqk = u512.get(); kn, knk = b512.get()
            rsum(s[:, 0:1], ps[:, 0:128], [pk], [sk])
            amul(s[:, 0:1], s[:, 0:1], 1.0 / 128, [sk], [sk])
            ts("dve", xc[:, 0:128], ps[:, 0:128], s[:, 0:1], ALU.subtract, [pk, sk], [xk_])
            act(sq[:, 0:128], xc[:, 0:128], AF.Square, [xk_], [sqk])
            rsum(s[:, 1:2], sq[:, 0:128], [sqk], [sk])
            act(s[:, 1:2], s[:, 1:2], AF.Sqrt, [sk, "eps"], [sk], bias=eps_t[:, 0:1], scale=1.0 / 128)
            recip(s[:, 1:2], s[:, 1:2], [sk], [sk])
            stt("dve", xc[:, 0:128], xc[:, 0:128], s[:, 1:2], gki_t[:], ALU.mult, ALU.mult, [xk_, sk, "gki_t"], [xk_])
            tt("dve", kn[:, 0:128], xc[:, 0:128], bki_t[:], ALU.add, [xk_, "bki_t"], [knk])
            def tail():
                ct, ctk = trb.get()
                transposes(ct[:, 0:1, :], kn[:, 0:128], 1, knk, ctk)
                dma("sp", kidxT_d[:, T * 128:(T + 1) * 128], ct[:, 0, :], [ctk], ["kidxT_d"])
            return tail

        def mk_rk(hg):
            def c_rk(tti, ps, pk):
                T = half * 8 + tti
                ra, rak = t512.get(); rb, rbk = u512.get(); kr, krk = b512.get(); kd, kdk = b512b.get()
                rav = ra[:].rearrange("p (h d) -> p h d", h=4); rbv = rb[:].rearrange("p (h d) -> p h d", h=4)
                rotary(ps[:, 0:512].rearrange("p (h d) -> p h d", h=4), pk, cos_t[:, tti, :], sin_t[:, tti, :], "ktab", rav, rak, rbv, rbk)
                cp("act", kr[:], ra[:], [rak], [krk])
                tt("dve", kd[:].rearrange("p (h d) -> p h d", h=4), rav,
                   dec_t[:, tti, hg * 4:(hg + 1) * 4].unsqueeze(2).to_broadcast([128, 4, 128]), ALU.mult, [rak, "ktab"], [kdk])
                dma("sp", kdec_d[T * 128:(T + 1) * 128, hg * 512:(hg + 1) * 512], kd[:], [kdk], ["kdec_d"])

                def tail():
                    ct, ctk = trb.get()
                    transposes(ct[:], kr[:], 4, krk, ctk)
                    dma("sp", kT_d[:, hg * 4:(hg + 1) * 4, T * 128:(T + 1) * 128], ct[:], [ctk], ["kT_d"])
                return tail
            return c_rk

        def mk_rv(i):
            def c_rv(tti, ps, pk):
                T = half * 8 + tti
                vb, vk = b512.get()
                cp("act", vb[:], ps[:, 0:512], [pk], [vk])
                dma("sp", v_d[T * 128:(T + 1) * 128, i * 512:(i + 1) * 512], vb[:], [vk], ["v_d"])
            return c_rv

        gemm_tm(hT, O_CKV, 512, c_ckv)
        gemm_tm(hT, O_KI, 128, c_ki)
        for hg in range(2):
            gemm_tm(hT, O_RK + hg * 512, 512, mk_rk(hg))
        for i in range(4):
            gemm_tm(hT, O_RV + i * 512, 512, mk_rv(i))

    gcq_t = bc_load("gcq_t", g_cq, 1024)
    cq_raw = kb.sbuf("cq_raw", [128, 8, 1024], F32)
    t1024 = kb.sbuf("t1024", [128, 1024], F32); cqn = Ring(kb, "cqn", 2, [128, 1024], BF16)
    trb8 = Ring(kb, "trb8", 2, [128, 8, 128], BF16)
    w64 = Ring(kb, "w64", 2, [128, 64], F32)
    dma("sp", hT[:], hq_d, ["hq_d"], ["hT"])
    dma("sp", cos_t[:], cosq.rearrange("(t p) c -> p t c", p=128), [], ["ktab"])
    dma("sp", sin_t[:], sinq.rearrange("(t p) c -> p t c", p=128), [], ["ktab"])
    dma("sp", dec_t[:], qdec_tab.rearrange("(t p) c -> p t c", p=128), [], ["ktab"])

    def mk_cq(g):
        def c_cq(tti, ps, pk):
            cp("act", cq_raw[:, tti, g * 512:(g + 1) * 512], ps[:, 0:512], [pk], [f"cq_raw{tti}"])
        return c_cq

    def c_wi(tti, ps, pk):
        wt_, wk_ = w64.get()
        amul(wt_[:], ps[:, 0:64], (64 ** -0.5) * (128 ** -0.5), [pk], [wk_])
        dma("sp", wi_d[tti * 128:(tti + 1) * 128, :], wt_[:], [wk_], ["wi_d"])

    def mk_rq(hg):
        def c_rq(tti, ps, pk):
            ra, rak = t512.get(); rb, rbk = u512.get(); qr, qrk = b512.get(); qd, qdk = b512b.get()
            rav = ra[:].rearrange("p (h d) -> p h d", h=4); rbv = rb[:].rearrange("p (h d) -> p h d", h=4)
            rotary(ps[:, 0:512].rearrange("p (h d) -> p h d", h=4), pk, cos_t[:, tti, :], sin_t[:, tti, :], "ktab", rav, rak, rbv, rbk)
            cp("act", qr[:], ra[:], [rak], [qrk])
            tt("dve", qd[:].rearrange("p (h d) -> p h d", h=4), rav,
               dec_t[:, tti, hg * 4:(hg + 1) * 4].unsqueeze(2).to_broadcast([128, 4, 128]), ALU.mult, [rak, "ktab"], [qdk])
            def tail():
                ct, ctk = trb.get()
                transposes(ct[:], qr[:], 4, qrk, ctk)
                dma("sp", qT_d[:, hg * 4:(hg + 1) * 4, tti * 128:(tti + 1) * 128], ct[:], [ctk], ["qT_d"])
                ct2, ctk2 = trb.get()
                transposes(ct2[:], qd[:], 4, qdk, ctk2)
                dma("sp", qdT_d[:, hg * 4:(hg + 1) * 4, tti * 128:(tti + 1) * 128], ct2[:], [ctk2], ["qdT_d"])
            return tail
        return c_rq

    def mk_rg(i):
        def c_rg(tti, ps, pk):
            gb, gk_ = b512.get()
            act(gb[:], ps[:, 0:512], AF.Silu, [pk], [gk_])
            dma("sp", rg_d[tti * 128:(tti + 1) * 128, i * 512:(i + 1) * 512], gb[:], [gk_], ["rg_d"])
        return c_rg

    for g in range(2):
        gemm_tm(hT, O_CQ + g * 512, 512, mk_cq(g))
    for tti in range(8):
        s, sk = sm.get(); cn, cnk = cqn.get()
        act(t1024[:], cq_raw[:, tti, :], AF.Square, [f"cq_raw{tti}"], ["t1024"])
        rsum(s[:, 0:1], t1024[:], ["t1024"], [sk])
        act(s[:, 0:1], s[:, 0:1], AF.Sqrt, [sk, "eps"], [sk], bias=eps_t[:, 0:1], scale=1.0 / 1024)
        recip(s[:, 0:1], s[:, 0:1], [sk], [sk])
        stt("dve", cn[:], cq_raw[:, tti, :], s[:, 0:1], gcq_t[:], ALU.mult, ALU.mult, [f"cq_raw{tti}", sk, "gcq_t"], [cnk])
        ct, ctk = trb8.get()
        transposes(ct[:, 0:4, :], cn[:, 0:512], 4, cnk, ctk)
        transposes(ct[:, 4:8, :], cn[:, 512:1024], 4, cnk, ctk)
        dma("sp", cqT_d[:, :, tti * 128:(tti + 1) * 128], ct[:], [ctk], ["cqT_d"])
    gemm_tm(hT, O_WI, 64, c_wi)
    for hg in range(2):
        gemm_tm(hT, O_RQ + hg * 512, 512, mk_rq(hg))
    for i in range(4):
        gemm_tm(hT, O_RG + i * 512, 512, mk_rg(i))
    kb.barrier(); kb.free_to(m2)
    if stages <= 2:
        return finish(nc, kb, outT)

    m3 = kb.mark()
    mret = kb.sbuf("mret", [128, 2, 8, 128], F32)
    dma("sp", mret[:], mret_in, [], ["mret"])
    gret_t = bc_load("gret_t", g_ret, 2048)
    S = kb.sbuf("S", [128, 8, 256], F32); Sbf = kb.sbuf("Sbf", [128, 8, 256], BF16)
    memset("dve", S[:], 0.0, ["S"]); memset("pool", Sbf[:], 0.0, ["Sbf"])
    qTr = Ring(kb, "qTr", 2, [128, 8, 128], BF16); qdr = Ring(kb, "qdr", 2, [128, 8, 128], BF16)
    kTr = Ring(kb, "kTr", 2, [128, 8, 256], BF16); kdr = Ring(kb, "kdr", 2, [128, 2, 1024], BF16)
    vr = Ring(kb, "vr", 2, [128, 2, 2048], BF16); rgr = Ring(kb, "rgr", 2, [128, 2048], BF16)
    ATr = Ring(kb, "ATr", 3, [128, 2, 128], BF16)
    ysq = kb.sbuf("ysq", [128, 2048], F32); yn = kb.sbuf("yn", [128, 2048], F32); yrb = Ring(kb, "yrb", 2, [128, 2048], BF16)
    ss8 = Ring(kb, "ss8", 2, [128, 8], F32)
    yrT = Ring(kb, "yrT", 2, [128, 16, 128], BF16)
    psS = PRing([pb4[:], pb5[:]], ["pb4", "pb5"])
    psK = PRing([pb6[:]], ["pb6"])
    g256 = [math.exp(256.0 * LOGG[h]) for h in range(8)]
    for j in range(8):
        qt, qk = qTr.get(); qd, qdk = qdr.get(); kt_, kk = kTr.get(); kd, kdk = kdr.get(); vt, vk = vr.get(); rg, rgk = rgr.get()
        dma("sp", qt[:], qT_d[:, :, j * 128:(j + 1) * 128], ["qT_d"], [qk])
        dma("sp", qd[:], qdT_d[:, :, j * 128:(j + 1) * 128], ["qdT_d"], [qdk])
        dma("sp", kt_[:], kT_d[:, :, j * 256:(j + 1) * 256], ["kT_d"], [kk])
        dma("sp", kd[:], kdec_d[j * 256:(j + 1) * 256, :].rearrange("(s p) c -> p s c", p=128), ["kdec_d"], [kdk])
        dma("sp", vt[:], v_d[j * 256:(j + 1) * 256, :].rearrange("(s p) c -> p s c", p=128), ["v_d"], [vk])
        dma("sp", rg[:], rg_d[j * 128:(j + 1) * 128, :], ["rg_d"], [rgk])
        for h in range(8):
            ps, pk = psS.get(); at, atk = ATr.get()
            mm(ps[:, 0:128], kt_[:, h, 0:128], qt[:, h, :], True, True, [kk, qk], [pk])
            mm(ps[:, 128:256], kt_[:, h, 128:256], qt[:, h, :], True, True, [kk, qk], [pk])
            tt("dve", at[:], ps[:, 0:256].rearrange("p (s t) -> p s t", s=2), mret[:, :, h, :], ALU.mult, [pk, "mret"], [atk])
            yk = bigk[h // 2]
            yo = pbig[:, h * 256:(h + 1) * 256]
            mm(yo, at[:, 0, :], vt[:, 0, h * 256:(h + 1) * 256], True, False, [atk, vk], [yk])
            mm(yo, at[:, 1, :], vt[:, 1, h * 256:(h + 1) * 256], False, False, [atk, vk], [yk])
            mm(yo, qd[:, h, :], Sbf[:, h, :], False, True, [qdk, "Sbf"], [yk])
        s8, s8k = ss8.get(); yb, ybk = yrb.get()
        act(ysq[:], pbig[:, :], AF.Square, bigk, ["ysq"])
        rsum(s8[:], ysq[:].rearrange("p (h v) -> p h v", h=8), ["ysq"], [s8k])
        act(s8[:], s8[:], AF.Sqrt, [s8k, "eps"], [s8k], bias=eps_t[:, 0:1], scale=1.0 / 256)
        recip(s8[:], s8[:], [s8k], [s8k])
        tt("dve", yn[:].rearrange("p (h v) -> p h v", h=8), pbig[:, :].rearrange("p (h v) -> p h v", h=8),
           s8[:].unsqueeze(2).to_broadcast([128, 8, 256]), ALU.mult, bigk + [s8k], ["yn"])
        tt("pool", yn[:], yn[:], gret_t[:], ALU.mult, ["yn", "gret_t"], ["yn"])
        tt("dve", yb[:], yn[:], rg[:], ALU.mult, ["yn", rgk], [ybk])
        yT, yTk = yrT.get()
        for q4 in range(4):
            transposes(yT[:, q4 * 4:(q4 + 1) * 4, :], yb[:, q4 * 512:(q4 + 1) * 512], 4, ybk, yTk)
        dma("sp", yrT_d[:, :, j * 128:(j + 1) * 128], yT[:], [yTk], ["yrT_d"])
        if j < 7:
            for h in range(8):
                ps, pk = psK.get()
                mm(ps[:, 0:256], kd[:, 0, h * 128:(h + 1) * 128], vt[:, 0, h * 256:(h + 1) * 256], True, False, [kdk, vk], [pk])
                mm(ps[:, 0:256], kd[:, 1, h * 128:(h + 1) * 128], vt[:, 1, h * 256:(h + 1) * 256], False, True, [kdk, vk], [pk])
                stt("dve", S[:, h, :], S[:, h, :], g256[h], ps[:, 0:256], ALU.mult, ALU.add, [pk, "S"], ["S"])
            cp("act", Sbf[:], S[:], ["S"], ["Sbf"])
    kb.barrier(); kb.free_to(m3)
    if stages <= 3:
        return finish(nc, kb, outT)

    m4 = kb.mark()
    maskT = kb.sbuf("maskT", [128, 72, 128], BF16)
    qaT = kb.sbuf("qaT", [128, 16, 1024], BF16)
    mA = kb.mark()
    cqT = kb.sbuf("cqT", [128, 8, 1024], BF16)
    dma("sp", cqT[:], cqT_d, ["cqT_d"], ["cqT"])
    kidxT = kb.sbuf("kidxT", [128, 2048], BF16)
    dma("sp", kidxT[:], kidxT_d, ["kidxT_d"], ["kidxT"])
    wi_t = kb.sbuf("wi_t", [128, 8, 64], F32)
    dma("sp", wi_t[:], wi_d.rearrange("(t p) c -> p t c", p=128), ["wi_d"], ["wi_t"])
    moff = [sum(2 * jj + 2 for jj in range(j)) for j in range(8)]
    m4b = kb.mark()
    acc = [kb.sbuf(f"acc{j}", [128, (2 * j + 2) * 128], F32) for j in range(8)]
    wqr = Ring(kb, "wqr", 1, [128, 8, 1024], BF16)
    qiT = Ring(kb, "qiT", 2, [128, 8, 1024], BF16)
    rl = Ring(kb, "rl", 3, [128, 512], F32)
    w_qi_v = w_qi.rearrange("(k p) e -> p k e", p=128)
    pr4 = PRing(big + [pb4[:], pb5[:], pb6[:]], bigk + ["pb4", "pb5", "pb6"])
    for hgp in range(8):
        wq, wqk = wqr.get(); qi, qik = qiT.get()
        dma("pool", wq[:], w_qi_v[:, :, hgp * 1024:(hgp + 1) * 1024], [], [wqk])
        for hh in range(8):
            for half in range(2):
                ps, pk = pr4.get()
                for k in range(8):
                    mm(ps[:, :], wq[:, k, hh * 128:(hh + 1) * 128], cqT[:, k, half * 512:(half + 1) * 512], k == 0, k == 7, [wqk, "cqT"], [pk])
                cp("act" if half == 0 else "dve", qi[:, hh, half * 512:(half + 1) * 512], ps[:, :], [pk], [qik])
        for j in range(8):
            W = (2 * j + 2) * 128
            for hh in range(8):
                hd = hgp * 8 + hh
                for s0 in range(0, W, 512):
                    n = min(512, W - s0)
                    ps, pk = pr4.get(); r_, rk_ = rl.get()
                    mm(ps[:, 0:n], qi[:, hh, j * 128:(j + 1) * 128], kidxT[:, s0:s0 + n], True, True, [qik, "kidxT"], [pk])
                    act(r_[:, 0:n], ps[:, 0:n], AF.Relu, [pk], [rk_])
                    if hd == 0:
                        ts("dve", acc[j][:, s0:s0 + n], r_[:, 0:n], wi_t[:, j, hd:hd + 1], ALU.mult, [rk_, "wi_t"], [f"acc{j}"])
                    else:
                        stt("dve", acc[j][:, s0:s0 + n], r_[:, 0:n], wi_t[:, j, hd:hd + 1], acc[j][:, s0:s0 + n], ALU.mult, ALU.add,
                            [rk_, "wi_t", f"acc{j}"], [f"acc{j}"])
    negb = kb.sbuf("negb", [128, 256], F32); adm01 = kb.sbuf("adm01", [128, 256], F32)
    dma("sp", negb[:], negb_in, [], ["negb"]); dma("sp", adm01[:], adm01_in, [], ["adm01"])
    wk0 = kb.sbuf("wk0", [128, 2048], F32); wk1 = kb.sbuf("wk1", [128, 2048], F32)
    mx8 = Ring(kb, "mx8", 2, [128, 8], F32)
    mrow_ = kb.sbuf("mrow_", [128, 2048], BF16)
    for j in range(8):
        W = (2 * j + 2) * 128
        a = acc[j]; ak = f"acc{j}"
        tt("dve", a[:, W - 256:W], a[:, W - 256:W], negb[:], ALU.add, [ak, "negb"], [ak])
        src, srck = a, ak
        bufs = [(wk0, "wk0"), (wk1, "wk1")]
        for it in range(32):
            mx, mxk = mx8.get()
            vmax(mx[:], src[:, 0:W], [srck], [mxk])
            if it < 31:
                dst, dstk = bufs[it % 2]
                mrep(dst[:, 0:W], mx[:], src[:, 0:W], [srck, mxk], [dstk])
                src, srck = dst, dstk
        ts("dve", mrow_[:, 0:W], a[:, 0:W], mx[:, 7:8], ALU.is_ge, [ak, mxk], ["mrow_"])
        tt("dve", mrow_[:, W - 256:W], mrow_[:, W - 256:W], adm01[:], ALU.mult, ["mrow_", "adm01"], ["mrow_"])
        for kt0 in range(0, 2 * j + 2, 4):
            n = min(4, 2 * j + 2 - kt0)
            transposes(maskT[:, moff[j] + kt0:moff[j] + kt0 + n, :], mrow_[:, kt0 * 128:(kt0 + n) * 128], n, "mrow_", "maskT")
    kb.barrier(); kb.free_to(m4b)
    if stages <= 4:
        return finish(nc, kb, outT)

    wuq = kb.sbuf("wuq", [128, 8, 2048], BF16)
    dma("pool", wuq[:], w_uq.rearrange("(k p) e -> p k e", p=128), [], ["wuq"])
    dma("sp", cqT[:], cqT_d, ["cqT_d"], ["cqT"])
    pr5 = PRing([pb4[:], pb5[:], pb6[:]], ["pb4", "pb5", "pb6"])
    for h in range(16):
        for half in range(2):
            ps, pk = pr5.get()
            for k in range(8):
                mm(ps[:, :], wuq[:, k, h * 128:(h + 1) * 128], cqT[:, k, half * 512:(half + 1) * 512], k == 0, k == 7, ["wuq", "cqT"], [pk])
            cp("act" if half == 0 else "dve", qaT[:, h, half * 512:(half + 1) * 512], ps[:, :], [pk], ["qaT"])
    kb.barrier(); kb.free_to(mA)
    ckvT = kb.sbuf("ckvT", [128, 4, 2048], BF16); ckv_tm = kb.sbuf("ckv_tm", [128, 16, 512], BF16)
    dma("sp", ckvT[:], ckvT_d, ["ckvT_d"], ["ckvT"])
    dma("sp", ckv_tm[:], ckv_tm_d.rearrange("(t p) c -> p t c", p=128), ["ckv_tm_d"], ["ckv_tm"])
    wukT = kb.sbuf("wukT", [128, 16, 512], BF16); wuv = kb.sbuf("wuv", [128, 4, 2048], BF16)
    dma("pool", wukT[:], w_ukT, [], ["wukT"])
    dma("pool", wuv[:], w_uv.rearrange("(k p) e -> p k e", p=128), [], ["wuv"])
    tb_sb = kb.sbuf("tb_sb", [32, 16], F32); oh_sb = kb.sbuf("oh_sb", [32, 512], F32); t15_sb = kb.sbuf("t15_sb", [16, 1], F32)
    dma("sp", tb_sb[:], t5b, [], ["tb_sb"]); dma("sp", oh_sb[:], oh_rel, [], ["oh_sb"]); dma("sp", t15_sb[:], t15, [], ["t15_sb"])
    amul(t15_sb[:], t15_sb[:], -1.0, ["t15_sb"], ["t15_sb"])
    mm(pb4[0:16, :], tb_sb[:], oh_sb[:], True, True, ["tb_sb", "oh_sb"], ["pb4"])
    ef_sb = kb.sbuf("ef_sb", [16, 512], F32)
    act(ef_sb[:], pb4[0:16, :], AF.Exp, ["pb4", "t15_sb"], ["ef_sb"], bias=t15_sb[:, 0:1], scale=1.0)
    dma("sp", ef_d, ef_sb[:], ["ef_sb"], ["ef_d"])
    EBp = kb.sbuf("EBp", [128, 3, 16, 128], BF16)
    mE = kb.mark()
    hk32 = kb.sbuf("hk32", [128, 16, 128], F32); hkb = kb.sbuf("hkb", [128, 16, 128], BF16)
    for n in range(3):
        src_ap = bass.AP(ef_d.tensor, n * 128, [[1, 128], [512, 16], [1, 128]])
        dma("sp", hk32[:], src_ap, ["ef_d"], ["hk32"])
        cp("dve", hkb[:], hk32[:], ["hk32"], ["hkb"])
        for h4 in range(4):
            ps, pk = pr5.get()
            for i in range(4):
                mm(ps[:, i * 128:(i + 1) * 128], hkb[:, h4 * 4 + i, :], anti[:], True, True, ["hkb", "anti"], [pk])
            cp("act", EBp[:, n, h4 * 4:(h4 + 1) * 4, :], ps[:, :].rearrange("p (h t) -> p h t", h=4), [pk], ["EBp"])
    kb.barrier(); kb.free_to(mE)
    qlat = Ring(kb, "qlat", 1, [128, 4, 16, 128], BF16)
    MBn = Ring(kb, "MBn", 1, [128, 3, 16, 128], BF16)
    Er = Ring(kb, "Er", 3, [128, 512], F32)
    PTr = Ring(kb, "PTr", 3, [128, 512], BF16)
    rz = Ring(kb, "rz", 2, [128, 512], F32)
    olat = Ring(kb, "olat", 1, [128, 4, 16, 128], BF16)
    yaT = Ring(kb, "yaT", 2, [128, 16, 128], BF16)
    prL = PRing([pb5[:], pb6[:]], ["pb5", "pb6"])
    for j in range(8):
        ql, qlk = qlat.get()
        for cc in range(4):
            for h4 in range(4):
                ps, pk = prL.get()
                for i in range(4):
                    h = h4 * 4 + i
                    mm(ps[:, i * 128:(i + 1) * 128], wukT[:, h, cc * 128:(cc + 1) * 128], qaT[:, h, j * 128:(j + 1) * 128], True, True, ["wukT", "qaT"], [pk])
                amul(ql[:, cc, h4 * 4:(h4 + 1) * 4, :], ps[:, :].rearrange("p (h t) -> p h t", h=4), 128 ** -0.5, [pk], [qlk])
        nk = 2 * j + 2
        near = [kt for kt in (2 * j - 1, 2 * j, 2 * j + 1) if kt >= 0]
        mb, mbk = MBn.get()
        for kt in near:
            n = kt - (2 * j - 1)
            tt("pool", mb[:, n, :, :], EBp[:, n, :, :], maskT[:, moff[j] + kt, :].unsqueeze(1).to_broadcast([128, 16, 128]), ALU.mult, ["EBp", "maskT"], [mbk])
        ol, olk = olat.get()
        pend5 = None
        for hg in range(4):
            for kt in range(nk):
                ps, pk = prL.get(); E, Ek = Er.get(); PT, PTk = PTr.get()
                for cc in range(4):
                    mm(ps[:, :], ckvT[:, cc, kt * 128:(kt + 1) * 128], ql[:, cc, hg * 4:(hg + 1) * 4, :], cc == 0, cc == 3, ["ckvT", qlk], [pk])
                act(E[:], ps[:, :], AF.Exp, [pk], [Ek])
                if kt in near:
                    n = kt - (2 * j - 1)
                    tt("dve", PT[:].rearrange("p (h t) -> p h t", h=4), E[:].rearrange("p (h t) -> p h t", h=4), mb[:, n, hg * 4:(hg + 1) * 4, :], ALU.mult, [Ek, mbk], [PTk])
                else:
                    tt("dve", PT[:].rearrange("p (h t) -> p h t", h=4), E[:].rearrange("p (h t) -> p h t", h=4),
                       maskT[:, moff[j] + kt, :].unsqueeze(1).to_broadcast([128, 4, 128]), ALU.mult, [Ek, "maskT"], [PTk])
                if pend5 is not None:
                    pend5()

                def tail5(kt=kt, PT=PT, PTk=PTk, nk=nk):
                    for cc in range(4):
                        mm(big[cc], ckv_tm[:, kt, cc * 128:(cc + 1) * 128], PT[:], kt == 0, kt == nk - 1, ["ckv_tm", PTk], [bigk[cc]])
                    mm(pb4[:, :], ones[:], PT[:], kt == 0, kt == nk - 1, ["ones", PTk], ["pb4"])
                pend5 = tail5
            pend5(); pend5 = None
            r_, rk_ = rz.get()
            recip(r_[:], pb4[:, :], ["pb4"], [rk_])
            for cc in range(4):
                tt("dve", ol[:, cc, hg * 4:(hg + 1) * 4, :], big[cc].rearrange("p (h t) -> p h t", h=4), r_[:].rearrange("p (h t) -> p h t", h=4), ALU.mult, [bigk[cc], rk_], [olk])
        ya, yak = yaT.get()
        for h4 in range(4):
            ps, pk = prL.get()
            for i in range(4):
                h = h4 * 4 + i
                for cc in range(4):
                    mm(ps[:, i * 128:(i + 1) * 128], wuv[:, cc, h * 128:(h + 1) * 128], ol[:, cc, h, :], cc == 0, cc == 3, ["wuv", olk], [pk])
            cp("act", ya[:, h4 * 4:(h4 + 1) * 4, :], ps[:, :].rearrange("p (h t) -> p h t", h=4), [pk], [yak])
        dma("sp", yaT_d[:, :, j * 128:(j + 1) * 128], ya[:], [yak], ["yaT_d"])
    kb.barrier(); kb.free_to(m4)
    if stages <= 5:
        return finish(nc, kb, outT)

    def gemm_fm(actT, akey, nk, w_view, col0, wring_, consumer):
        wt, wk = wring_.get()
        dma("pool", wt[:, 0:nk, :], w_view[:, :, col0:col0 + 512], [], [wk])
        for cc in range(4):
            for half in range(2):
                ps, pk = prF.get()
                for k in range(nk):
                    mm(ps[:, :], wt[:, k, cc * 128:(cc + 1) * 128], actT[:, k, half * 512:(half + 1) * 512], k == 0, k == nk - 1, [wk, akey], [pk])
                consumer(col0 // 128 + cc, half, ps, pk)

    prF = PRing(big + [pb4[:], pb5[:], pb6[:]], bigk + ["pb4", "pb5", "pb6"])
    m6 = kb.mark()
    actA = kb.sbuf("actA", [128, 32, 1024], BF16)
    wrF = Ring(kb, "wrF", 2, [128, 32, 512], BF16)
    bg = kb.sbuf("bg", [128, 64], F32)
    dma("sp", bg[:], b_gate, [], ["bg"])
    dma("sp", actA[:], hq_d, ["hq_d"], ["actA"])
    o512 = Ring(kb, "o512", 3, [128, 512], BF16)
    w_gate_v = w_gate.rearrange("(k p) e -> p k e", p=128)

    def c_gate(gc, half, ps, pk):
        o, ok = o512.get()
        act(o[:], ps[:, :], AF.Sigmoid, [pk, "bg"], [ok], bias=bg[:, gc:gc + 1], scale=1.0)
        dma("sp", gate_d[:, gc, half * 512:(half + 1) * 512], o[:], [ok], ["gate_d"])

    for g in range(16):
        gemm_fm(actA, "actA", 32, w_gate_v, g * 512, wrF, c_gate)
    kb.barrier(); kb.free_to(m6)

    m6 = kb.mark()
    yaS = kb.sbuf("yaS", [128, 16, 1024], BF16); yrS = kb.sbuf("yrS", [128, 16, 1024], BF16)
    dma("sp", yaS[:], yaT_d, ["yaT_d"], ["yaS"]); dma("sp", yrS[:], yrT_d, ["yrT_d"], ["yrS"])
    wa = Ring(kb, "wa", 2, [128, 16, 512], BF16); wrr = Ring(kb, "wrr", 2, [128, 16, 512], BF16)
    gar = Ring(kb, "gar", 2, [128, 512], BF16); grr = Ring(kb, "grr", 2, [128, 512], BF16)
    f512 = Ring(kb, "f512", 2, [128, 512], F32); f512b = Ring(kb, "f512b", 2, [128, 512], F32)
    o512 = Ring(kb, "o512", 3, [128, 512], BF16)
    w_upa_v = w_up[0:2048, :].rearrange("(k p) e -> p k e", p=128)
    w_upr_v = w_up[2048:4096, :].rearrange("(k p) e -> p k e", p=128)
    for g in range(8):
        wat, wak = wa.get(); wrt, wrk = wrr.get()
        dma("pool", wat[:], w_upa_v[:, :, g * 512:(g + 1) * 512], [], [wak])
        dma("pool", wrt[:], w_upr_v[:, :, g * 512:(g + 1) * 512], [], [wrk])
        for cc in range(4):
            dc = g * 4 + cc
            for half in range(2):
                psa, pka = prF.get(); psr, pkr = prF.get()
                for k in range(16):
                    mm(psa[:, :], wat[:, k, cc * 128:(cc + 1) * 128], yaS[:, k, half * 512:(half + 1) * 512], k == 0, k == 15, [wak, "yaS"], [pka])
                for k in range(16):
                    mm(psr[:, :], wrt[:, k, cc * 128:(cc + 1) * 128], yrS[:, k, half * 512:(half + 1) * 512], k == 0, k == 15, [wrk, "yrS"], [pkr])
                ga, gak = gar.get(); gr, grk = grr.get(); t1, t1k = f512.get(); t2, t2k = f512b.get(); o, ok = o512.get()
                dma("sp", ga[:], gate_d[:, dc, half * 512:(half + 1) * 512], ["gate_d"], [gak])
                dma("sp", gr[:], gate_d[:, 32 + dc, half * 512:(half + 1) * 512], ["gate_d"], [grk])
                tt("dve", t1[:], psa[:, :], ga[:], ALU.mult, [pka, gak], [t1k])
                tt("dve", t2[:], psr[:, :], gr[:], ALU.mult, [pkr, grk], [t2k])
                tt("pool", o[:], t1[:], t2[:], ALU.add, [t1k, t2k], [ok])
                dma("sp", mrg_d[:, dc, half * 512:(half + 1) * 512], o[:], [ok], ["mrg_d"])
    kb.barrier(); kb.free_to(m6)

    m6 = kb.mark()
    actA = kb.sbuf("actA", [128, 32, 1024], BF16)
    wrF = Ring(kb, "wrF", 2, [128, 32, 512], BF16)
    dma("sp", actA[:], mrg_d, ["mrg_d"], ["actA"])
    xr5 = Ring(kb, "xr5", 3, [128, 512], F32); x1r = Ring(kb, "x1r", 3, [128, 512], F32)
    w_out_v = w_out.rearrange("(k p) e -> p k e", p=128)

    def c_out(dc, half, ps, pk):
        xt, xk_ = xr5.get(); x1, x1k = x1r.get()
        dma("sp", xt[:], xq_v[:, dc, half * 512:(half + 1) * 512], [], [xk_])
        stt("dve", x1[:], ps[:, :], modT[:, 64 + dc:65 + dc], xt[:], ALU.mult, ALU.add, [pk, xk_, "modT"], [x1k])
        dma("sp", x1T_d[:, dc, half * 512:(half + 1) * 512], x1[:], [x1k], ["x1T_d"])

    for g in range(8):
        gemm_fm(actA, "actA", 32, w_out_v, g * 512, wrF, c_out)
    kb.barrier(); kb.free_to(m6)
    phase_norm(x1T_d, 1024, A2[:], modT[:, 96:128], h2T_d, "h2T_d")
    if stages <= 6:
        return finish(nc, kb, outT)

    pst_d = dscr("pst_d", [8, 128, 2, 8, 128], F32); dl_d = dscr("dl_d", [8, 128, 8], F32)
    m7 = kb.mark()
    qTs = kb.sbuf("qTs", [128, 16, 1024], BF16)
    kT2 = kb.sbuf("kT2", [128, 2, 128], BF16)
    dma("pool", kT2[:], keysT.rearrange("s d k -> d s k"), [], ["kT2"])
    m7g = kb.mark()
    actA = kb.sbuf("actA", [128, 32, 1024], BF16)
    wrF = Ring(kb, "wrF", 2, [128, 32, 512], BF16)
    dma("sp", actA[:], h2T_d, ["h2T_d"], ["actA"])
    w_pq_v = w_pq.rearrange("(k p) e -> p k e", p=128)

    def c_pq(qc, half, ps, pk):
        cp("act" if half == 0 else "dve", qTs[:, qc, half * 512:(half + 1) * 512], ps[:, :], [pk], ["qTs"])

    for g in range(4):
        gemm_fm(actA, "actA", 32, w_pq_v, g * 512, wrF, c_pq)
    kb.barrier(); kb.free_to(m7g)
    s_sb = Ring(kb, "s_sb", 2, [128, 16, 128], F32)
    tmpk = Ring(kb, "tmpk", 2, [128, 128], F32)
    v16 = Ring(kb, "v16", 2, [128, 16, 16], F32)
    cand = Ring(kb, "cand", 2, [128, 8, 256], F32); cand2 = Ring(kb, "cand2", 2, [128, 256], F32)
    top16 = Ring(kb, "top16", 2, [128, 8, 16], F32)
    e16 = Ring(kb, "e16", 2, [128, 8, 16], F32)
    sm8 = Ring(kb, "sm8", 2, [128, 4, 8], F32)
    pstS = Ring(kb, "pstS", 2, [128, 2, 8, 128], F32)
    for tti in range(8):
        ss, ssk = s_sb.get()
        for qc in range(16):
            mm(pbig[:, qc * 128:(qc + 1) * 128], qTs[:, qc, tti * 128:(tti + 1) * 128], kT2[:, qc % 2, :], True, True, ["qTs", "kT2"], [bigk[qc // 4]])
        cp("act", ss[:], pbig[:, :].rearrange("p (q k) -> p q k", q=16), bigk, [ssk])
        v, vk_ = v16.get()
        for qc in range(16):
            tm, tmk = tmpk.get()
            vmax(v[:, qc, 0:8], ss[:, qc, :], [ssk], [vk_])
            mrep(tm[:], v[:, qc, 0:8], ss[:, qc, :], [ssk, vk_], [tmk])
            vmax(v[:, qc, 8:16], tm[:], [tmk], [vk_])
        cd, cdk = cand.get(); tp, tpk = top16.get()
        vv = v[:].rearrange("p (h s) a -> p h s a", s=2)
        tt("dve", cd[:].rearrange("p h (a b) -> p h a b", a=16), vv[:, :, 0, :].unsqueeze(3).to_broadcast([128, 8, 16, 16]),
           vv[:, :, 1, :].unsqueeze(2).to_broadcast([128, 8, 16, 16]), ALU.add, [vk_], [cdk])
        for h in range(8):
            c2, c2k = cand2.get()
            vmax(tp[:, h, 0:8], cd[:, h, :], [cdk], [tpk])
            mrep(c2[:], tp[:, h, 0:8], cd[:, h, :], [cdk, tpk], [c2k])
            vmax(tp[:, h, 8:16], c2[:], [c2k], [tpk])
        ee, eek = e16.get(); s8, s8k = sm8.get(); po, pok = pstS.get()
        tt("dve", ee[:], tp[:], tp[:, :, 0:1].to_broadcast([128, 8, 16]), ALU.subtract, [tpk], [eek])
        act(ee[:], ee[:], AF.Exp, [eek], [eek])
        rsum(s8[:, 0, :], ee[:], [eek], [s8k])
        act(s8[:, 1, :], s8[:, 0, :], AF.Ln, [s8k], [s8k])
        tt("dve", s8[:, 2, :], s8[:, 1, :], tp[:, :, 0], ALU.add, [s8k, tpk], [s8k])
        tt("dve", s8[:, 3, :], tp[:, :, 15], s8[:, 2, :], ALU.subtract, [s8k, tpk], [s8k])
        ssv = ss[:].rearrange("p (h s) k -> p h s k", s=2)
        tt("dve", po[:, 0, :, :], ssv[:, :, 0, :], s8[:, 2, :].unsqueeze(2).to_broadcast([128, 8, 128]), ALU.subtract, [ssk, s8k], [pok])
        ts("dve", s8[:, 3, :], s8[:, 3, :], -1.0e-5, ALU.add, [s8k], [s8k])
        cp("pool", po[:, 1, :, :], ssv[:, :, 1, :], [ssk], [pok])
        dma("sp", pst_d[tti], po[:], [pok], ["pst_d"])
        dma("sp", dl_d[tti], s8[:, 3, :], [s8k], ["dl_d"])
    kb.barrier(); kb.free_to(m7)

    m7 = kb.mark()
    actA = kb.sbuf("actA", [128, 32, 1024], BF16)
    dma("sp", actA[:], h2T_d, ["h2T_d"], ["actA"])
    pstA = kb.sbuf("pstA", [128, 8, 2, 8, 128], F32); dlA = kb.sbuf("dlA", [128, 8, 8], F32)
    for tti in range(8):
        dma("sp", pstA[:, tti], pst_d[tti], ["pst_d"], ["pstA"])
        dma("sp", dlA[:, tti, :], dl_d[tti], ["dl_d"], ["dlA"])
    uTr = Ring(kb, "uTr", 2, [128, 32, 256], BF16)
    Sr = Ring(kb, "Sr", 2, [128, 8, 2, 128], F32); Er2 = Ring(kb, "Er2", 2, [128, 8, 2, 128], BF16); G8r = Ring(kb, "G8r", 3, [128, 8, 2, 128], BF16)
    geTr = Ring(kb, "geTr", 2, [128, 2, 512], BF16); cTr2 = Ring(kb, "cTr2", 2, [128, 2, 512], BF16)
    uT_v = uT.rearrange("(k p) e -> p k e", p=128)
    prAct = PRing([(big[0], big[1]), (big[2], big[3])], [(bigk[0], bigk[1]), (bigk[2], bigk[3])])
    prG = PRing([(pb4[:], pb5[:]), (pb6[:], pb7[:])], [("pb4", "pb5"), ("pb6", "pb7")])
    for eg in range(64):
        ut, utk = uTr.get()
        dma("pool", ut[:], uT_v[:, :, eg * 256:(eg + 1) * 256], [], [utk])
        for tq in range(2):
            aps, aks = prAct.get(); gps, gks = prG.get()
            geT, geTk = geTr.get(); cT_, cTk = cTr2.get()
            for i in range(2):
                for k in range(32):
                    mm(aps[i], ut[:, k, i * 128:(i + 1) * 128], actA[:, k, tq * 512:(tq + 1) * 512], k == 0, k == 31, ["actA", utk], [aks[i]])
            for i in range(2):
                act(geT[:, i, :], aps[i], AF.Gelu, [aks[i]], [geTk + str(i)])
            for t4 in range(4):
                tti = tq * 4 + t4
                S_, Sk = Sr.get(); E_, Ek = Er2.get(); G8, G8k = G8r.get()
                tt("pool", S_[:], pstA[:, tti, 1, :, :].unsqueeze(2).to_broadcast([128, 8, 2, 128]),
                   pstA[:, tti, 0, :, eg * 2:(eg + 1) * 2].unsqueeze(3).to_broadcast([128, 8, 2, 128]), ALU.add, ["pstA"], [Sk])
                act(E_[:], S_[:], AF.Exp, [Sk], [Ek])
                for h in range(8):
                    stt("dve", G8[:, h, :, :], S_[:, h, :, :], dlA[:, tti, h:h + 1], E_[:, h, :, :], ALU.is_ge, ALU.mult, [Sk, Ek, "dlA"], [f"{G8k}h{h}"])
                for i in range(2):
                    for h in range(8):
                        mm(gps[i][:, t4 * 128:(t4 + 1) * 128], G8[:, h, i, :], ident[:], h == 0, h == 7, [f"{G8k}h{h}", "ident"], [gks[i]])
            for i in range(2):
                tt("dve", cT_[:, i, :], gps[i], geT[:, i, :], ALU.mult, [gks[i], geTk + str(i)], [cTk])
            dma("sp", coefT_d[eg * 256:(eg + 1) * 256, tq * 512:(tq + 1) * 512].rearrange("(i e) t -> e i t", e=128), cT_[:], [cTk], ["coefT_d"])
    kb.barrier(); kb.free_to(m7)

    m7 = kb.mark()
    vtr = Ring(kb, "vtr", 10, [128, 512], BF16); cTr = Ring(kb, "cTr", 10, [128, 1024], BF16)
    xr5 = Ring(kb, "xr5", 3, [128, 512], F32); x1r = Ring(kb, "x1r", 3, [128, 512], F32)
    banks = big + [pb4[:], pb5[:], pb6[:], pb7[:]]
    bkeys = bigk + ["pb4", "pb5", "pb6", "pb7"]
    for ds in range(8):
        for et in range(128):
            vt, vk_ = vtr.get(); ct, ctk = cTr.get()
            dma("pool", vt[:], v_exp[et * 128:(et + 1) * 128, ds * 512:(ds + 1) * 512], [], [vk_])
            dma("sp", ct[:], coefT_d[et * 128:(et + 1) * 128, :], ["coefT_d"], [ctk])
            for cc in range(4):
                for half in range(2):
                    bi = cc * 2 + half
                    wkeys = [bkeys[bi]]
                    mm(banks[bi], vt[:, cc * 128:(cc + 1) * 128], ct[:, half * 512:(half + 1) * 512], et == 0, et == 127, [vk_, ctk], wkeys)
        for cc in range(4):
            dc = ds * 4 + cc
            for half in range(2):
                bi = cc * 2 + half
                xt, xk_ = xr5.get(); x2, x2k = x1r.get()
                dma("sp", xt[:], x1T_d[:, dc, half * 512:(half + 1) * 512], ["x1T_d"], [xk_])
                stt("dve", x2[:], banks[bi], modT[:, 160 + dc:161 + dc], xt[:], ALU.mult, ALU.add, [bkeys[bi], xk_, "modT"], [x2k])
                dma("sp", x2T_d[:, dc, half * 512:(half + 1) * 512], x2[:], [x2k], ["x2T_d"])
    kb.barrier(); kb.free_to(m7)

    gfin = kb.sbuf("gfin", [128, 32], F32)
    dma("sp", gfin[:], g_final, [], ["gfin"])
    phase_norm(x2T_d, 1024, gfin[:], None, outT.rearrange("(k p) t -> p k t", p=128), "outT")
    return finish(nc, kb, outT)


def finish(nc, kb, outT):
    kb.barrier()
    kb.emit()
    kb.close()
    return nc


def _t5_bucket_np(rel):
    n = np.abs(rel)
    lr = np.log(np.maximum(n, 1).astype(np.float32) / np.float32(8)) / np.float32(math.log(128 / 8))
    large = np.minimum(8 + (lr * np.float32(8)).astype(np.int32), 15)
    return np.where(rel > 0, 16, 0) + np.where(n < 8, n, large)


_CONST_CACHE = {}


def _consts(p):
    if p in _CONST_CACHE:
        return _CONST_CACHE[p]
    f32 = np.float32
    own = np.concatenate([np.arange((2 * j + p) * 128, (2 * j + p + 1) * 128) for j in range(8)])
    inv = (1.0 / (f32(10000.0) ** np.linspace(0.0, 1.0, 64, dtype=f32))).astype(f32)
    pos = np.arange(2048, dtype=f32)
    ang = pos[:, None] * inv[None, :]
    cos, sin = np.cos(ang).astype(f32), np.sin(ang).astype(f32)
    ks = f32(128 ** -0.5)
    cos2 = np.concatenate([cos, cos], 1); sin2 = np.concatenate([-sin, sin], 1)
    logg = np.array(LOGG, dtype=np.float64)
    s_in = np.arange(2048) % 256
    kdec = np.exp((255 - s_in)[:, None] * logg[None, :]).astype(f32)
    qdec = np.exp(((own % 256) + 1)[:, None] * logg[None, :]).astype(f32)
    sg = (np.arange(2)[:, None] * 128 + np.arange(128)[None, :])
    tg = 128 * p + np.arange(128)
    diff = tg[None, None, :] - sg[:, :, None]
    m = np.where(diff[:, :, None, :] >= 0, np.exp(np.maximum(diff, 0)[:, :, None, :] * logg[None, None, :, None]), 0.0)
    mret = np.ascontiguousarray(np.transpose(m, (1, 0, 2, 3))).astype(f32)
    tq = np.arange(128); s2 = np.arange(256)
    adm01 = ((s2[None, :] // 64) <= (2 * p + tq[:, None] // 64)).astype(f32)
    negb = ((adm01 - 1.0) * 1.0e30).astype(f32)
    mm_ = np.arange(512)
    rel = mm_ - 127 - (1 + p) * 128
    bk = _t5_bucket_np(rel)
    oh = (bk[None, :] == np.arange(32)[:, None]).astype(f32)
    oh[:, 511] = 0.0
    c = dict(own=own, cosk=(cos2 * ks).astype(f32), sink=(sin2 * ks).astype(f32), kdec_tab=kdec,
             cosq=np.ascontiguousarray(cos2[own]), sinq=np.ascontiguousarray(sin2[own]), qdec_tab=qdec,
             mret=mret, adm01=adm01, negb=negb, oh_rel=oh,
             ident=np.eye(128, dtype=f32), anti=np.ascontiguousarray(np.eye(128, dtype=f32)[::-1]))
    _CONST_CACHE[p] = c
    return c


def _shared(inp):
    f = lambda a: np.ascontiguousarray(a, dtype=np.float32)
    ch = lambda v: f(np.asarray(v).reshape(-1, 128).T)
    sh = dict(
        w_ada=f(inp["w_ada"][0]), b_ada=f(inp["b_ada"][0].reshape(1, -1)),
        g_mix=ch(inp["g_mix"][0]), g_ffn=ch(inp["g_ffn"][0]), g_final=ch(inp["g_final"]),
        w_in=f(inp["w_in"][0]), g_cq=f(inp["g_cq"][0]), g_ckv=f(inp["g_ckv"][0]), g_ki=f(inp["g_ki"][0]), b_ki=f(inp["b_ki"][0]),
        g_ret=f(inp["g_ret"][0].reshape(-1)),
        w_uq=f(inp["w_uq"][0].reshape(1024, 2048)), w_ukT=f(np.transpose(inp["w_uk"][0], (2, 1, 0))),
        w_uv=f(inp["w_uv"][0].reshape(512, 2048)), w_qi=f(inp["w_qi"][0].reshape(1024, 8192)),
        t5b=f(inp["t5_bias"]), t15=f(inp["t5_bias"][15].reshape(16, 1)),
        w_up=f(inp["w_up"][0]), w_gate=f(inp["w_gate"][0]), b_gate=ch(inp["b_gate"][0]), w_out=f(inp["w_out"][0]),
        w_pq=f(inp["w_pq"][0]), keysT=f(np.transpose(inp["sub_keys"][0], (0, 2, 1))),
        uT=f(inp["u_exp"][0].T), v_exp=f(inp["v_exp"][0]),
    )
    return sh


def make_in_maps(inp, cores=None, names=None):
    sh = _shared(inp)
    x = np.asarray(inp["x"], dtype=np.float32); c = np.asarray(inp["c"], dtype=np.float32)
    maps = []
    for core in (range(8) if cores is None else cores):
        b, p = core // 2, core % 2
        cs = _consts(p)
        m = dict(sh)
        m["_core"] = core
        m["xkT"] = np.ascontiguousarray(x[b].T)
        m["xqT"] = np.ascontiguousarray(x[b][cs["own"]].T)
        m["cT"] = np.ascontiguousarray(c[b].reshape(32, 128).T)
        for k in ("cosk", "sink", "kdec_tab", "cosq", "sinq", "qdec_tab", "mret", "adm01", "negb", "oh_rel", "ident", "anti"):
            m[k] = cs[k]
        m.pop("_core")
        if names is not None:
            m = {k: v for k, v in m.items() if k in names}
        maps.append(m)
    return maps


def kernel(**inputs):
    inp = {k: np.asarray(v) for k, v in inputs.items()}
    nc = build()
    maps = make_in_maps(inp, names=set(_USED["names"]))
    res = run_bass_kernel_spmd(nc, maps, core_ids=list(range(8)))
    out = np.zeros((4, 2048, 4096), dtype=np.float32)
    for core in range(8):
        b, p = core // 2, core % 2
        own = _consts(p)["own"]
        out[b, own, :] = np.asarray(res.results[core]["outT"]).T
    return out
```

```python
import math
import numpy as np
import concourse.bass as bass
import concourse.mybir as mybir
from concourse.bass_utils import run_bass_kernel_spmd

F32 = mybir.dt.float32
BF16 = mybir.dt.bfloat16
ALU = mybir.AluOpType
AF = mybir.ActivationFunctionType
AX = mybir.AxisListType

EPOCH = 12000
N_DMA_SEMS = 40
EPS = 1e-6
NEG = -1.0e30


class KB:
    ENGS = ("pe", "act", "dve", "pool", "sp")

    def __init__(self, nc):
        self.nc = nc
        self.stack = []
        self.streams = {e: [] for e in self.ENGS}
        self.cnt = {e: 0 for e in self.ENGS}
        self.cur_sem = {}
        self.known = {e: {} for e in self.ENGS}
        self.res = {}
        self.sem_objs = {}
        self.n_sems = 0
        for e in self.ENGS:
            self.cur_sem[e] = self._new_sem(f"s_{e}")
        self.dma_sems = [self._new_sem(f"s_dma{i}") for i in range(N_DMA_SEMS)]
        self.dma_cnt = [0] * N_DMA_SEMS
        self.dma_rr = 0
        self.n_inst = 0

    def _new_sem(self, name):
        cm = self.nc.semaphore(f"{name}_{self.n_sems}")
        s = cm.__enter__()
        self.stack.append(cm)
        sid = self.n_sems
        self.n_sems += 1
        self.sem_objs[sid] = s
        return sid

    def sbuf(self, name, shape, dtype):
        self.n_alloc = getattr(self, "n_alloc", 0) + 1
        cm = self.nc.sbuf_tensor(f"sb{self.n_alloc}_{name}", list(shape), dtype)
        t = cm.__enter__()
        self.stack.append(cm)
        return t

    def psum(self, name, shape, dtype):
        cm = self.nc.psum_tensor(name, list(shape), dtype)
        t = cm.__enter__()
        self.stack.append(cm)
        return t

    def mark(self):
        return len(self.stack)

    def free_to(self, mark):
        while len(self.stack) > mark:
            cm = self.stack.pop()
            cm.__exit__(None, None, None)

    def _need(self, eng, reads, writes, pe_accum=False):
        need = {}

        def add(ev):
            if ev is None:
                return
            sid, val = ev
            if need.get(sid, 0) < val:
                need[sid] = val

        for r in reads:
            st = self.res.get(r)
            if st is not None:
                add(st[0])
        for w in writes:
            st = self.res.get(w)
            if st is not None:
                if not (pe_accum and st[0] is not None and st[0][0] == self.cur_sem[eng]):
                    add(st[0])
                for sid, val in st[1].items():
                    add((sid, val))
        waits = []
        kn = self.known[eng]
        own = self.cur_sem[eng]
        for sid, val in need.items():
            if kn.get(sid, 0) >= val:
                continue
            if sid == own and eng in ("pe", "sp"):
                continue
            kn[sid] = val
            waits.append((sid, val))
        return waits

    def _commit(self, ev, reads, writes):
        for w in writes:
            self.res[w] = [ev, {}]
        for r in reads:
            st = self.res.setdefault(r, [None, {}])
            sid, val = ev
            if st[1].get(sid, 0) < val:
                st[1][sid] = val

    def op(self, eng, fn, reads=(), writes=(), pe_accum=False):
        if self.cnt[eng] >= EPOCH:
            self.cur_sem[eng] = self._new_sem(f"s_{eng}")
            self.cnt[eng] = 0
        excl = [r for r in reads if r.startswith("pb")]
        if excl:
            reads = [r for r in reads if not r.startswith("pb")]
            writes = list(writes) + excl
        waits = self._need(eng, reads, writes, pe_accum)
        self.cnt[eng] += 1
        ev = (self.cur_sem[eng], self.cnt[eng])
        self.streams[eng].append((waits, fn, (ev[0], 1)))
        self._commit(ev, reads, writes)
        self.n_inst += 1
        return ev

    def dma(self, q, fn, reads=(), writes=()):
        k = self.dma_rr
        self.dma_rr = (self.dma_rr + 1) % N_DMA_SEMS
        sid = self.dma_sems[k]
        waits = self._need(q, reads, writes)
        prev = self.dma_cnt[k]
        if prev > 0 and self.known[q].get(sid, 0) < prev:
            self.known[q][sid] = prev
            waits.append((sid, prev))
        self.dma_cnt[k] += 16
        ev = (sid, self.dma_cnt[k])
        self.streams[q].append((waits, fn, (sid, 16)))
        self._commit(ev, reads, writes)
        self.n_inst += 1
        return ev

    def barrier(self):
        evs = []
        for e in self.ENGS:
            if self.cnt[e] > 0:
                evs.append((self.cur_sem[e], self.cnt[e]))
        for k in range(N_DMA_SEMS):
            if self.dma_cnt[k] > 0:
                evs.append((self.dma_sems[k], self.dma_cnt[k]))
        for e in self.ENGS:
            waits = []
            for sid, val in evs:
                if sid == self.cur_sem[e]:
                    continue
                if self.known[e].get(sid, 0) < val:
                    self.known[e][sid] = val
                    waits.append((sid, val))
            if waits:
                self.streams[e].append((waits, None, None))
        self.res = {}

    def emit(self):
        nc = self.nc
        so = self.sem_objs
        streams = self.streams
        with nc.Block() as block:
            def run(engobj, items):
                for waits, fn, inc in items:
                    for sid, val in waits:
                        engobj.wait_ge(so[sid], val)
                    if fn is not None:
                        ins = fn(engobj)
                        ins.then_inc(so[inc[0]], inc[1])

            @block.tensor
            def _(e):
                run(e, streams["pe"])

            @block.scalar
            def _(e):
                run(e, streams["act"])

            @block.vector
            def _(e):
                run(e, streams["dve"])

            @block.gpsimd
            def _(e):
                run(e, streams["pool"])

            @block.sync
            def _(e):
                run(e, streams["sp"])

    def close(self):
        self.free_to(0)


class Ring:
    def __init__(self, kb, name, n, shape, dtype):
        self.tiles = [kb.sbuf(f"{name}{i}", shape, dtype) for i in range(n)]
        self.keys = [f"{name}{i}" for i in range(n)]
        self.i = 0

    def get(self):
        t, k = self.tiles[self.i], self.keys[self.i]
        self.i = (self.i + 1) % len(self.tiles)
        return t, k


class PRing:
    def __init__(self, aps, keys):
        self.aps, self.keys, self.i = aps, keys, 0

    def get(self):
        t, k = self.aps[self.i], self.keys[self.i]
        self.i = (self.i + 1) % len(self.aps)
        return t, k


O_CQ, O_CKV, O_KI, O_WI, O_RQ, O_RK, O_RV, O_RG = 0, 1024, 1536, 1664, 1728, 2752, 3776, 5824
LOGG = [math.log(1.0 - 2.0 ** (-5.0 - h)) for h in range(8)]


_USED = {}


def build(stages=99, dbg=()):
    nc = bass.Bass("TRN2", target_bir_lowering=False)
    kb = KB(nc)

    used = []
    _USED["names"] = used

    def din(name, shape, dt=F32):
        used.append(name)
        return nc.dram_tensor(name, list(shape), dt, kind="ExternalInput").ap()

    def dscr(name, shape, dt):
        kind = "ExternalOutput" if name in dbg else "Internal"
        return nc.dram_tensor(name, list(shape), dt, kind=kind).ap()

    xkT = din("xkT", [4096, 2048]); xqT = din("xqT", [4096, 1024]); cT = din("cT", [128, 32])
    w_ada = din("w_ada", [4096, 24576]); b_ada = din("b_ada", [1, 24576])
    g_mix = din("g_mix", [128, 32]); g_ffn = din("g_ffn", [128, 32]); g_final = din("g_final", [128, 32])
    w_in = din("w_in", [4096, 7872])
    g_cq = din("g_cq", [1024]); g_ckv = din("g_ckv", [512]); g_ki = din("g_ki", [128]); b_ki = din("b_ki", [128])
    g_ret = din("g_ret", [2048])
    if stages >= 4:
        w_uq = din("w_uq", [1024, 2048]); w_ukT = din("w_ukT", [128, 16, 512]); w_uv = din("w_uv", [512, 2048])
        w_qi = din("w_qi", [1024, 8192])
        t5b = din("t5b", [32, 16]); t15 = din("t15", [16, 1]); oh_rel = din("oh_rel", [32, 512])
    if stages >= 6:
        w_up = din("w_up", [4096, 4096]); w_gate = din("w_gate", [4096, 8192]); b_gate = din("b_gate", [128, 64])
        w_out = din("w_out", [4096, 4096])
    if stages >= 7:
        w_pq = din("w_pq", [4096, 2048]); keysT = din("keysT", [2, 128, 128])
        uT = din("uT", [4096, 16384]); v_exp = din("v_exp", [16384, 4096])
    ident_in = din("ident", [128, 128]); anti_in = din("anti", [128, 128])
    cosk = din("cosk", [2048, 128]); sink = din("sink", [2048, 128]); kdec_tab = din("kdec_tab", [2048, 8])
    cosq = din("cosq", [1024, 128]); sinq = din("sinq", [1024, 128]); qdec_tab = din("qdec_tab", [1024, 8])
    mret_in = din("mret", [128, 2, 8, 128]); adm01_in = din("adm01", [128, 256]); negb_in = din("negb", [128, 256])
    outT = nc.dram_tensor("outT", [4096, 1024], F32, kind="ExternalOutput").ap()

    mod_d = dscr("mod_d", [1, 24576], F32)
    hk_d = dscr("hk_d", [128, 32, 2048], BF16); hq_d = dscr("hq_d", [128, 32, 1024], BF16)
    ckv_tm_d = dscr("ckv_tm_d", [2048, 512], BF16); ckvT_d = dscr("ckvT_d", [128, 4, 2048], BF16)
    kidxT_d = dscr("kidxT_d", [128, 2048], BF16)
    kT_d = dscr("kT_d", [128, 8, 2048], BF16); kdec_d = dscr("kdec_d", [2048, 1024], BF16); v_d = dscr("v_d", [2048, 2048], BF16)
    cqT_d = dscr("cqT_d", [128, 8, 1024], BF16); wi_d = dscr("wi_d", [1024, 64], F32)
    qT_d = dscr("qT_d", [128, 8, 1024], BF16); qdT_d = dscr("qdT_d", [128, 8, 1024], BF16); rg_d = dscr("rg_d", [1024, 2048], BF16)
    yaT_d = dscr("yaT_d", [128, 16, 1024], BF16); yrT_d = dscr("yrT_d", [128, 16, 1024], BF16)
    ef_d = dscr("ef_d", [16, 512], F32)
    gate_d = dscr("gate_d", [128, 64, 1024], BF16); mrg_d = dscr("mrg_d", [128, 32, 1024], BF16)
    x1T_d = dscr("x1T_d", [128, 32, 1024], F32); h2T_d = dscr("h2T_d", [128, 32, 1024], BF16)
    ge_d = dscr("ge_d", [1024, 16384], BF16); coefT_d = dscr("coefT_d", [16384, 1024], BF16)
    x2T_d = dscr("x2T_d", [128, 32, 1024], F32)

    def dma1(q, out, in_, reads, writes):
        kb.dma(q, lambda e: e.dma_start(out=out, in_=in_), reads, writes)

    def dma(q, out, in_, reads, writes):
        sh = out.shape
        if len(sh) == 3 and sh[0] * sh[1] > 1024 and len(in_.shape) == 3 and in_.shape[1] == sh[1]:
            step = max(1, 1024 // sh[0])
            for a in range(0, sh[1], step):
                dma1(q, out[:, a:a + step, :], in_[:, a:a + step, :], reads, writes)
        else:
            dma1(q, out, in_, reads, writes)

    def mm(out, lhsT, rhs, start, stop, reads, writes):
        kb.op("pe", lambda e: e.matmul(out, lhsT, rhs, start=start, stop=stop), reads, writes, pe_accum=not start)

    def tr(out, in_, reads, writes):
        kb.op("pe", lambda e: e.transpose(out, in_, ident[:]), list(reads) + ["ident"], writes)

    def act(out, in_, func, reads, writes, bias=None, scale=None):
        kw = {}
        if bias is not None:
            kw["bias"] = bias
        if scale is not None:
            kw["scale"] = scale
        kb.op("act", lambda e: e.activation(out=out, in_=in_, func=func, **kw), reads, writes)

    def tt(eng, out, in0, in1, op, reads, writes):
        kb.op(eng, lambda e: e.tensor_tensor(out=out, in0=in0, in1=in1, op=op), reads, writes)

    def ts(eng, out, in0, s1, op0, reads, writes, s2=None, op1=None):
        if op1 is None:
            kb.op(eng, lambda e: e.tensor_scalar(out=out, in0=in0, scalar1=s1, scalar2=None, op0=op0), reads, writes)
        else:
            kb.op(eng, lambda e: e.tensor_scalar(out=out, in0=in0, scalar1=s1, scalar2=s2, op0=op0, op1=op1), reads, writes)

    def stt(eng, out, in0, scalar, in1, op0, op1, reads, writes):
        kb.op(eng, lambda e: e.scalar_tensor_tensor(out=out, in0=in0, scalar=scalar, in1=in1, op0=op0, op1=op1), reads, writes)

    def cp(eng, out, in_, reads, writes):
        if eng == "act":
            kb.op("act", lambda e: e.copy(out, in_), reads, writes)
        else:
            kb.op(eng, lambda e: e.tensor_copy(out, in_), reads, writes)

    def recip(out, in_, reads, writes):
        kb.op("dve", lambda e: e.reciprocal(out=out, in_=in_), reads, writes)

    def rsum(out, in_, reads, writes):
        kb.op("dve", lambda e: e.reduce_sum(out=out, in_=in_, axis=AX.X), reads, writes)

    def vmax(out, in_, reads, writes):
        kb.op("dve", lambda e: e.max(out=out, in_=in_), reads, writes)

    def mrep(out, rep, vals, reads, writes):
        kb.op("dve", lambda e: e.match_replace(out=out, in_to_replace=rep, in_values=vals, imm_value=NEG), reads, writes)

    def amul(out, in_, c, reads, writes):
        kb.op("act", lambda e: e.mul(out, in_, c), reads, writes)

    def memset(eng, ap, val, writes):
        kb.op(eng, lambda e: e.memset(ap, val), (), writes)

    ident = kb.sbuf("ident", [128, 128], BF16)
    anti = kb.sbuf("anti", [128, 128], BF16)
    ones = kb.sbuf("ones", [128, 128], BF16)
    eps_t = kb.sbuf("eps_t", [128, 1], F32)
    modT = kb.sbuf("modT", [128, 192], F32)
    A1 = kb.sbuf("A1", [128, 32], F32); A2 = kb.sbuf("A2", [128, 32], F32)
    pbig = kb.psum("pbig", [128, 2048], F32)
    pb4 = kb.psum("pb4", [128, 512], F32); pb5 = kb.psum("pb5", [128, 512], F32); pb6 = kb.psum("pb6", [128, 512], F32)
    pb7 = kb.psum("pb7", [128, 512], F32)
    pst = pb7[:].bitcast(BF16)
    tslots = PRing([pst[:, 0:1024]], ["pb7"])
    big = [pbig[:, i * 512:(i + 1) * 512] for i in range(4)]
    bigk = [f"pbig{i}" for i in range(4)]

    dma("pool", ident[:], ident_in, [], ["ident"])
    dma("pool", anti[:], anti_in, [], ["anti"])
    memset("dve", ones[:], 1.0, ["ones"])
    memset("dve", eps_t[:], EPS, ["eps"])

    def transposes(dst, src, n, rk, wk, eng="act"):
        slot, sk = tslots.get()
        for i in range(n):
            tr(slot[:, i * 128:(i + 1) * 128], src[:, i * 128:(i + 1) * 128], [rk], [sk])
        cp(eng, dst, slot[:, 0:n * 128].rearrange("p (n c) -> p n c", n=n), [sk], [wk])

    m0 = kb.mark()
    c32 = kb.sbuf("c32", [128, 32], F32); cact = kb.sbuf("cact", [128, 32], BF16)
    dma("sp", c32[:], cT, [], ["c32"])
    act(cact[:], c32[:], AF.Silu, ["c32"], ["cact"])
    w_ada_v = w_ada.rearrange("(k p) e -> p k e", p=128)
    wring = Ring(kb, "wada", 2, [128, 32, 512], BF16)
    brow = Ring(kb, "brow", 2, [1, 512], F32); mrow = Ring(kb, "mrow", 2, [1, 512], F32)
    p0 = PRing([pb4[:], pb5[:]], ["pb4", "pb5"])
    p0b = PRing([pb6[:]], ["pb6"])
    one32 = kb.sbuf("one32", [1, 1], F32)
    memset("dve", one32[:], 1.0, ["one32"])
    wring32 = Ring(kb, "wada32", 2, [128, 32, 512], F32)
    cact32 = kb.sbuf("cact32", [128, 32], F32)
    act(cact32[:], c32[:], AF.Silu, ["c32"], ["cact32"])
    for g in range(48):
        ps, pk = p0.get()
        if g % 2 == 0:
            wt, wk = wring.get()
            dma("pool", wt[:], w_ada_v[:, :, g * 512:(g + 1) * 512], [], [wk])
            for k in range(32):
                mm(ps[0:1, :], cact[:, k:k + 1], wt[:, k, :], k == 0, k == 31, [wk, "cact"], [pk])
        else:
            wt, wk = wring32.get()
            dma("sp", wt[:], w_ada_v[:, :, g * 512:(g + 1) * 512], [], [wk])
            for k in range(32):
                mm(ps[0:1, :], cact32[:, k:k + 1], wt[:, k, :], k == 0, k == 31, [wk, "cact32"], [pk])
        bt, bk = brow.get(); mt, mk = mrow.get()
        dma("sp", bt[:], b_ada[0:1, g * 512:(g + 1) * 512], [], [bk])
        tt("dve", mt[:], ps[0:1, :], bt[:], ALU.add, [pk, bk], [mk])
        ps2, pk2 = p0b.get()
        for i in range(4):
            mm(ps2[:, i:i + 1], mt[0:1, i * 128:(i + 1) * 128], one32[0:1, 0:1], True, True, [mk, "one32"], [pk2])
        cp("act", modT[:, g * 4:(g + 1) * 4], ps2[:, 0:4], [pk2], ["modT"])
    gm = kb.sbuf("gm", [128, 32], F32); gf = kb.sbuf("gf", [128, 32], F32)
    dma("sp", gm[:], g_mix, [], ["gm"]); dma("sp", gf[:], g_ffn, [], ["gf"])
    stt("dve", A1[:], modT[:, 32:64], 1.0, gm[:], ALU.add, ALU.mult, ["modT", "gm"], ["A1"])
    stt("dve", A2[:], modT[:, 128:160], 1.0, gf[:], ALU.add, ALU.mult, ["modT", "gf"], ["A2"])
    kb.barrier(); kb.free_to(m0)
    if stages <= 0:
        return finish(nc, kb, outT)

    def phase_norm(xT_v, ntok, A, Bv, dst_v, dkey, gvec=None):
        m = kb.mark()
        xr = Ring(kb, "xr", 2, [128, 32, 256], F32); sqr = Ring(kb, "sqr", 2, [128, 32, 256], BF16)
        hr = Ring(kb, "hr", 2, [128, 32, 256], BF16 if dst_v.dtype == BF16 else F32)
        rr = Ring(kb, "rr", 2, [128, 256], F32)
        pr = PRing([pb4[:], pb5[:]], ["pb4", "pb5"])
        for ti in range(ntok // 256):
            xt, kx = xr.get(); sq, ksq = sqr.get(); ht, kh = hr.get(); rt, krt = rr.get(); ps, pk = pr.get()
            dma("sp", xt[:], xT_v[:, :, ti * 256:(ti + 1) * 256], [], [kx])
            act(sq[:], xt[:], AF.Square, [kx], [ksq])
            for k in range(32):
                mm(ps[:, 0:256], ones[:], sq[:, k, :], k == 0, k == 31, [ksq, "ones"], [pk])
            act(rt[:], ps[:, 0:256], AF.Sqrt, [pk, "eps"], [krt], bias=eps_t[:, 0:1], scale=1.0 / 4096)
            recip(rt[:], rt[:], [krt], [krt])
            tt("dve", xt[:], xt[:], rt[:].unsqueeze(1).to_broadcast([128, 32, 256]), ALU.mult, [kx, krt], [kx])
            if Bv is not None:
                tt("pool", xt[:], xt[:], A.unsqueeze(2).to_broadcast([128, 32, 256]), ALU.mult, [kx, "A1", "A2", "modT"], [kx])
                tt("dve", ht[:], xt[:], Bv.unsqueeze(2).to_broadcast([128, 32, 256]), ALU.add, [kx, "modT"], [kh])
            else:
                tt("pool", ht[:], xt[:], A.unsqueeze(2).to_broadcast([128, 32, 256]), ALU.mult, [kx, "gfin"], [kh])
            dma("sp", dst_v[:, :, ti * 256:(ti + 1) * 256], ht[:], [kh], [dkey])
        kb.barrier(); kb.free_to(m)

    xk_v = xkT.rearrange("(k p) t -> p k t", p=128)
    xq_v = xqT.rearrange("(k p) t -> p k t", p=128)
    phase_norm(xk_v, 2048, A1[:], modT[:, 0:32], hk_d, "hk_d")
    phase_norm(xq_v, 1024, A1[:], modT[:, 0:32], hq_d, "hq_d")
    if stages <= 1:
        return finish(nc, kb, outT)

    w_in_v = w_in.rearrange("(k p) e -> p k e", p=128)

    def bc_load(name, src, n):
        t = kb.sbuf(name, [128, n], F32)
        dma("sp", t[:], src.partition_broadcast(128), [], [name])
        return t

    m2 = kb.mark()
    gckv_t = bc_load("gckv_t", g_ckv, 512); gki_t = bc_load("gki_t", g_ki, 128); bki_t = bc_load("bki_t", b_ki, 128)
    hT = kb.sbuf("hT", [128, 32, 1024], BF16)
    wr = Ring(kb, "wr", 2, [128, 32, 512], BF16)
    cos_t = kb.sbuf("cos_t", [128, 8, 128], F32); sin_t = kb.sbuf("sin_t", [128, 8, 128], F32); dec_t = kb.sbuf("dec_t", [128, 8, 8], F32)
    t512 = Ring(kb, "t512", 2, [128, 512], F32); u512 = Ring(kb, "u512", 2, [128, 512], F32)
    b512 = Ring(kb, "b512", 3, [128, 512], BF16); b512b = Ring(kb, "b512b", 2, [128, 512], BF16)
    trb = Ring(kb, "trb", 3, [128, 4, 128], BF16)
    sm = Ring(kb, "sm", 4, [128, 2], F32)
    pr2 = PRing(big + [pb4[:], pb5[:], pb6[:]], bigk + ["pb4", "pb5", "pb6"])

    def gemm_tm(hTt, col0, ncols, consumer):
        wt, wk = wr.get()
        dma("pool", wt[:, :, 0:ncols], w_in_v[:, :, col0:col0 + ncols], [], [wk])
        pend = None
        for tti in range(8):
            ps, pk = pr2.get()
            for k in range(32):
                mm(ps[:, 0:ncols], hTt[:, k, tti * 128:(tti + 1) * 128], wt[:, k, 0:ncols], k == 0, k == 31, [wk, "hT"], [pk])
            if pend is not None:
                pend()
            pend = consumer(tti, ps, pk)
        if pend is not None:
            pend()

    def rms_rows(ps_ap, pk, n, gtab, gk, outbf, ok, extra_reads=()):
        t, tk = t512.get(); s, sk = sm.get()
        act(t[:, 0:n], ps_ap, AF.Square, [pk] + list(extra_reads), [tk])
        rsum(s[:, 0:1], t[:, 0:n], [tk], [sk])
        act(s[:, 0:1], s[:, 0:1], AF.Sqrt, [sk, "eps"], [sk], bias=eps_t[:, 0:1], scale=1.0 / n)
        recip(s[:, 0:1], s[:, 0:1], [sk], [sk])
        stt("dve", outbf, ps_ap, s[:, 0:1], gtab, ALU.mult, ALU.mult, [pk, sk, gk] + list(extra_reads), [ok])

    def rotary(psv_ap, pk, cosr, sinr, tabk, ra, rak, rb, rbk):
        tt("dve", ra, psv_ap, cosr.unsqueeze(1).to_broadcast([128, 4, 128]), ALU.mult, [pk, tabk], [rak])
        tt("dve", rb[:, :, 0:64], psv_ap[:, :, 64:128], sinr[:, 0:64].unsqueeze(1).to_broadcast([128, 4, 64]), ALU.mult, [pk, tabk], [rbk])
        tt("dve", rb[:, :, 64:128], psv_ap[:, :, 0:64], sinr[:, 64:128].unsqueeze(1).to_broadcast([128, 4, 64]), ALU.mult, [pk, tabk], [rbk])
        tt("pool", ra, ra, rb, ALU.add, [rak, rbk], [rak])

    for half in range(2):
        dma("sp", hT[:], hk_d[:, :, half * 1024:(half + 1) * 1024], ["hk_d"], ["hT"])
        dma("sp", cos_t[:], cosk[half * 1024:(half + 1) * 1024, :].rearrange("(t p) c -> p t c", p=128), [], ["ktab"])
        dma("sp", sin_t[:], sink[half * 1024:(half + 1) * 1024, :].rearrange("(t p) c -> p t c", p=128), [], ["ktab"])
        dma("sp", dec_t[:], kdec_tab[half * 1024:(half + 1) * 1024, :].rearrange("(t p) c -> p t c", p=128), [], ["ktab"])

        def c_ckv(tti, ps, pk):
            T = half * 8 + tti
            cn, ck = b512.get()
            rms_rows(ps[:, 0:512], pk, 512, gckv_t[:], "gckv_t", cn[:], ck)
            dma("sp", ckv_tm_d[T * 128:(T + 1) * 128, :], cn[:], [ck], ["ckv_tm_d"])

            def tail():
                ct, ctk = trb.get()
                transposes(ct[:], cn[:], 4, ck, ctk)
                dma("sp", ckvT_d[:, :, T * 128:(T + 1) * 128], ct[:], [ctk], ["ckvT_d"])
            return tail

        def c_ki(tti, ps, pk):
            T = half * 8 + tti
            s, sk = sm.get(); xc, xk_ = t512.get(); sq, sqk = u512.get(); kn, knk = b512.get()
            rsum(s[:, 0:1], ps[:, 0:128], [pk], [sk])
            amul(s[:, 0:1], s[:, 0:1], 1.0 / 128, [sk], [sk])
            ts("dve", xc[:, 0:128], ps[:, 0:128], s[:, 0:1], ALU.subtract, [pk, sk], [xk_])
            act(sq[:, 0:128], xc[:, 0:128], AF.Square, [xk_], [sqk])
            rsum(s[:, 1:2], sq[:, 0:128], [sqk], [sk])
            act(s[:, 1:2], s[:, 1:2], AF.Sqrt, [sk, "eps"], [sk], bias=eps_t[:, 0:1], scale=1.0 / 128)
            recip(s[:, 1:2], s[:, 1:2], [sk], [sk])
            stt("dve", xc[:, 0:128], xc[:, 0:128], s[:, 1:2], gki_t[:], ALU.mult, ALU.mult, [xk_, sk, "gki_t"], [xk_])
            tt("dve", kn[:, 0:128], xc[:, 0:128], bki_t[:], ALU.add, [xk_, "bki_t"], [knk])
            def tail():
                ct, ctk = trb.get()
                transposes(ct[:, 0:1, :], kn[:, 0:128], 1, knk, ctk)
                dma("sp", kidxT_d[:, T * 128:(T + 1) * 128], ct[:, 0, :], [ctk], ["kidxT_d"])
            return tail

        def mk_rk(hg):
            def c_rk(tti, ps, pk):
                T = half * 8 + tti
                ra, rak = t512.get(); rb, rbk = u512.get(); kr, krk = b512.get(); kd, kdk = b512b.get()
                rav = ra[:].rearrange("p (h d) -> p h d", h=4); rbv = rb[:].rearrange("p (h d) -> p h d", h=4)
                rotary(ps[:, 0:512].rearrange("p (h d) -> p h d", h=4), pk, cos_t[:, tti, :], sin_t[:, tti, :], "ktab", rav, rak, rbv, rbk)
                cp("act", kr[:], ra[:], [rak], [krk])
                tt("dve", kd[:].rearrange("p (h d) -> p h d", h=4), rav,
                   dec_t[:, tti, hg * 4:(hg + 1) * 4].unsqueeze(2).to_broadcast([128, 4, 128]), ALU.mult, [rak, "ktab"], [kdk])
                dma("sp", kdec_d[T * 128:(T + 1) * 128, hg * 512:(hg + 1) * 512], kd[:], [kdk], ["kdec_d"])

                def tail():
                    ct, ctk = trb.get()
                    transposes(ct[:], kr[:], 4, krk, ctk)
                    dma("sp", kT_d[:, hg * 4:(hg + 1) * 4, T * 128:(T + 1) * 128], ct[:], [ctk], ["kT_d"])
                return tail
            return c_rk

        def mk_rv(i):
            def c_rv(tti, ps, pk):
                T = half * 8 + tti
                vb, vk = b512.get()
                cp("act", vb[:], ps[:, 0:512], [pk], [vk])
                dma("sp", v_d[T * 128:(T + 1) * 128, i * 512:(i + 1) * 512], vb[:], [vk], ["v_d"])
            return c_rv

        gemm_tm(hT, O_CKV, 512, c_ckv)
        gemm_tm(hT, O_KI, 128, c_ki)
        for hg in range(2):
            gemm_tm(hT, O_RK + hg * 512, 512, mk_rk(hg))
        for i in range(4):
            gemm_tm(hT, O_RV + i * 512, 512, mk_rv(i))

    gcq_t = bc_load("gcq_t", g_cq, 1024)
    cq_raw = kb.sbuf("cq_raw", [128, 8, 1024], F32)
    t1024 = kb.sbuf("t1024", [128, 1024], F32); cqn = Ring(kb, "cqn", 2, [128, 1024], BF16)
    trb8 = Ring(kb, "trb8", 2, [128, 8, 128], BF16)
    w64 = Ring(kb, "w64", 2, [128, 64], F32)
    dma("sp", hT[:], hq_d, ["hq_d"], ["hT"])
    dma("sp", cos_t[:], cosq.rearrange("(t p) c -> p t c", p=128), [], ["ktab"])
    dma("sp", sin_t[:], sinq.rearrange("(t p) c -> p t c", p=128), [], ["ktab"])
    dma("sp", dec_t[:], qdec_tab.rearrange("(t p) c -> p t c", p=128), [], ["ktab"])

    def mk_cq(g):
        def c_cq(tti, ps, pk):
            cp("act", cq_raw[:, tti, g * 512:(g + 1) * 512], ps[:, 0:512], [pk], [f"cq_raw{tti}"])
        return c_cq

    def c_wi(tti, ps, pk):
        wt_, wk_ = w64.get()
        amul(wt_[:], ps[:, 0:64], (64 ** -0.5) * (128 ** -0.5), [pk], [wk_])
        dma("sp", wi_d[tti * 128:(tti + 1) * 128, :], wt_[:], [wk_], ["wi_d"])

    def mk_rq(hg):
        def c_rq(tti, ps, pk):
            ra, rak = t512.get(); rb, rbk = u512.get(); qr, qrk = b512.get(); qd, qdk = b512b.get()
            rav = ra[:].rearrange("p (h d) -> p h d", h=4); rbv = rb[:].rearrange("p (h d) -> p h d", h=4)
            rotary(ps[:, 0:512].rearrange("p (h d) -> p h d", h=4), pk, cos_t[:, tti, :], sin_t[:, tti, :], "ktab", rav, rak, rbv, rbk)
            cp("act", qr[:], ra[:], [rak], [qrk])
            tt("dve", qd[:].rearrange("p (h d) -> p h d", h=4), rav,
               dec_t[:, tti, hg * 4:(hg + 1) * 4].unsqueeze(2).to_broadcast([128, 4, 128]), ALU.mult, [rak, "ktab"], [qdk])
            def tail():
                ct, ctk = trb.get()
                transposes(ct[:], qr[:], 4, qrk, ctk)
                dma("sp", qT_d[:, hg * 4:(hg + 1) * 4, tti * 128:(tti + 1) * 128], ct[:], [ctk], ["qT_d"])
                ct2, ctk2 = trb.get()
                transposes(ct2[:], qd[:], 4, qdk, ctk2)
                dma("sp", qdT_d[:, hg * 4:(hg + 1) * 4, tti * 128:(tti + 1) * 128], ct2[:], [ctk2], ["qdT_d"])
            return tail
        return c_rq

    def mk_rg(i):
        def c_rg(tti, ps, pk):
            gb, gk_ = b512.get()
            act(gb[:], ps[:, 0:512], AF.Silu, [pk], [gk_])
            dma("sp", rg_d[tti * 128:(tti + 1) * 128, i * 512:(i + 1) * 512], gb[:], [gk_], ["rg_d"])
        return c_rg

    for g in range(2):
        gemm_tm(hT, O_CQ + g * 512, 512, mk_cq(g))
    for tti in range(8):
        s, sk = sm.get(); cn, cnk = cqn.get()
        act(t1024[:], cq_raw[:, tti, :], AF.Square, [f"cq_raw{tti}"], ["t1024"])
        rsum(s[:, 0:1], t1024[:], ["t1024"], [sk])
        act(s[:, 0:1], s[:, 0:1], AF.Sqrt, [sk, "eps"], [sk], bias=eps_t[:, 0:1], scale=1.0 / 1024)
        recip(s[:, 0:1], s[:, 0:1], [sk], [sk])
        stt("dve", cn[:], cq_raw[:, tti, :], s[:, 0:1], gcq_t[:], ALU.mult, ALU.mult, [f"cq_raw{tti}", sk, "gcq_t"], [cnk])
        ct, ctk = trb8.get()
        transposes(ct[:, 0:4, :], cn[:, 0:512], 4, cnk, ctk)
        transposes(ct[:, 4:8, :], cn[:, 512:1024], 4, cnk, ctk)
        dma("sp", cqT_d[:, :, tti * 128:(tti + 1) * 128], ct[:], [ctk], ["cqT_d"])
    gemm_tm(hT, O_WI, 64, c_wi)
    for hg in range(2):
        gemm_tm(hT, O_RQ + hg * 512, 512, mk_rq(hg))
    for i in range(4):
        gemm_tm(hT, O_RG + i * 512, 512, mk_rg(i))
    kb.barrier(); kb.free_to(m2)
    if stages <= 2:
        return finish(nc, kb, outT)

    m3 = kb.mark()
    mret = kb.sbuf("mret", [128, 2, 8, 128], F32)
    dma("sp", mret[:], mret_in, [], ["mret"])
    gret_t = bc_load("gret_t", g_ret, 2048)
    S = kb.sbuf("S", [128, 8, 256], F32); Sbf = kb.sbuf("Sbf", [128, 8, 256], BF16)
    memset("dve", S[:], 0.0, ["S"]); memset("pool", Sbf[:], 0.0, ["Sbf"])
    qTr = Ring(kb, "qTr", 2, [128, 8, 128], BF16); qdr = Ring(kb, "qdr", 2, [128, 8, 128], BF16)
    kTr = Ring(kb, "kTr", 2, [128, 8, 256], BF16); kdr = Ring(kb, "kdr", 2, [128, 2, 1024], BF16)
    vr = Ring(kb, "vr", 2, [128, 2, 2048], BF16); rgr = Ring(kb, "rgr", 2, [128, 2048], BF16)
    ATr = Ring(kb, "ATr", 3, [128, 2, 128], BF16)
    ysq = kb.sbuf("ysq", [128, 2048], F32); yn = kb.sbuf("yn", [128, 2048], F32); yrb = Ring(kb, "yrb", 2, [128, 2048], BF16)
    ss8 = Ring(kb, "ss8", 2, [128, 8], F32)
    yrT = Ring(kb, "yrT", 2, [128, 16, 128], BF16)
    psS = PRing([pb4[:], pb5[:]], ["pb4", "pb5"])
    psK = PRing([pb6[:]], ["pb6"])
    g256 = [math.exp(256.0 * LOGG[h]) for h in range(8)]
    for j in range(8):
        qt, qk = qTr.get(); qd, qdk = qdr.get(); kt_, kk = kTr.get(); kd, kdk = kdr.get(); vt, vk = vr.get(); rg, rgk = rgr.get()
        dma("sp", qt[:], qT_d[:, :, j * 128:(j + 1) * 128], ["qT_d"], [qk])
        dma("sp", qd[:], qdT_d[:, :, j * 128:(j + 1) * 128], ["qdT_d"], [qdk])
        dma("sp", kt_[:], kT_d[:, :, j * 256:(j + 1) * 256], ["kT_d"], [kk])
        dma("sp", kd[:], kdec_d[j * 256:(j + 1) * 256, :].rearrange("(s p) c -> p s c", p=128), ["kdec_d"], [kdk])
        dma("sp", vt[:], v_d[j * 256:(j + 1) * 256, :].rearrange("(s p) c -> p s c", p=128), ["v_d"], [vk])
        dma("sp", rg[:], rg_d[j * 128:(j + 1) * 128, :], ["rg_d"], [rgk])
        for h in range(8):
            ps, pk = psS.get(); at, atk = ATr.get()
            mm(ps[:, 0:128], kt_[:, h, 0:128], qt[:, h, :], True, True, [kk, qk], [pk])
            mm(ps[:, 128:256], kt_[:, h, 128:256], qt[:, h, :], True, True, [kk, qk], [pk])
            tt("dve", at[:], ps[:, 0:256].rearrange("p (s t) -> p s t", s=2), mret[:, :, h, :], ALU.mult, [pk, "mret"], [atk])
            yk = bigk[h // 2]
            yo = pbig[:, h * 256:(h + 1) * 256]
            mm(yo, at[:, 0, :], vt[:, 0, h * 256:(h + 1) * 256], True, False, [atk, vk], [yk])
            mm(yo, at[:, 1, :], vt[:, 1, h * 256:(h + 1) * 256], False, False, [atk, vk], [yk])
            mm(yo, qd[:, h, :], Sbf[:, h, :], False, True, [qdk, "Sbf"], [yk])
        s8, s8k = ss8.get(); yb, ybk = yrb.get()
        act(ysq[:], pbig[:, :], AF.Square, bigk, ["ysq"])
        rsum(s8[:], ysq[:].rearrange("p (h v) -> p h v", h=8), ["ysq"], [s8k])
        act(s8[:], s8[:], AF.Sqrt, [s8k, "eps"], [s8k], bias=eps_t[:, 0:1], scale=1.0 / 256)
        recip(s8[:], s8[:], [s8k], [s8k])
        tt("dve", yn[:].rearrange("p (h v) -> p h v", h=8), pbig[:, :].rearrange("p (h v) -> p h v", h=8),
           s8[:].unsqueeze(2).to_broadcast([128, 8, 256]), ALU.mult, bigk + [s8k], ["yn"])
        tt("pool", yn[:], yn[:], gret_t[:], ALU.mult, ["yn", "gret_t"], ["yn"])
        tt("dve", yb[:], yn[:], rg[:], ALU.mult, ["yn", rgk], [ybk])
        yT, yTk = yrT.get()
        for q4 in range(4):
            transposes(yT[:, q4 * 4:(q4 + 1) * 4, :], yb[:, q4 * 512:(q4 + 1) * 512], 4, ybk, yTk)
        dma("sp", yrT_d[:, :, j * 128:(j + 1) * 128], yT[:], [yTk], ["yrT_d"])
        if j < 7:
            for h in range(8):
                ps, pk = psK.get()
                mm(ps[:, 0:256], kd[:, 0, h * 128:(h + 1) * 128], vt[:, 0, h * 256:(h + 1) * 256], True, False, [kdk, vk], [pk])
                mm(ps[:, 0:256], kd[:, 1, h * 128:(h + 1) * 128], vt[:, 1, h * 256:(h + 1) * 256], False, True, [kdk, vk], [pk])
                stt("dve", S[:, h, :], S[:, h, :], g256[h], ps[:, 0:256], ALU.mult, ALU.add, [pk, "S"], ["S"])
            cp("act", Sbf[:], S[:], ["S"], ["Sbf"])
    kb.barrier(); kb.free_to(m3)
    if stages <= 3:
        return finish(nc, kb, outT)

    m4 = kb.mark()
    maskT = kb.sbuf("maskT", [128, 72, 128], BF16)
    qaT = kb.sbuf("qaT", [128, 16, 1024], BF16)
    mA = kb.mark()
    cqT = kb.sbuf("cqT", [128, 8, 1024], BF16)
    dma("sp", cqT[:], cqT_d, ["cqT_d"], ["cqT"])
    kidxT = kb.sbuf("kidxT", [128, 2048], BF16)
    dma("sp", kidxT[:], kidxT_d, ["kidxT_d"], ["kidxT"])
    wi_t = kb.sbuf("wi_t", [128, 8, 64], F32)
    dma("sp", wi_t[:], wi_d.rearrange("(t p) c -> p t c", p=128), ["wi_d"], ["wi_t"])
    moff = [sum(2 * jj + 2 for jj in range(j)) for j in range(8)]
    m4b = kb.mark()
    acc = [kb.sbuf(f"acc{j}", [128, (2 * j + 2) * 128], F32) for j in range(8)]
    wqr = Ring(kb, "wqr", 1, [128, 8, 1024], BF16)
    qiT = Ring(kb, "qiT", 2, [128, 8, 1024], BF16)
    rl = Ring(kb, "rl", 3, [128, 512], F32)
    w_qi_v = w_qi.rearrange("(k p) e -> p k e", p=128)
    pr4 = PRing(big + [pb4[:], pb5[:], pb6[:]], bigk + ["pb4", "pb5", "pb6"])
    for hgp in range(8):
        wq, wqk = wqr.get(); qi, qik = qiT.get()
        dma("pool", wq[:], w_qi_v[:, :, hgp * 1024:(hgp + 1) * 1024], [], [wqk])
        for hh in range(8):
            for half in range(2):
                ps, pk = pr4.get()
                for k in range(8):
                    mm(ps[:, :], wq[:, k, hh * 128:(hh + 1) * 128], cqT[:, k, half * 512:(half + 1) * 512], k == 0, k == 7, [wqk, "cqT"], [pk])
                cp("act" if half == 0 else "dve", qi[:, hh, half * 512:(half + 1) * 512], ps[:, :], [pk], [qik])
        for j in range(8):
            W = (2 * j + 2) * 128
            for hh in range(8):
                hd = hgp * 8 + hh
                for s0 in range(0, W, 512):
                    n = min(512, W - s0)
                    ps, pk = pr4.get(); r_, rk_ = rl.get()
                    mm(ps[:, 0:n], qi[:, hh, j * 128:(j + 1) * 128], kidxT[:, s0:s0 + n], True, True, [qik, "kidxT"], [pk])
                    act(r_[:, 0:n], ps[:, 0:n], AF.Relu, [pk], [rk_])
                    if hd == 0:
                        ts("dve", acc[j][:, s0:s0 + n], r_[:, 0:n], wi_t[:, j, hd:hd + 1], ALU.mult, [rk_, "wi_t"], [f"acc{j}"])
                    else:
                        stt("dve", acc[j][:, s0:s0 + n], r_[:, 0:n], wi_t[:, j, hd:hd + 1], acc[j][:, s0:s0 + n], ALU.mult, ALU.add,
                            [rk_, "wi_t", f"acc{j}"], [f"acc{j}"])
    negb = kb.sbuf("negb", [128, 256], F32); adm01 = kb.sbuf("adm01", [128, 256], F32)
    dma("sp", negb[:], negb_in, [], ["negb"]); dma("sp", adm01[:], adm01_in, [], ["adm01"])
    wk0 = kb.sbuf("wk0", [128, 2048], F32); wk1 = kb.sbuf("wk1", [128, 2048], F32)
    mx8 = Ring(kb, "mx8", 2, [128, 8], F32)
    mrow_ = kb.sbuf("mrow_", [128, 2048], BF16)
    for j in range(8):
        W = (2 * j + 2) * 128
        a = acc[j]; ak = f"acc{j}"
        tt("dve", a[:, W - 256:W], a[:, W - 256:W], negb[:], ALU.add, [ak, "negb"], [ak])
        src, srck = a, ak
        bufs = [(wk0, "wk0"), (wk1, "wk1")]
        for it in range(32):
            mx, mxk = mx8.get()
            vmax(mx[:], src[:, 0:W], [srck], [mxk])
            if it < 31:
                dst, dstk = bufs[it % 2]
                mrep(dst[:, 0:W], mx[:], src[:, 0:W], [srck, mxk], [dstk])
                src, srck = dst, dstk
        ts("dve", mrow_[:, 0:W], a[:, 0:W], mx[:, 7:8], ALU.is_ge, [ak, mxk], ["mrow_"])
        tt("dve", mrow_[:, W - 256:W], mrow_[:, W - 256:W], adm01[:], ALU.mult, ["mrow_", "adm01"], ["mrow_"])
        for kt0 in range(0, 2 * j + 2, 4):
            n = min(4, 2 * j + 2 - kt0)
            transposes(maskT[:, moff[j] + kt0:moff[j] + kt0 + n, :], mrow_[:, kt0 * 128:(kt0 + n) * 128], n, "mrow_", "maskT")
    kb.barrier(); kb.free_to(m4b)
    if stages <= 4:
        return finish(nc, kb, outT)

    wuq = kb.sbuf("wuq", [128, 8, 2048], BF16)
    dma("pool", wuq[:], w_uq.rearrange("(k p) e -> p k e", p=128), [], ["wuq"])
    dma("sp", cqT[:], cqT_d, ["cqT_d"], ["cqT"])
    pr5 = PRing([pb4[:], pb5[:], pb6[:]], ["pb4", "pb5", "pb6"])
    for h in range(16):
        for half in range(2):
            ps, pk = pr5.get()
            for k in range(8):
                mm(ps[:, :], wuq[:, k, h * 128:(h + 1) * 128], cqT[:, k, half * 512:(half + 1) * 512], k == 0, k == 7, ["wuq", "cqT"], [pk])
            cp("act" if half == 0 else "dve", qaT[:, h, half * 512:(half + 1) * 512], ps[:, :], [pk], ["qaT"])
    kb.barrier(); kb.free_to(mA)
    ckvT = kb.sbuf("ckvT", [128, 4, 2048], BF16); ckv_tm = kb.sbuf("ckv_tm", [128, 16, 512], BF16)
    dma("sp", ckvT[:], ckvT_d, ["ckvT_d"], ["ckvT"])
    dma("sp", ckv_tm[:], ckv_tm_d.rearrange("(t p) c -> p t c", p=128), ["ckv_tm_d"], ["ckv_tm"])
    wukT = kb.sbuf("wukT", [128, 16, 512], BF16); wuv = kb.sbuf("wuv", [128, 4, 2048], BF16)
    dma("pool", wukT[:], w_ukT, [], ["wukT"])
    dma("pool", wuv[:], w_uv.rearrange("(k p) e -> p k e", p=128), [], ["wuv"])
    tb_sb = kb.sbuf("tb_sb", [32, 16], F32); oh_sb = kb.sbuf("oh_sb", [32, 512], F32); t15_sb = kb.sbuf("t15_sb", [16, 1], F32)
    dma("sp", tb_sb[:], t5b, [], ["tb_sb"]); dma("sp", oh_sb[:], oh_rel, [], ["oh_sb"]); dma("sp", t15_sb[:], t15, [], ["t15_sb"])
    amul(t15_sb[:], t15_sb[:], -1.0, ["t15_sb"], ["t15_sb"])
    mm(pb4[0:16, :], tb_sb[:], oh_sb[:], True, True, ["tb_sb", "oh_sb"], ["pb4"])
    ef_sb = kb.sbuf("ef_sb", [16, 512], F32)
    act(ef_sb[:], pb4[0:16, :], AF.Exp, ["pb4", "t15_sb"], ["ef_sb"], bias=t15_sb[:, 0:1], scale=1.0)
    dma("sp", ef_d, ef_sb[:], ["ef_sb"], ["ef_d"])
    EBp = kb.sbuf("EBp", [128, 3, 16, 128], BF16)
    mE = kb.mark()
    hk32 = kb.sbuf("hk32", [128, 16, 128], F32); hkb = kb.sbuf("hkb", [128, 16, 128], BF16)
    for n in range(3):
        src_ap = bass.AP(ef_d.tensor, n * 128, [[1, 128], [512, 16], [1, 128]])
        dma("sp", hk32[:], src_ap, ["ef_d"], ["hk32"])
        cp("dve", hkb[:], hk32[:], ["hk32"], ["hkb"])
        for h4 in range(4):
            ps, pk = pr5.get()
            for i in range(4):
                mm(ps[:, i * 128:(i + 1) * 128], hkb[:, h4 * 4 + i, :], anti[:], True, True, ["hkb", "anti"], [pk])
            cp("act", EBp[:, n, h4 * 4:(h4 + 1) * 4, :], ps[:, :].rearrange("p (h t) -> p h t", h=4), [pk], ["EBp"])
    kb.barrier(); kb.free_to(mE)
    qlat = Ring(kb, "qlat", 1, [128, 4, 16, 128], BF16)
    MBn = Ring(kb, "MBn", 1, [128, 3, 16, 128], BF16)
    Er = Ring(kb, "Er", 3, [128, 512], F32)
    PTr = Ring(kb, "PTr", 3, [128, 512], BF16)
    rz = Ring(kb, "rz", 2, [128, 512], F32)
    olat = Ring(kb, "olat", 1, [128, 4, 16, 128], BF16)
    yaT = Ring(kb, "yaT", 2, [128, 16, 128], BF16)
    prL = PRing([pb5[:], pb6[:]], ["pb5", "pb6"])
    for j in range(8):
        ql, qlk = qlat.get()
        for cc in range(4):
            for h4 in range(4):
                ps, pk = prL.get()
                for i in range(4):
                    h = h4 * 4 + i
                    mm(ps[:, i * 128:(i + 1) * 128], wukT[:, h, cc * 128:(cc + 1) * 128], qaT[:, h, j * 128:(j + 1) * 128], True, True, ["wukT", "qaT"], [pk])
                amul(ql[:, cc, h4 * 4:(h4 + 1) * 4, :], ps[:, :].rearrange("p (h t) -> p h t", h=4), 128 ** -0.5, [pk], [qlk])
        nk = 2 * j + 2
        near = [kt for kt in (2 * j - 1, 2 * j, 2 * j + 1) if kt >= 0]
        mb, mbk = MBn.get()
        for kt in near:
            n = kt - (2 * j - 1)
            tt("pool", mb[:, n, :, :], EBp[:, n, :, :], maskT[:, moff[j] + kt, :].unsqueeze(1).to_broadcast([128, 16, 128]), ALU.mult, ["EBp", "maskT"], [mbk])
        ol, olk = olat.get()
        pend5 = None
        for hg in range(4):
            for kt in range(nk):
                ps, pk = prL.get(); E, Ek = Er.get(); PT, PTk = PTr.get()
                for cc in range(4):
                    mm(ps[:, :], ckvT[:, cc, kt * 128:(kt + 1) * 128], ql[:, cc, hg * 4:(hg + 1) * 4, :], cc == 0, cc == 3, ["ckvT", qlk], [pk])
                act(E[:], ps[:, :], AF.Exp, [pk], [Ek])
                if kt in near:
                    n = kt - (2 * j - 1)
                    tt("dve", PT[:].rearrange("p (h t) -> p h t", h=4), E[:].rearrange("p (h t) -> p h t", h=4), mb[:, n, hg * 4:(hg + 1) * 4, :], ALU.mult, [Ek, mbk], [PTk])
                else:
                    tt("dve", PT[:].rearrange("p (h t) -> p h t", h=4), E[:].rearrange("p (h t) -> p h t", h=4),
                       maskT[:, moff[j] + kt, :].unsqueeze(1).to_broadcast([128, 4, 128]), ALU.mult, [Ek, "maskT"], [PTk])
                if pend5 is not None:
                    pend5()

                def tail5(kt=kt, PT=PT, PTk=PTk, nk=nk):
                    for cc in range(4):
                        mm(big[cc], ckv_tm[:, kt, cc * 128:(cc + 1) * 128], PT[:], kt == 0, kt == nk - 1, ["ckv_tm", PTk], [bigk[cc]])
                    mm(pb4[:, :], ones[:], PT[:], kt == 0, kt == nk - 1, ["ones", PTk], ["pb4"])
                pend5 = tail5
            pend5(); pend5 = None
            r_, rk_ = rz.get()
            recip(r_[:], pb4[:, :], ["pb4"], [rk_])
            for cc in range(4):
                tt("dve", ol[:, cc, hg * 4:(hg + 1) * 4, :], big[cc].rearrange("p (h t) -> p h t", h=4), r_[:].rearrange("p (h t) -> p h t", h=4), ALU.mult, [bigk[cc], rk_], [olk])
        ya, yak = yaT.get()
        for h4 in range(4):
            ps, pk = prL.get()
            for i in range(4):
                h = h4 * 4 + i
                for cc in range(4):
                    mm(ps[:, i * 128:(i + 1) * 128], wuv[:, cc, h * 128:(h + 1) * 128], ol[:, cc, h, :], cc == 0, cc == 3, ["wuv", olk], [pk])
            cp("act", ya[:, h4 * 4:(h4 + 1) * 4, :], ps[:, :].rearrange("p (h t) -> p h t", h=4), [pk], [yak])
        dma("sp", yaT_d[:, :, j * 128:(j + 1) * 128], ya[:], [yak], ["yaT_d"])
    kb.barrier(); kb.free_to(m4)
    if stages <= 5:
        return finish(nc, kb, outT)

    def gemm_fm(actT, akey, nk, w_view, col0, wring_, consumer):
        wt, wk = wring_.get()
        dma("pool", wt[:, 0:nk, :], w_view[:, :, col0:col0 + 512], [], [wk])
        for cc in range(4):
            for half in range(2):
                ps, pk = prF.get()
                for k in range(nk):
                    mm(ps[:, :], wt[:, k, cc * 128:(cc + 1) * 128], actT[:, k, half * 512:(half + 1) * 512], k == 0, k == nk - 1, [wk, akey], [pk])
                consumer(col0 // 128 + cc, half, ps, pk)

    prF = PRing(big + [pb4[:], pb5[:], pb6[:]], bigk + ["pb4", "pb5", "pb6"])
    m6 = kb.mark()
    actA = kb.sbuf("actA", [128, 32, 1024], BF16)
    wrF = Ring(kb, "wrF", 2, [128, 32, 512], BF16)
    bg = kb.sbuf("bg", [128, 64], F32)
    dma("sp", bg[:], b_gate, [], ["bg"])
    dma("sp", actA[:], hq_d, ["hq_d"], ["actA"])
    o512 = Ring(kb, "o512", 3, [128, 512], BF16)
    w_gate_v = w_gate.rearrange("(k p) e -> p k e", p=128)

    def c_gate(gc, half, ps, pk):
        o, ok = o512.get()
        act(o[:], ps[:, :], AF.Sigmoid, [pk, "bg"], [ok], bias=bg[:, gc:gc + 1], scale=1.0)
        dma("sp", gate_d[:, gc, half * 512:(half + 1) * 512], o[:], [ok], ["gate_d"])

    for g in range(16):
        gemm_fm(actA, "actA", 32, w_gate_v, g * 512, wrF, c_gate)
    kb.barrier(); kb.free_to(m6)

    m6 = kb.mark()
    yaS = kb.sbuf("yaS", [128, 16, 1024], BF16); yrS = kb.sbuf("yrS", [128, 16, 1024], BF16)
    dma("sp", yaS[:], yaT_d, ["yaT_d"], ["yaS"]); dma("sp", yrS[:], yrT_d, ["yrT_d"], ["yrS"])
    wa = Ring(kb, "wa", 2, [128, 16, 512], BF16); wrr = Ring(kb, "wrr", 2, [128, 16, 512], BF16)
    gar = Ring(kb, "gar", 2, [128, 512], BF16); grr = Ring(kb, "grr", 2, [128, 512], BF16)
    f512 = Ring(kb, "f512", 2, [128, 512], F32); f512b = Ring(kb, "f512b", 2, [128, 512], F32)
    o512 = Ring(kb, "o512", 3, [128, 512], BF16)
    w_upa_v = w_up[0:2048, :].rearrange("(k p) e -> p k e", p=128)
    w_upr_v = w_up[2048:4096, :].rearrange("(k p) e -> p k e", p=128)
    for g in range(8):
        wat, wak = wa.get(); wrt, wrk = wrr.get()
        dma("pool", wat[:], w_upa_v[:, :, g * 512:(g + 1) * 512], [], [wak])
        dma("pool", wrt[:], w_upr_v[:, :, g * 512:(g + 1) * 512], [], [wrk])
        for cc in range(4):
            dc = g * 4 + cc
            for half in range(2):
                psa, pka = prF.get(); psr, pkr = prF.get()
                for k in range(16):
                    mm(psa[:, :], wat[:, k, cc * 128:(cc + 1) * 128], yaS[:, k, half * 512:(half + 1) * 512], k == 0, k == 15, [wak, "yaS"], [pka])
                for k in range(16):
                    mm(psr[:, :], wrt[:, k, cc * 128:(cc + 1) * 128], yrS[:, k, half * 512:(half + 1) * 512], k == 0, k == 15, [wrk, "yrS"], [pkr])
                ga, gak = gar.get(); gr, grk = grr.get(); t1, t1k = f512.get(); t2, t2k = f512b.get(); o, ok = o512.get()
                dma("sp", ga[:], gate_d[:, dc, half * 512:(half + 1) * 512], ["gate_d"], [gak])
                dma("sp", gr[:], gate_d[:, 32 + dc, half * 512:(half + 1) * 512], ["gate_d"], [grk])
                tt("dve", t1[:], psa[:, :], ga[:], ALU.mult, [pka, gak], [t1k])
                tt("dve", t2[:], psr[:, :], gr[:], ALU.mult, [pkr, grk], [t2k])
                tt("pool", o[:], t1[:], t2[:], ALU.add, [t1k, t2k], [ok])
                dma("sp", mrg_d[:, dc, half * 512:(half + 1) * 512], o[:], [ok], ["mrg_d"])
    kb.barrier(); kb.free_to(m6)

    m6 = kb.mark()
    actA = kb.sbuf("actA", [128, 32, 1024], BF16)
    wrF = Ring(kb, "wrF", 2, [128, 32, 512], BF16)
    dma("sp", actA[:], mrg_d, ["mrg_d"], ["actA"])
    xr5 = Ring(kb, "xr5", 3, [128, 512], F32); x1r = Ring(kb, "x1r", 3, [128, 512], F32)
    w_out_v = w_out.rearrange("(k p) e -> p k e", p=128)

    def c_out(dc, half, ps, pk):
        xt, xk_ = xr5.get(); x1, x1k = x1r.get()
        dma("sp", xt[:], xq_v[:, dc, half * 512:(half + 1) * 512], [], [xk_])
        stt("dve", x1[:], ps[:, :], modT[:, 64 + dc:65 + dc], xt[:], ALU.mult, ALU.add, [pk, xk_, "modT"], [x1k])
        dma("sp", x1T_d[:, dc, half * 512:(half + 1) * 512], x1[:], [x1k], ["x1T_d"])

    for g in range(8):
        gemm_fm(actA, "actA", 32, w_out_v, g * 512, wrF, c_out)
    kb.barrier(); kb.free_to(m6)
    phase_norm(x1T_d, 1024, A2[:], modT[:, 96:128], h2T_d, "h2T_d")
    if stages <= 6:
        return finish(nc, kb, outT)

    pst_d = dscr("pst_d", [8, 128, 2, 8, 128], F32); dl_d = dscr("dl_d", [8, 128, 8], F32)
    m7 = kb.mark()
    qTs = kb.sbuf("qTs", [128, 16, 1024], BF16)
    kT2 = kb.sbuf("kT2", [128, 2, 128], BF16)
    dma("pool", kT2[:], keysT.rearrange("s d k -> d s k"), [], ["kT2"])
    m7g = kb.mark()
    actA = kb.sbuf("actA", [128, 32, 1024], BF16)
    wrF = Ring(kb, "wrF", 2, [128, 32, 512], BF16)
    dma("sp", actA[:], h2T_d, ["h2T_d"], ["actA"])
    w_pq_v = w_pq.rearrange("(k p) e -> p k e", p=128)

    def c_pq(qc, half, ps, pk):
        cp("act" if half == 0 else "dve", qTs[:, qc, half * 512:(half + 1) * 512], ps[:, :], [pk], ["qTs"])

    for g in range(4):
        gemm_fm(actA, "actA", 32, w_pq_v, g * 512, wrF, c_pq)
    kb.barrier(); kb.free_to(m7g)
    s_sb = Ring(kb, "s_sb", 2, [128, 16, 128], F32)
    tmpk = Ring(kb, "tmpk", 2, [128, 128], F32)
    v16 = Ring(kb, "v16", 2, [128, 16, 16], F32)
    cand = Ring(kb, "cand", 2, [128, 8, 256], F32); cand2 = Ring(kb, "cand2", 2, [128, 256], F32)
    top16 = Ring(kb, "top16", 2, [128, 8, 16], F32)
    e16 = Ring(kb, "e16", 2, [128, 8, 16], F32)
    sm8 = Ring(kb, "sm8", 2, [128, 4, 8], F32)
    pstS = Ring(kb, "pstS", 2, [128, 2, 8, 128], F32)
    for tti in range(8):
        ss, ssk = s_sb.get()
        for qc in range(16):
            mm(pbig[:, qc * 128:(qc + 1) * 128], qTs[:, qc, tti * 128:(tti + 1) * 128], kT2[:, qc % 2, :], True, True, ["qTs", "kT2"], [bigk[qc // 4]])
        cp("act", ss[:], pbig[:, :].rearrange("p (q k) -> p q k", q=16), bigk, [ssk])
        v, vk_ = v16.get()
        for qc in range(16):
            tm, tmk = tmpk.get()
            vmax(v[:, qc, 0:8], ss[:, qc, :], [ssk], [vk_])
            mrep(tm[:], v[:, qc, 0:8], ss[:, qc, :], [ssk, vk_], [tmk])
            vmax(v[:, qc, 8:16], tm[:], [tmk], [vk_])
        cd, cdk = cand.get(); tp, tpk = top16.get()
        vv = v[:].rearrange("p (h s) a -> p h s a", s=2)
        tt("dve", cd[:].rearrange("p h (a b) -> p h a b", a=16), vv[:, :, 0, :].unsqueeze(3).to_broadcast([128, 8, 16, 16]),
           vv[:, :, 1, :].unsqueeze(2).to_broadcast([128, 8, 16, 16]), ALU.add, [vk_], [cdk])
        for h in range(8):
            c2, c2k = cand2.get()
            vmax(tp[:, h, 0:8], cd[:, h, :], [cdk], [tpk])
            mrep(c2[:], tp[:, h, 0:8], cd[:, h, :], [cdk, tpk], [c2k])
            vmax(tp[:, h, 8:16], c2[:], [c2k], [tpk])
        ee, eek = e16.get(); s8, s8k = sm8.get(); po, pok = pstS.get()
        tt("dve", ee[:], tp[:], tp[:, :, 0:1].to_broadcast([128, 8, 16]), ALU.subtract, [tpk], [eek])
        act(ee[:], ee[:], AF.Exp, [eek], [eek])
        rsum(s8[:, 0, :], ee[:], [eek], [s8k])
        act(s8[:, 1, :], s8[:, 0, :], AF.Ln, [s8k], [s8k])
        tt("dve", s8[:, 2, :], s8[:, 1, :], tp[:, :, 0], ALU.add, [s8k, tpk], [s8k])
        tt("dve", s8[:, 3, :], tp[:, :, 15], s8[:, 2, :], ALU.subtract, [s8k, tpk], [s8k])
        ssv = ss[:].rearrange("p (h s) k -> p h s k", s=2)
        tt("dve", po[:, 0, :, :], ssv[:, :, 0, :], s8[:, 2, :].unsqueeze(2).to_broadcast([128, 8, 128]), ALU.subtract, [ssk, s8k], [pok])
        ts("dve", s8[:, 3, :], s8[:, 3, :], -1.0e-5, ALU.add, [s8k], [s8k])
        cp("pool", po[:, 1, :, :], ssv[:, :, 1, :], [ssk], [pok])
        dma("sp", pst_d[tti], po[:], [pok], ["pst_d"])
        dma("sp", dl_d[tti], s8[:, 3, :], [s8k], ["dl_d"])
    kb.barrier(); kb.free_to(m7)

    m7 = kb.mark()
    actA = kb.sbuf("actA", [128, 32, 1024], BF16)
    dma("sp", actA[:], h2T_d, ["h2T_d"], ["actA"])
    pstA = kb.sbuf("pstA", [128, 8, 2, 8, 128], F32); dlA = kb.sbuf("dlA", [128, 8, 8], F32)
    for tti in range(8):
        dma("sp", pstA[:, tti], pst_d[tti], ["pst_d"], ["pstA"])
        dma("sp", dlA[:, tti, :], dl_d[tti], ["dl_d"], ["dlA"])
    uTr = Ring(kb, "uTr", 2, [128, 32, 256], BF16)
    Sr = Ring(kb, "Sr", 2, [128, 8, 2, 128], F32); Er2 = Ring(kb, "Er2", 2, [128, 8, 2, 128], BF16); G8r = Ring(kb, "G8r", 3, [128, 8, 2, 128], BF16)
    geTr = Ring(kb, "geTr", 2, [128, 2, 512], BF16); cTr2 = Ring(kb, "cTr2", 2, [128, 2, 512], BF16)
    uT_v = uT.rearrange("(k p) e -> p k e", p=128)
    prAct = PRing([(big[0], big[1]), (big[2], big[3])], [(bigk[0], bigk[1]), (bigk[2], bigk[3])])
    prG = PRing([(pb4[:], pb5[:]), (pb6[:], pb7[:])], [("pb4", "pb5"), ("pb6", "pb7")])
    for eg in range(64):
        ut, utk = uTr.get()
        dma("pool", ut[:], uT_v[:, :, eg * 256:(eg + 1) * 256], [], [utk])
        for tq in range(2):
            aps, aks = prAct.get(); gps, gks = prG.get()
            geT, geTk = geTr.get(); cT_, cTk = cTr2.get()
            for t4 in range(4):
                tti = tq * 4 + t4
                i_ = t4 // 2
                for k in range((t4 % 2) * 16, (t4 % 2) * 16 + 16):
                    mm(aps[i_], ut[:, k, i_ * 128:(i_ + 1) * 128], actA[:, k, tq * 512:(tq + 1) * 512], k == 0, k == 31, ["actA", utk], [aks[i_]])
                if t4 % 2 == 1:
                    act(geT[:, i_, :], aps[i_], AF.Gelu, [aks[i_]], [geTk + str(i_)])
                S_, Sk = Sr.get(); E_, Ek = Er2.get(); G8, G8k = G8r.get()
                tt("pool", S_[:], pstA[:, tti, 1, :, :].unsqueeze(2).to_broadcast([128, 8, 2, 128]),
                   pstA[:, tti, 0, :, eg * 2:(eg + 1) * 2].unsqueeze(3).to_broadcast([128, 8, 2, 128]), ALU.add, ["pstA"], [Sk])
                act(E_[:], S_[:], AF.Exp, [Sk], [Ek])
                for h in range(8):
                    stt("dve", G8[:, h, :, :], S_[:, h, :, :], dlA[:, tti, h:h + 1], E_[:, h, :, :], ALU.is_ge, ALU.mult, [Sk, Ek, "dlA"], [f"{G8k}h{h}"])
                for i in range(2):
                    for h in range(8):
                        mm(gps[i][:, t4 * 128:(t4 + 1) * 128], G8[:, h, i, :], ident[:], h == 0, h == 7, [f"{G8k}h{h}", "ident"], [gks[i]])
            for i in range(2):
                tt("dve", cT_[:, i, :], gps[i], geT[:, i, :], ALU.mult, [gks[i], geTk + str(i)], [cTk])
            dma("sp", coefT_d[eg * 256:(eg + 1) * 256, tq * 512:(tq + 1) * 512].rearrange("(i e) t -> e i t", e=128), cT_[:], [cTk], ["coefT_d"])
    kb.barrier(); kb.free_to(m7)

    m7 = kb.mark()
    vtr = Ring(kb, "vtr", 10, [128, 512], BF16); cTr = Ring(kb, "cTr", 10, [128, 1024], BF16)
    xr5 = Ring(kb, "xr5", 3, [128, 512], F32); x1r = Ring(kb, "x1r", 3, [128, 512], F32)
    banks = big + [pb4[:], pb5[:], pb6[:], pb7[:]]
    bkeys = bigk + ["pb4", "pb5", "pb6", "pb7"]
    for ds in range(8):
        for et in range(128):
            vt, vk_ = vtr.get(); ct, ctk = cTr.get()
            dma("pool", vt[:], v_exp[et * 128:(et + 1) * 128, ds * 512:(ds + 1) * 512], [], [vk_])
            dma("sp", ct[:], coefT_d[et * 128:(et + 1) * 128, :], ["coefT_d"], [ctk])
            for cc in range(4):
                for half in range(2):
                    bi = cc * 2 + half
                    wkeys = [bkeys[bi]]
                    mm(banks[bi], vt[:, cc * 128:(cc + 1) * 128], ct[:, half * 512:(half + 1) * 512], et == 0, et == 127, [vk_, ctk], wkeys)
        for cc in range(4):
            dc = ds * 4 + cc
            for half in range(2):
                bi = cc * 2 + half
                xt, xk_ = xr5.get(); x2, x2k = x1r.get()
                dma("sp", xt[:], x1T_d[:, dc, half * 512:(half + 1) * 512], ["x1T_d"], [xk_])
                stt("dve", x2[:], banks[bi], modT[:, 160 + dc:161 + dc], xt[:], ALU.mult, ALU.add, [bkeys[bi], xk_, "modT"], [x2k])
                dma("sp", x2T_d[:, dc, half * 512:(half + 1) * 512], x2[:], [x2k], ["x2T_d"])
    kb.barrier(); kb.free_to(m7)

    gfin = kb.sbuf("gfin", [128, 32], F32)
    dma("sp", gfin[:], g_final, [], ["gfin"])
    phase_norm(x2T_d, 1024, gfin[:], None, outT.rearrange("(k p) t -> p k t", p=128), "outT")
    return finish(nc, kb, outT)


def finish(nc, kb, outT):
    kb.barrier()
    kb.emit()
    kb.close()
    return nc


def _t5_bucket_np(rel):
    n = np.abs(rel)
    lr = np.log(np.maximum(n, 1).astype(np.float32) / np.float32(8)) / np.float32(math.log(128 / 8))
    large = np.minimum(8 + (lr * np.float32(8)).astype(np.int32), 15)
    return np.where(rel > 0, 16, 0) + np.where(n < 8, n, large)


_CONST_CACHE = {}


def _consts(p):
    if p in _CONST_CACHE:
        return _CONST_CACHE[p]
    f32 = np.float32
    own = np.concatenate([np.arange((2 * j + p) * 128, (2 * j + p + 1) * 128) for j in range(8)])
    inv = (1.0 / (f32(10000.0) ** np.linspace(0.0, 1.0, 64, dtype=f32))).astype(f32)
    pos = np.arange(2048, dtype=f32)
    ang = pos[:, None] * inv[None, :]
    cos, sin = np.cos(ang).astype(f32), np.sin(ang).astype(f32)
    ks = f32(128 ** -0.5)
    cos2 = np.concatenate([cos, cos], 1); sin2 = np.concatenate([-sin, sin], 1)
    logg = np.array(LOGG, dtype=np.float64)
    s_in = np.arange(2048) % 256
    kdec = np.exp((255 - s_in)[:, None] * logg[None, :]).astype(f32)
    qdec = np.exp(((own % 256) + 1)[:, None] * logg[None, :]).astype(f32)
    sg = (np.arange(2)[:, None] * 128 + np.arange(128)[None, :])
    tg = 128 * p + np.arange(128)
    diff = tg[None, None, :] - sg[:, :, None]
    m = np.where(diff[:, :, None, :] >= 0, np.exp(np.maximum(diff, 0)[:, :, None, :] * logg[None, None, :, None]), 0.0)
    mret = np.ascontiguousarray(np.transpose(m, (1, 0, 2, 3))).astype(f32)
    tq = np.arange(128); s2 = np.arange(256)
    adm01 = ((s2[None, :] // 64) <= (2 * p + tq[:, None] // 64)).astype(f32)
    negb = ((adm01 - 1.0) * 1.0e30).astype(f32)
    mm_ = np.arange(512)
    rel = mm_ - 127 - (1 + p) * 128
    bk = _t5_bucket_np(rel)
    oh = (bk[None, :] == np.arange(32)[:, None]).astype(f32)
    oh[:, 511] = 0.0
    c = dict(own=own, cosk=(cos2 * ks).astype(f32), sink=(sin2 * ks).astype(f32), kdec_tab=kdec,
             cosq=np.ascontiguousarray(cos2[own]), sinq=np.ascontiguousarray(sin2[own]), qdec_tab=qdec,
             mret=mret, adm01=adm01, negb=negb, oh_rel=oh,
             ident=np.eye(128, dtype=f32), anti=np.ascontiguousarray(np.eye(128, dtype=f32)[::-1]))
    _CONST_CACHE[p] = c
    return c


def _shared(inp):
    f = lambda a: np.ascontiguousarray(a, dtype=np.float32)
    ch = lambda v: f(np.asarray(v).reshape(-1, 128).T)
    sh = dict(
        w_ada=f(inp["w_ada"][0]), b_ada=f(inp["b_ada"][0].reshape(1, -1)),
        g_mix=ch(inp["g_mix"][0]), g_ffn=ch(inp["g_ffn"][0]), g_final=ch(inp["g_final"]),
        w_in=f(inp["w_in"][0]), g_cq=f(inp["g_cq"][0]), g_ckv=f(inp["g_ckv"][0]), g_ki=f(inp["g_ki"][0]), b_ki=f(inp["b_ki"][0]),
        g_ret=f(inp["g_ret"][0].reshape(-1)),
        w_uq=f(inp["w_uq"][0].reshape(1024, 2048)), w_ukT=f(np.transpose(inp["w_uk"][0], (2, 1, 0))),
        w_uv=f(inp["w_uv"][0].reshape(512, 2048)), w_qi=f(inp["w_qi"][0].reshape(1024, 8192)),
        t5b=f(inp["t5_bias"]), t15=f(inp["t5_bias"][15].reshape(16, 1)),
        w_up=f(inp["w_up"][0]), w_gate=f(inp["w_gate"][0]), b_gate=ch(inp["b_gate"][0]), w_out=f(inp["w_out"][0]),
        w_pq=f(inp["w_pq"][0]), keysT=f(np.transpose(inp["sub_keys"][0], (0, 2, 1))),
        uT=f(inp["u_exp"][0].T), v_exp=f(inp["v_exp"][0]),
    )
    return sh


def make_in_maps(inp, cores=None, names=None):
    sh = _shared(inp)
    x = np.asarray(inp["x"], dtype=np.float32); c = np.asarray(inp["c"], dtype=np.float32)
    maps = []
    for core in (range(8) if cores is None else cores):
        b, p = core // 2, core % 2
        cs = _consts(p)
        m = dict(sh)
        m["_core"] = core
        m["xkT"] = np.ascontiguousarray(x[b].T)
        m["xqT"] = np.ascontiguousarray(x[b][cs["own"]].T)
        m["cT"] = np.ascontiguousarray(c[b].reshape(32, 128).T)
        for k in ("cosk", "sink", "kdec_tab", "cosq", "sinq", "qdec_tab", "mret", "adm01", "negb", "oh_rel", "ident", "anti"):
            m[k] = cs[k]
        m.pop("_core")
        if names is not None:
            m = {k: v for k, v in m.items() if k in names}
        maps.append(m)
    return maps


def kernel(**inputs):
    inp = {k: np.asarray(v) for k, v in inputs.items()}
    nc = build()
    maps = make_in_maps(inp, names=set(_USED["names"]))
    res = run_bass_kernel_spmd(nc, maps, core_ids=list(range(8)))
    out = np.zeros((4, 2048, 4096), dtype=np.float32)
    for core in range(8):
        b, p = core // 2, core % 2
        own = _consts(p)["own"]
        out[b, own, :] = np.asarray(res.results[core]["outT"]).T
    return out
```

```python
import math
import numpy as np
import concourse.bass as bass
import concourse.mybir as mybir
from concourse.bass_utils import run_bass_kernel_spmd

F32 = mybir.dt.float32
BF16 = mybir.dt.bfloat16
ALU = mybir.AluOpType
AF = mybir.ActivationFunctionType
AX = mybir.AxisListType

EPOCH = 12000
N_DMA_SEMS = 40
EPS = 1e-6
NEG = -1.0e30


class KB:
    ENGS = ("pe", "act", "dve", "pool", "sp")

    def __init__(self, nc):
        self.nc = nc
        self.stack = []
        self.streams = {e: [] for e in self.ENGS}
        self.cnt = {e: 0 for e in self.ENGS}
        self.cur_sem = {}
        self.known = {e: {} for e in self.ENGS}
        self.res = {}
        self.sem_objs = {}
        self.n_sems = 0
        for e in self.ENGS:
            self.cur_sem[e] = self._new_sem(f"s_{e}")
        self.dma_sems = [self._new_sem(f"s_dma{i}") for i in range(N_DMA_SEMS)]
        self.dma_cnt = [0] * N_DMA_SEMS
        self.dma_rr = 0
        self.n_inst = 0

    def _new_sem(self, name):
        cm = self.nc.semaphore(f"{name}_{self.n_sems}")
        s = cm.__enter__()
        self.stack.append(cm)
        sid = self.n_sems
        self.n_sems += 1
        self.sem_objs[sid] = s
        return sid

    def sbuf(self, name, shape, dtype):
        self.n_alloc = getattr(self, "n_alloc", 0) + 1
        cm = self.nc.sbuf_tensor(f"sb{self.n_alloc}_{name}", list(shape), dtype)
        t = cm.__enter__()
        self.stack.append(cm)
        return t

    def psum(self, name, shape, dtype):
        cm = self.nc.psum_tensor(name, list(shape), dtype)
        t = cm.__enter__()
        self.stack.append(cm)
        return t

    def mark(self):
        return len(self.stack)

    def free_to(self, mark):
        while len(self.stack) > mark:
            cm = self.stack.pop()
            cm.__exit__(None, None, None)

    def _need(self, eng, reads, writes, pe_accum=False):
        need = {}

        def add(ev):
            if ev is None:
                return
            sid, val = ev
            if need.get(sid, 0) < val:
                need[sid] = val

        for r in reads:
            st = self.res.get(r)
            if st is not None:
                add(st[0])
        for w in writes:
            st = self.res.get(w)
            if st is not None:
                if not (pe_accum and st[0] is not None and st[0][0] == self.cur_sem[eng]):
                    add(st[0])
                for sid, val in st[1].items():
                    add((sid, val))
        waits = []
        kn = self.known[eng]
        own = self.cur_sem[eng]
        for sid, val in need.items():
            if kn.get(sid, 0) >= val:
                continue
            if sid == own and eng in ("pe", "sp"):
                continue
            kn[sid] = val
            waits.append((sid, val))
        return waits

    def _commit(self, ev, reads, writes):
        for w in writes:
            self.res[w] = [ev, {}]
        for r in reads:
            st = self.res.setdefault(r, [None, {}])
            sid, val = ev
            if st[1].get(sid, 0) < val:
                st[1][sid] = val

    def op(self, eng, fn, reads=(), writes=(), pe_accum=False):
        if self.cnt[eng] >= EPOCH:
            self.cur_sem[eng] = self._new_sem(f"s_{eng}")
            self.cnt[eng] = 0
        excl = [r for r in reads if r.startswith("pb")]
        if excl:
            reads = [r for r in reads if not r.startswith("pb")]
            writes = list(writes) + excl
        waits = self._need(eng, reads, writes, pe_accum)
        self.cnt[eng] += 1
        ev = (self.cur_sem[eng], self.cnt[eng])
        self.streams[eng].append((waits, fn, (ev[0], 1)))
        self._commit(ev, reads, writes)
        self.n_inst += 1
        return ev

    def dma(self, q, fn, reads=(), writes=()):
        k = self.dma_rr
        self.dma_rr = (self.dma_rr + 1) % N_DMA_SEMS
        sid = self.dma_sems[k]
        waits = self._need(q, reads, writes)
        prev = self.dma_cnt[k]
        if prev > 0 and self.known[q].get(sid, 0) < prev:
            self.known[q][sid] = prev
            waits.append((sid, prev))
        self.dma_cnt[k] += 16
        ev = (sid, self.dma_cnt[k])
        self.streams[q].append((waits, fn, (sid, 16)))
        self._commit(ev, reads, writes)
        self.n_inst += 1
        return ev

    def barrier(self):
        evs = []
        for e in self.ENGS:
            if self.cnt[e] > 0:
                evs.append((self.cur_sem[e], self.cnt[e]))
        for k in range(N_DMA_SEMS):
            if self.dma_cnt[k] > 0:
                evs.append((self.dma_sems[k], self.dma_cnt[k]))
        for e in self.ENGS:
            waits = []
            for sid, val in evs:
                if sid == self.cur_sem[e]:
                    continue
                if self.known[e].get(sid, 0) < val:
                    self.known[e][sid] = val
                    waits.append((sid, val))
            if waits:
                self.streams[e].append((waits, None, None))
        self.res = {}

    def emit(self):
        nc = self.nc
        so = self.sem_objs
        streams = self.streams
        with nc.Block() as block:
            def run(engobj, items):
                for waits, fn, inc in items:
                    for sid, val in waits:
                        engobj.wait_ge(so[sid], val)
                    if fn is not None:
                        ins = fn(engobj)
                        ins.then_inc(so[inc[0]], inc[1])

            @block.tensor
            def _(e):
                run(e, streams["pe"])

            @block.scalar
            def _(e):
                run(e, streams["act"])

            @block.vector
            def _(e):
                run(e, streams["dve"])

            @block.gpsimd
            def _(e):
                run(e, streams["pool"])

            @block.sync
            def _(e):
                run(e, streams["sp"])

    def close(self):
        self.free_to(0)


class Ring:
    def __init__(self, kb, name, n, shape, dtype):
        self.tiles = [kb.sbuf(f"{name}{i}", shape, dtype) for i in range(n)]
        self.keys = [f"{name}{i}" for i in range(n)]
        self.i = 0

    def get(self):
        t, k = self.tiles[self.i], self.keys[self.i]
        self.i = (self.i + 1) % len(self.tiles)
        return t, k


class PRing:
    def __init__(self, aps, keys):
        self.aps, self.keys, self.i = aps, keys, 0

    def get(self):
        t, k = self.aps[self.i], self.keys[self.i]
        self.i = (self.i + 1) % len(self.aps)
        return t, k


O_CQ, O_CKV, O_KI, O_WI, O_RQ, O_RK, O_RV, O_RG = 0, 1024, 1536, 1664, 1728, 2752, 3776, 5824
LOGG = [math.log(1.0 - 2.0 ** (-5.0 - h)) for h in range(8)]


_USED = {}


def build(stages=99, dbg=()):
    nc = bass.Bass("TRN2", target_bir_lowering=False)
    kb = KB(nc)

    used = []
    _USED["names"] = used

    def din(name, shape, dt=F32):
        used.append(name)
        return nc.dram_tensor(name, list(shape), dt, kind="ExternalInput").ap()

    def dscr(name, shape, dt):
        kind = "ExternalOutput" if name in dbg else "Internal"
        return nc.dram_tensor(name, list(shape), dt, kind=kind).ap()

    xkT = din("xkT", [4096, 2048]); xqT = din("xqT", [4096, 1024]); cT = din("cT", [128, 32])
    w_ada = din("w_ada", [4096, 24576]); b_ada = din("b_ada", [1, 24576])
    g_mix = din("g_mix", [128, 32]); g_ffn = din("g_ffn", [128, 32]); g_final = din("g_final", [128, 32])
    w_in = din("w_in", [4096, 7872])
    g_cq = din("g_cq", [1024]); g_ckv = din("g_ckv", [512]); g_ki = din("g_ki", [128]); b_ki = din("b_ki", [128])
    g_ret = din("g_ret", [2048])
    if stages >= 4:
        w_uq = din("w_uq", [1024, 2048]); w_ukT = din("w_ukT", [128, 16, 512]); w_uv = din("w_uv", [512, 2048])
        w_qi = din("w_qi", [1024, 8192])
        t5b = din("t5b", [32, 16]); t15 = din("t15", [16, 1]); oh_rel = din("oh_rel", [32, 512])
    if stages >= 6:
        w_up = din("w_up", [4096, 4096]); w_gate = din("w_gate", [4096, 8192]); b_gate = din("b_gate", [128, 64])
        w_out = din("w_out", [4096, 4096])
    if stages >= 7:
        w_pq = din("w_pq", [4096, 2048]); keysT = din("keysT", [2, 128, 128])
        uT = din("uT", [4096, 16384]); v_exp = din("v_exp", [16384, 4096])
    ident_in = din("ident", [128, 128]); anti_in = din("anti", [128, 128])
    cosk = din("cosk", [2048, 128]); sink = din("sink", [2048, 128]); kdec_tab = din("kdec_tab", [2048, 8])
    cosq = din("cosq", [1024, 128]); sinq = din("sinq", [1024, 128]); qdec_tab = din("qdec_tab", [1024, 8])
    mret_in = din("mret", [128, 2, 8, 128]); adm01_in = din("adm01", [128, 256]); negb_in = din("negb", [128, 256])
    outT = nc.dram_tensor("outT", [4096, 1024], F32, kind="ExternalOutput").ap()

    mod_d = dscr("mod_d", [1, 24576], F32)
    hk_d = dscr("hk_d", [128, 32, 2048], BF16); hq_d = dscr("hq_d", [128, 32, 1024], BF16)
    ckv_tm_d = dscr("ckv_tm_d", [2048, 512], BF16); ckvT_d = dscr("ckvT_d", [128, 4, 2048], BF16)
    kidxT_d = dscr("kidxT_d", [128, 2048], BF16)
    kT_d = dscr("kT_d", [128, 8, 2048], BF16); kdec_d = dscr("kdec_d", [2048, 1024], BF16); v_d = dscr("v_d", [2048, 2048], BF16)
    cqT_d = dscr("cqT_d", [128, 8, 1024], BF16); wi_d = dscr("wi_d", [1024, 64], F32)
    qT_d = dscr("qT_d", [128, 8, 1024], BF16); qdT_d = dscr("qdT_d", [128, 8, 1024], BF16); rg_d = dscr("rg_d", [1024, 2048], BF16)
    yaT_d = dscr("yaT_d", [128, 16, 1024], BF16); yrT_d = dscr("yrT_d", [128, 16, 1024], BF16)
    ef_d = dscr("ef_d", [16, 512], F32)
    gate_d = dscr("gate_d", [128, 64, 1024], BF16); mrg_d = dscr("mrg_d", [128, 32, 1024], BF16)
    x1T_d = dscr("x1T_d", [128, 32, 1024], F32); h2T_d = dscr("h2T_d", [128, 32, 1024], BF16)
    ge_d = dscr("ge_d", [1024, 16384], BF16); coefT_d = dscr("coefT_d", [16384, 1024], BF16)
    x2T_d = dscr("x2T_d", [128, 32, 1024], F32)

    def dma1(q, out, in_, reads, writes):
        kb.dma(q, lambda e: e.dma_start(out=out, in_=in_), reads, writes)

    def dma(q, out, in_, reads, writes):
        sh = out.shape
        if len(sh) == 3 and sh[0] * sh[1] > 1024 and len(in_.shape) == 3 and in_.shape[1] == sh[1]:
            step = max(1, 1024 // sh[0])
            for a in range(0, sh[1], step):
                dma1(q, out[:, a:a + step, :], in_[:, a:a + step, :], reads, writes)
        else:
            dma1(q, out, in_, reads, writes)

    def mm(out, lhsT, rhs, start, stop, reads, writes):
        kb.op("pe", lambda e: e.matmul(out, lhsT, rhs, start=start, stop=stop), reads, writes, pe_accum=not start)

    def tr(out, in_, reads, writes):
        kb.op("pe", lambda e: e.transpose(out, in_, ident[:]), list(reads) + ["ident"], writes)

    def act(out, in_, func, reads, writes, bias=None, scale=None):
        kw = {}
        if bias is not None:
            kw["bias"] = bias
        if scale is not None:
            kw["scale"] = scale
        kb.op("act", lambda e: e.activation(out=out, in_=in_, func=func, **kw), reads, writes)

    def tt(eng, out, in0, in1, op, reads, writes):
        kb.op(eng, lambda e: e.tensor_tensor(out=out, in0=in0, in1=in1, op=op), reads, writes)

    def ts(eng, out, in0, s1, op0, reads, writes, s2=None, op1=None):
        if op1 is None:
            kb.op(eng, lambda e: e.tensor_scalar(out=out, in0=in0, scalar1=s1, scalar2=None, op0=op0), reads, writes)
        else:
            kb.op(eng, lambda e: e.tensor_scalar(out=out, in0=in0, scalar1=s1, scalar2=s2, op0=op0, op1=op1), reads, writes)

    def stt(eng, out, in0, scalar, in1, op0, op1, reads, writes):
        kb.op(eng, lambda e: e.scalar_tensor_tensor(out=out, in0=in0, scalar=scalar, in1=in1, op0=op0, op1=op1), reads, writes)

    def cp(eng, out, in_, reads, writes):
        if eng == "act":
            kb.op("act", lambda e: e.copy(out, in_), reads, writes)
        else:
            kb.op(eng, lambda e: e.tensor_copy(out, in_), reads, writes)

    def recip(out, in_, reads, writes):
        kb.op("dve", lambda e: e.reciprocal(out=out, in_=in_), reads, writes)

    def rsum(out, in_, reads, writes):
        kb.op("dve", lambda e: e.reduce_sum(out=out, in_=in_, axis=AX.X), reads, writes)

    def vmax(out, in_, reads, writes):
        kb.op("dve", lambda e: e.max(out=out, in_=in_), reads, writes)

    def mrep(out, rep, vals, reads, writes):
        kb.op("dve", lambda e: e.match_replace(out=out, in_to_replace=rep, in_values=vals, imm_value=NEG), reads, writes)

    def amul(out, in_, c, reads, writes):
        kb.op("act", lambda e: e.mul(out, in_, c), reads, writes)

    def memset(eng, ap, val, writes):
        kb.op(eng, lambda e: e.memset(ap, val), (), writes)

    ident = kb.sbuf("ident", [128, 128], BF16)
    anti = kb.sbuf("anti", [128, 128], BF16)
    ones = kb.sbuf("ones", [128, 128], BF16)
    eps_t = kb.sbuf("eps_t", [128, 1], F32)
    modT = kb.sbuf("modT", [128, 192], F32)
    A1 = kb.sbuf("A1", [128, 32], F32); A2 = kb.sbuf("A2", [128, 32], F32)
    pbig = kb.psum("pbig", [128, 2048], F32)
    pb4 = kb.psum("pb4", [128, 512], F32); pb5 = kb.psum("pb5", [128, 512], F32); pb6 = kb.psum("pb6", [128, 512], F32)
    pb7 = kb.psum("pb7", [128, 512], F32)
    pst = pb7[:].bitcast(BF16)
    tslots = PRing([pst[:, 0:1024]], ["pb7"])
    big = [pbig[:, i * 512:(i + 1) * 512] for i in range(4)]
    bigk = [f"pbig{i}" for i in range(4)]

    dma("pool", ident[:], ident_in, [], ["ident"])
    dma("pool", anti[:], anti_in, [], ["anti"])
    memset("dve", ones[:], 1.0, ["ones"])
    memset("dve", eps_t[:], EPS, ["eps"])

    def transposes(dst, src, n, rk, wk, eng="act"):
        slot, sk = tslots.get()
        for i in range(n):
            tr(slot[:, i * 128:(i + 1) * 128], src[:, i * 128:(i + 1) * 128], [rk], [sk])
        cp(eng, dst, slot[:, 0:n * 128].rearrange("p (n c) -> p n c", n=n), [sk], [wk])

    m0 = kb.mark()
    c32 = kb.sbuf("c32", [128, 32], F32); cact = kb.sbuf("cact", [128, 32], BF16)
    dma("sp", c32[:], cT, [], ["c32"])
    act(cact[:], c32[:], AF.Silu, ["c32"], ["cact"])
    w_ada_v = w_ada.rearrange("(k p) e -> p k e", p=128)
    wring = Ring(kb, "wada", 2, [128, 32, 512], BF16)
    brow = Ring(kb, "brow", 2, [1, 512], F32); mrow = Ring(kb, "mrow", 2, [1, 512], F32)
    p0 = PRing([pb4[:], pb5[:]], ["pb4", "pb5"])
    p0b = PRing([pb6[:]], ["pb6"])
    one32 = kb.sbuf("one32", [1, 1], F32)
    memset("dve", one32[:], 1.0, ["one32"])
    wring32 = Ring(kb, "wada32", 2, [128, 32, 512], F32)
    cact32 = kb.sbuf("cact32", [128, 32], F32)
    act(cact32[:], c32[:], AF.Silu, ["c32"], ["cact32"])
    for g in range(48):
        ps, pk = p0.get()
        if g % 2 == 0:
            wt, wk = wring.get()
            dma("pool", wt[:], w_ada_v[:, :, g * 512:(g + 1) * 512], [], [wk])
            for k in range(32):
                mm(ps[0:1, :], cact[:, k:k + 1], wt[:, k, :], k == 0, k == 31, [wk, "cact"], [pk])
        else:
            wt, wk = wring32.get()
            dma("sp", wt[:], w_ada_v[:, :, g * 512:(g + 1) * 512], [], [wk])
            for k in range(32):
                mm(ps[0:1, :], cact32[:, k:k + 1], wt[:, k, :], k == 0, k == 31, [wk, "cact32"], [pk])
        bt, bk = brow.get(); mt, mk = mrow.get()
        dma("sp", bt[:], b_ada[0:1, g * 512:(g + 1) * 512], [], [bk])
        tt("dve", mt[:], ps[0:1, :], bt[:], ALU.add, [pk, bk], [mk])
        ps2, pk2 = p0b.get()
        for i in range(4):
            mm(ps2[:, i:i + 1], mt[0:1, i * 128:(i + 1) * 128], one32[0:1, 0:1], True, True, [mk, "one32"], [pk2])
        cp("act", modT[:, g * 4:(g + 1) * 4], ps2[:, 0:4], [pk2], ["modT"])
    gm = kb.sbuf("gm", [128, 32], F32); gf = kb.sbuf("gf", [128, 32], F32)
    dma("sp", gm[:], g_mix, [], ["gm"]); dma("sp", gf[:], g_ffn, [], ["gf"])
    stt("dve", A1[:], modT[:, 32:64], 1.0, gm[:], ALU.add, ALU.mult, ["modT", "gm"], ["A1"])
    stt("dve", A2[:], modT[:, 128:160], 1.0, gf[:], ALU.add, ALU.mult, ["modT", "gf"], ["A2"])
    kb.barrier(); kb.free_to(m0)
    if stages <= 0:
        return finish(nc, kb, outT)

    def phase_norm(xT_v, ntok, A, Bv, dst_v, dkey, gvec=None):
        m = kb.mark()
        xr = Ring(kb, "xr", 2, [128, 32, 256], F32); sqr = Ring(kb, "sqr", 2, [128, 32, 256], BF16)
        hr = Ring(kb, "hr", 2, [128, 32, 256], BF16 if dst_v.dtype == BF16 else F32)
        rr = Ring(kb, "rr", 2, [128, 256], F32)
        pr = PRing([pb4[:], pb5[:]], ["pb4", "pb5"])
        for ti in range(ntok // 256):
            xt, kx = xr.get(); sq, ksq = sqr.get(); ht, kh = hr.get(); rt, krt = rr.get(); ps, pk = pr.get()
            dma("sp", xt[:], xT_v[:, :, ti * 256:(ti + 1) * 256], [], [kx])
            act(sq[:], xt[:], AF.Square, [kx], [ksq])
            for k in range(32):
                mm(ps[:, 0:256], ones[:], sq[:, k, :], k == 0, k == 31, [ksq, "ones"], [pk])
            act(rt[:], ps[:, 0:256], AF.Sqrt, [pk, "eps"], [krt], bias=eps_t[:, 0:1], scale=1.0 / 4096)
            recip(rt[:], rt[:], [krt], [krt])
            tt("dve", xt[:], xt[:], rt[:].unsqueeze(1).to_broadcast([128, 32, 256]), ALU.mult, [kx, krt], [kx])
            if Bv is not None:
                tt("pool", xt[:], xt[:], A.unsqueeze(2).to_broadcast([128, 32, 256]), ALU.mult, [kx, "A1", "A2", "modT"], [kx])
                tt("dve", ht[:], xt[:], Bv.unsqueeze(2).to_broadcast([128, 32, 256]), ALU.add, [kx, "modT"], [kh])
            else:
                tt("pool", ht[:], xt[:], A.unsqueeze(2).to_broadcast([128, 32, 256]), ALU.mult, [kx, "gfin"], [kh])
            dma("sp", dst_v[:, :, ti * 256:(ti + 1) * 256], ht[:], [kh], [dkey])
        kb.barrier(); kb.free_to(m)

    xk_v = xkT.rearrange("(k p) t -> p k t", p=128)
    xq_v = xqT.rearrange("(k p) t -> p k t", p=128)
    phase_norm(xk_v, 2048, A1[:], modT[:, 0:32], hk_d, "hk_d")
    phase_norm(xq_v, 1024, A1[:], modT[:, 0:32], hq_d, "hq_d")
    if stages <= 1:
        return finish(nc, kb, outT)

    w_in_v = w_in.rearrange("(k p) e -> p k e", p=128)

    def bc_load(name, src, n):
        t = kb.sbuf(name, [128, n], F32)
        dma("sp", t[:], src.partition_broadcast(128), [], [name])
        return t

    m2 = kb.mark()
    gckv_t = bc_load("gckv_t", g_ckv, 512); gki_t = bc_load("gki_t", g_ki, 128); bki_t = bc_load("bki_t", b_ki, 128)
    hT = kb.sbuf("hT", [128, 32, 1024], BF16)
    wr = Ring(kb, "wr", 2, [128, 32, 512], BF16)
    cos_t = kb.sbuf("cos_t", [128, 8, 128], F32); sin_t = kb.sbuf("sin_t", [128, 8, 128], F32); dec_t = kb.sbuf("dec_t", [128, 8, 8], F32)
    t512 = Ring(kb, "t512", 2, [128, 512], F32); u512 = Ring(kb, "u512", 2, [128, 512], F32)
    b512 = Ring(kb, "b512", 3, [128, 512], BF16); b512b = Ring(kb, "b512b", 2, [128, 512], BF16)
    trb = Ring(kb, "trb", 3, [128, 4, 128], BF16)
    sm = Ring(kb, "sm", 4, [128, 2], F32)
    pr2 = PRing(big + [pb4[:], pb5[:], pb6[:]], bigk + ["pb4", "pb5", "pb6"])

    def gemm_tm(hTt, col0, ncols, consumer):
        wt, wk = wr.get()
        dma("pool", wt[:, :, 0:ncols], w_in_v[:, :, col0:col0 + ncols], [], [wk])
        pend = None
        for tti in range(8):
            ps, pk = pr2.get()
            for k in range(32):
                mm(ps[:, 0:ncols], hTt[:, k, tti * 128:(tti + 1) * 128], wt[:, k, 0:ncols], k == 0, k == 31, [wk, "hT"], [pk])
            if pend is not None:
                pend()
            pend = consumer(tti, ps, pk)
        if pend is not None:
            pend()

    def rms_rows(ps_ap, pk, n, gtab, gk, outbf, ok, extra_reads=()):
        t, tk = t512.get(); s, sk = sm.get()
        act(t[:, 0:n], ps_ap, AF.Square, [pk] + list(extra_reads), [tk])
        rsum(s[:, 0:1], t[:, 0:n], [tk], [sk])
        act(s[:, 0:1], s[:, 0:1], AF.Sqrt, [sk, "eps"], [sk], bias=eps_t[:, 0:1], scale=1.0 / n)
        recip(s[:, 0:1], s[:, 0:1], [sk], [sk])
        stt("dve", outbf, ps_ap, s[:, 0:1], gtab, ALU.mult, ALU.mult, [pk, sk, gk] + list(extra_reads), [ok])

    def rotary(psv_ap, pk, cosr, sinr, tabk, ra, rak, rb, rbk):
        tt("dve", ra, psv_ap, cosr.unsqueeze(1).to_broadcast([128, 4, 128]), ALU.mult, [pk, tabk], [rak])
        tt("dve", rb[:, :, 0:64], psv_ap[:, :, 64:128], sinr[:, 0:64].unsqueeze(1).to_broadcast([128, 4, 64]), ALU.mult, [pk, tabk], [rbk])
        tt("dve", rb[:, :, 64:128], psv_ap[:, :, 0:64], sinr[:, 64:128].unsqueeze(1).to_broadcast([128, 4, 64]), ALU.mult, [pk, tabk], [rbk])
        tt("pool", ra, ra, rb, ALU.add, [rak, rbk], [rak])

    for half in range(2):
        dma("sp", hT[:], hk_d[:, :, half * 1024:(half + 1) * 1024], ["hk_d"], ["hT"])
        dma("sp", cos_t[:], cosk[half * 1024:(half + 1) * 1024, :].rearrange("(t p) c -> p t c", p=128), [], ["ktab"])
        dma("sp", sin_t[:], sink[half * 1024:(half + 1) * 1024, :].rearrange("(t p) c -> p t c", p=128), [], ["ktab"])
        dma("sp", dec_t[:], kdec_tab[half * 1024:(half + 1) * 1024, :].rearrange("(t p) c -> p t c", p=128), [], ["ktab"])

        def c_ckv(tti, ps, pk):
            T = half * 8 + tti
            cn, ck = b512.get()
            rms_rows(ps[:, 0:512], pk, 512, gckv_t[:], "gckv_t", cn[:], ck)
            dma("sp", ckv_tm_d[T * 128:(T + 1) * 128, :], cn[:], [ck], ["ckv_tm_d"])

            def tail():
                ct, ctk = trb.get()
                transposes(ct[:], cn[:], 4, ck, ctk)
                dma("sp", ckvT_d[:, :, T * 128:(T + 1) * 128], ct[:], [ctk], ["ckvT_d"])
            return tail

        def c_ki(tti, ps, pk):
            T = half * 8 + tti
            s, sk = sm.get(); xc, xk_ = t512.get(); sq, sqk = u512.get(); kn, knk = b512.get()
            rsum(s[:, 0:1], ps[:, 0:128], [pk], [sk])
            amul(s[:, 0:1], s[:, 0:1], 1.0 / 128, [sk], [sk])
            ts("dve", xc[:, 0:128], ps[:, 0:128], s[:, 0:1], ALU.subtract, [pk, sk], [xk_])
            act(sq[:, 0:128], xc[:, 0:128], AF.Square, [xk_], [sqk])
            rsum(s[:, 1:2], sq[:, 0:128], [sqk], [sk])
            act(s[:, 1:2], s[:, 1:2], AF.Sqrt, [sk, "eps"], [sk], bias=eps_t[:, 0:1], scale=1.0 / 128)
            recip(s[:, 1:2], s[:, 1:2], [sk], [sk])
            stt("dve", xc[:, 0:128], xc[:, 0:128], s[:, 1:2], gki_t[:], ALU.mult, ALU.mult, [xk_, sk, "gki_t"], [xk_])
            tt("dve", kn[:, 0:128], xc[:, 0:128], bki_t[:], ALU.add, [xk_, "bki_t"], [knk])
            def tail():
                ct, ctk = trb.get()
                transposes(ct[:, 0:1, :], kn[:, 0:128], 1, knk, ctk)
                dma("sp", kidxT_d[:, T * 128:(T + 1) * 128], ct[:, 0, :], [ctk], ["kidxT_d"])
            return tail

        def mk_rk(hg):
            def c_rk(tti, ps, pk):
                T = half * 8 + tti
                ra, rak = t512.get(); rb, rbk = u512.get(); kr, krk = b512.get(); kd, kdk = b512b.get()
                rav = ra[:].rearrange("p (h d) -> p h d", h=4); rbv = rb[:].rearrange("p (h d) -> p h d", h=4)
                rotary(ps[:, 0:512].rearrange("p (h d) -> p h d", h=4), pk, cos_t[:, tti, :], sin_t[:, tti, :], "ktab", rav, rak, rbv, rbk)
                cp("act", kr[:], ra[:], [rak], [krk])
                tt("dve", kd[:].rearrange("p (h d) -> p h d", h=4), rav,
                   dec_t[:, tti, hg * 4:(hg + 1) * 4].unsqueeze(2).to_broadcast([128, 4, 128]), ALU.mult, [rak, "ktab"], [kdk])
                dma("sp", kdec_d[T * 128:(T + 1) * 128, hg * 512:(hg + 1) * 512], kd[:], [kdk], ["kdec_d"])

                def tail():
                    ct, ctk = trb.get()
                    transposes(ct[:], kr[:], 4, krk, ctk)
                    dma("sp", kT_d[:, hg * 4:(hg + 1) * 4, T * 128:(T + 1) * 128], ct[:], [ctk], ["kT_d"])
                return tail
            return c_rk

        def mk_rv(i):
            def c_rv(tti, ps, pk):
                T = half * 8 + tti
                vb, vk = b512.get()
                cp("act", vb[:], ps[:, 0:512], [pk], [vk])
                dma("sp", v_d[T * 128:(T + 1) * 128, i * 512:(i + 1) * 512], vb[:], [vk], ["v_d"])
            return c_rv

        gemm_tm(hT, O_CKV, 512, c_ckv)
        gemm_tm(hT, O_KI, 128, c_ki)
        for hg in range(2):
            gemm_tm(hT, O_RK + hg * 512, 512, mk_rk(hg))
        for i in range(4):
            gemm_tm(hT, O_RV + i * 512, 512, mk_rv(i))

    gcq_t = bc_load("gcq_t", g_cq, 1024)
    cq_raw = kb.sbuf("cq_raw", [128, 8, 1024], F32)
    t1024 = kb.sbuf("t1024", [128, 1024], F32); cqn = Ring(kb, "cqn", 2, [128, 1024], BF16)
    trb8 = Ring(kb, "trb8", 2, [128, 8, 128], BF16)
    w64 = Ring(kb, "w64", 2, [128, 64], F32)
    dma("sp", hT[:], hq_d, ["hq_d"], ["hT"])
    dma("sp", cos_t[:], cosq.rearrange("(t p) c -> p t c", p=128), [], ["ktab"])
    dma("sp", sin_t[:], sinq.rearrange("(t p) c -> p t c", p=128), [], ["ktab"])
    dma("sp", dec_t[:], qdec_tab.rearrange("(t p) c -> p t c", p=128), [], ["ktab"])

    def mk_cq(g):
        def c_cq(tti, ps, pk):
            cp("act", cq_raw[:, tti, g * 512:(g + 1) * 512], ps[:, 0:512], [pk], [f"cq_raw{tti}"])
        return c_cq

    def c_wi(tti, ps, pk):
        wt_, wk_ = w64.get()
        amul(wt_[:], ps[:, 0:64], (64 ** -0.5) * (128 ** -0.5), [pk], [wk_])
        dma("sp", wi_d[tti * 128:(tti + 1) * 128, :], wt_[:], [wk_], ["wi_d"])

    def mk_rq(hg):
        def c_rq(tti, ps, pk):
            ra, rak = t512.get(); rb, rbk = u512.get(); qr, qrk = b512.get(); qd, qdk = b512b.get()
            rav = ra[:].rearrange("p (h d) -> p h d", h=4); rbv = rb[:].rearrange("p (h d) -> p h d", h=4)
            rotary(ps[:, 0:512].rearrange("p (h d) -> p h d", h=4), pk, cos_t[:, tti, :], sin_t[:, tti, :], "ktab", rav, rak, rbv, rbk)
            cp("act", qr[:], ra[:], [rak], [qrk])
            tt("dve", qd[:].rearrange("p (h d) -> p h d", h=4), rav,
               dec_t[:, tti, hg * 4:(hg + 1) * 4].unsqueeze(2).to_broadcast([128, 4, 128]), ALU.mult, [rak, "ktab"], [qdk])
            def tail():
                ct, ctk = trb.get()
                transposes(ct[:], qr[:], 4, qrk, ctk)
                dma("sp", qT_d[:, hg * 4:(hg + 1) * 4, tti * 128:(tti + 1) * 128], ct[:], [ctk], ["qT_d"])
                ct2, ctk2 = trb.get()
                transposes(ct2[:], qd[:], 4, qdk, ctk2)
                dma("sp", qdT_d[:, hg * 4:(hg + 1) * 4, tti * 128:(tti + 1) * 128], ct2[:], [ctk2], ["qdT_d"])
            return tail
        return c_rq

    def mk_rg(i):
        def c_rg(tti, ps, pk):
            gb, gk_ = b512.get()
            act(gb[:], ps[:, 0:512], AF.Silu, [pk], [gk_])
            dma("sp", rg_d[tti * 128:(tti + 1) * 128, i * 512:(i + 1) * 512], gb[:], [gk_], ["rg_d"])
        return c_rg

    for g in range(2):
        gemm_tm(hT, O_CQ + g * 512, 512, mk_cq(g))
    for tti in range(8):
        s, sk = sm.get(); cn, cnk = cqn.get()
        act(t1024[:], cq_raw[:, tti, :], AF.Square, [f"cq_raw{tti}"], ["t1024"])
        rsum(s[:, 0:1], t1024[:], ["t1024"], [sk])
        act(s[:, 0:1], s[:, 0:1], AF.Sqrt, [sk, "eps"], [sk], bias=eps_t[:, 0:1], scale=1.0 / 1024)
        recip(s[:, 0:1], s[:, 0:1], [sk], [sk])
        stt("dve", cn[:], cq_raw[:, tti, :], s[:, 0:1], gcq_t[:], ALU.mult, ALU.mult, [f"cq_raw{tti}", sk, "gcq_t"], [cnk])
        ct, ctk = trb8.get()
        transposes(ct[:, 0:4, :], cn[:, 0:512], 4, cnk, ctk)
        transposes(ct[:, 4:8, :], cn[:, 512:1024], 4, cnk, ctk)
        dma("sp", cqT_d[:, :, tti * 128:(tti + 1) * 128], ct[:], [ctk], ["cqT_d"])
    gemm_tm(hT, O_WI, 64, c_wi)
    for hg in range(2):
        gemm_tm(hT, O_RQ + hg * 512, 512, mk_rq(hg))
    for i in range(4):
        gemm_tm(hT, O_RG + i * 512, 512, mk_rg(i))
    kb.barrier(); kb.free_to(m2)
    if stages <= 2:
        return finish(nc, kb, outT)

    m3 = kb.mark()
    mret = kb.sbuf("mret", [128, 2, 8, 128], F32)
    dma("sp", mret[:], mret_in, [], ["mret"])
    gret_t = bc_load("gret_t", g_ret, 2048)
    S = kb.sbuf("S", [128, 8, 256], F32); Sbf = kb.sbuf("Sbf", [128, 8, 256], BF16)
    memset("dve", S[:], 0.0, ["S"]); memset("pool", Sbf[:], 0.0, ["Sbf"])
    qTr = Ring(kb, "qTr", 2, [128, 8, 128], BF16); qdr = Ring(kb, "qdr", 2, [128, 8, 128], BF16)
    kTr = Ring(kb, "kTr", 2, [128, 8, 256], BF16); kdr = Ring(kb, "kdr", 2, [128, 2, 1024], BF16)
    vr = Ring(kb, "vr", 2, [128, 2, 2048], BF16); rgr = Ring(kb, "rgr", 2, [128, 2048], BF16)
    ATr = Ring(kb, "ATr", 3, [128, 2, 128], BF16)
    ysq = kb.sbuf("ysq", [128, 2048], F32); yn = kb.sbuf("yn", [128, 2048], F32); yrb = Ring(kb, "yrb", 2, [128, 2048], BF16)
    ss8 = Ring(kb, "ss8", 2, [128, 8], F32)
    yrT = Ring(kb, "yrT", 2, [128, 16, 128], BF16)
    psS = PRing([pb4[:], pb5[:]], ["pb4", "pb5"])
    psK = PRing([pb6[:]], ["pb6"])
    g256 = [math.exp(256.0 * LOGG[h]) for h in range(8)]
    for j in range(8):
        qt, qk = qTr.get(); qd, qdk = qdr.get(); kt_, kk = kTr.get(); kd, kdk = kdr.get(); vt, vk = vr.get(); rg, rgk = rgr.get()
        dma("sp", qt[:], qT_d[:, :, j * 128:(j + 1) * 128], ["qT_d"], [qk])
        dma("sp", qd[:], qdT_d[:, :, j * 128:(j + 1) * 128], ["qdT_d"], [qdk])
        dma("sp", kt_[:], kT_d[:, :, j * 256:(j + 1) * 256], ["kT_d"], [kk])
        dma("sp", kd[:], kdec_d[j * 256:(j + 1) * 256, :].rearrange("(s p) c -> p s c", p=128), ["kdec_d"], [kdk])
        dma("sp", vt[:], v_d[j * 256:(j + 1) * 256, :].rearrange("(s p) c -> p s c", p=128), ["v_d"], [vk])
        dma("sp", rg[:], rg_d[j * 128:(j + 1) * 128, :], ["rg_d"], [rgk])
        for h in range(8):
            ps, pk = psS.get(); at, atk = ATr.get()
            mm(ps[:, 0:128], kt_[:, h, 0:128], qt[:, h, :], True, True, [kk, qk], [pk])
            mm(ps[:, 128:256], kt_[:, h, 128:256], qt[:, h, :], True, True, [kk, qk], [pk])
            tt("dve", at[:], ps[:, 0:256].rearrange("p (s t) -> p s t", s=2), mret[:, :, h, :], ALU.mult, [pk, "mret"], [atk])
            yk = bigk[h // 2]
            yo = pbig[:, h * 256:(h + 1) * 256]
            mm(yo, at[:, 0, :], vt[:, 0, h * 256:(h + 1) * 256], True, False, [atk, vk], [yk])
            mm(yo, at[:, 1, :], vt[:, 1, h * 256:(h + 1) * 256], False, False, [atk, vk], [yk])
            mm(yo, qd[:, h, :], Sbf[:, h, :], False, True, [qdk, "Sbf"], [yk])
        s8, s8k = ss8.get(); yb, ybk = yrb.get()
        act(ysq[:], pbig[:, :], AF.Square, bigk, ["ysq"])
        rsum(s8[:], ysq[:].rearrange("p (h v) -> p h v", h=8), ["ysq"], [s8k])
        act(s8[:], s8[:], AF.Sqrt, [s8k, "eps"], [s8k], bias=eps_t[:, 0:1], scale=1.0 / 256)
        recip(s8[:], s8[:], [s8k], [s8k])
        tt("dve", yn[:].rearrange("p (h v) -> p h v", h=8), pbig[:, :].rearrange("p (h v) -> p h v", h=8),
           s8[:].unsqueeze(2).to_broadcast([128, 8, 256]), ALU.mult, bigk + [s8k], ["yn"])
        tt("pool", yn[:], yn[:], gret_t[:], ALU.mult, ["yn", "gret_t"], ["yn"])
        tt("dve", yb[:], yn[:], rg[:], ALU.mult, ["yn", rgk], [ybk])
        yT, yTk = yrT.get()
        for q4 in range(4):
            transposes(yT[:, q4 * 4:(q4 + 1) * 4, :], yb[:, q4 * 512:(q4 + 1) * 512], 4, ybk, yTk)
        dma("sp", yrT_d[:, :, j * 128:(j + 1) * 128], yT[:], [yTk], ["yrT_d"])
        if j < 7:
            for h in range(8):
                ps, pk = psK.get()
                mm(ps[:, 0:256], kd[:, 0, h * 128:(h + 1) * 128], vt[:, 0, h * 256:(h + 1) * 256], True, False, [kdk, vk], [pk])
                mm(ps[:, 0:256], kd[:, 1, h * 128:(h + 1) * 128], vt[:, 1, h * 256:(h + 1) * 256], False, True, [kdk, vk], [pk])
                stt("dve", S[:, h, :], S[:, h, :], g256[h], ps[:, 0:256], ALU.mult, ALU.add, [pk, "S"], ["S"])
            cp("act", Sbf[:], S[:], ["S"], ["Sbf"])
    kb.barrier(); kb.free_to(m3)
    if stages <= 3:
        return finish(nc, kb, outT)

    m4 = kb.mark()
    maskT = kb.sbuf("maskT", [128, 72, 128], BF16)
    qaT = kb.sbuf("qaT", [128, 16, 1024], BF16)
    mA = kb.mark()
    cqT = kb.sbuf("cqT", [128, 8, 1024], BF16)
    dma("sp", cqT[:], cqT_d, ["cqT_d"], ["cqT"])
    kidxT = kb.sbuf("kidxT", [128, 2048], BF16)
    dma("sp", kidxT[:], kidxT_d, ["kidxT_d"], ["kidxT"])
    wi_t = kb.sbuf("wi_t", [128, 8, 64], F32)
    dma("sp", wi_t[:], wi_d.rearrange("(t p) c -> p t c", p=128), ["wi_d"], ["wi_t"])
    moff = [sum(2 * jj + 2 for jj in range(j)) for j in range(8)]
    m4b = kb.mark()
    acc = [kb.sbuf(f"acc{j}", [128, (2 * j + 2) * 128], F32) for j in range(8)]
    wqr = Ring(kb, "wqr", 1, [128, 8, 1024], BF16)
    qiT = Ring(kb, "qiT", 2, [128, 8, 1024], BF16)
    rl = Ring(kb, "rl", 3, [128, 512], F32)
    w_qi_v = w_qi.rearrange("(k p) e -> p k e", p=128)
    pr4 = PRing(big + [pb4[:], pb5[:], pb6[:]], bigk + ["pb4", "pb5", "pb6"])
    for hgp in range(8):
        wq, wqk = wqr.get(); qi, qik = qiT.get()
        dma("pool", wq[:], w_qi_v[:, :, hgp * 1024:(hgp + 1) * 1024], [], [wqk])
        for hh in range(8):
            for half in range(2):
                ps, pk = pr4.get()
                for k in range(8):
                    mm(ps[:, :], wq[:, k, hh * 128:(hh + 1) * 128], cqT[:, k, half * 512:(half + 1) * 512], k == 0, k == 7, [wqk, "cqT"], [pk])
                cp("act" if half == 0 else "dve", qi[:, hh, half * 512:(half + 1) * 512], ps[:, :], [pk], [qik])
        for j in range(8):
            W = (2 * j + 2) * 128
            for hh in range(8):
                hd = hgp * 8 + hh
                for s0 in range(0, W, 512):
                    n = min(512, W - s0)
                    ps, pk = pr4.get(); r_, rk_ = rl.get()
                    mm(ps[:, 0:n], qi[:, hh, j * 128:(j + 1) * 128], kidxT[:, s0:s0 + n], True, True, [qik, "kidxT"], [pk])
                    act(r_[:, 0:n], ps[:, 0:n], AF.Relu, [pk], [rk_])
                    if hd == 0:
                        ts("dve", acc[j][:, s0:s0 + n], r_[:, 0:n], wi_t[:, j, hd:hd + 1], ALU.mult, [rk_, "wi_t"], [f"acc{j}"])
                    else:
                        stt("dve", acc[j][:, s0:s0 + n], r_[:, 0:n], wi_t[:, j, hd:hd + 1], acc[j][:, s0:s0 + n], ALU.mult, ALU.add,
                            [rk_, "wi_t", f"acc{j}"], [f"acc{j}"])
    negb = kb.sbuf("negb", [128, 256], F32); adm01 = kb.sbuf("adm01", [128, 256], F32)
    dma("sp", negb[:], negb_in, [], ["negb"]); dma("sp", adm01[:], adm01_in, [], ["adm01"])
    wk0 = kb.sbuf("wk0", [128, 2048], F32); wk1 = kb.sbuf("wk1", [128, 2048], F32)
    mx8 = Ring(kb, "mx8", 2, [128, 8], F32)
    mrow_ = kb.sbuf("mrow_", [128, 2048], BF16)
    for j in range(8):
        W = (2 * j + 2) * 128
        a = acc[j]; ak = f"acc{j}"
        tt("dve", a[:, W - 256:W], a[:, W - 256:W], negb[:], ALU.add, [ak, "negb"], [ak])
        src, srck = a, ak
        bufs = [(wk0, "wk0"), (wk1, "wk1")]
        for it in range(32):
            mx, mxk = mx8.get()
            vmax(mx[:], src[:, 0:W], [srck], [mxk])
            if it < 31:
                dst, dstk = bufs[it % 2]
                mrep(dst[:, 0:W], mx[:], src[:, 0:W], [srck, mxk], [dstk])
                src, srck = dst, dstk
        ts("dve", mrow_[:, 0:W], a[:, 0:W], mx[:, 7:8], ALU.is_ge, [ak, mxk], ["mrow_"])
        tt("dve", mrow_[:, W - 256:W], mrow_[:, W - 256:W], adm01[:], ALU.mult, ["mrow_", "adm01"], ["mrow_"])
        for kt0 in range(0, 2 * j + 2, 4):
            n = min(4, 2 * j + 2 - kt0)
            transposes(maskT[:, moff[j] + kt0:moff[j] + kt0 + n, :], mrow_[:, kt0 * 128:(kt0 + n) * 128], n, "mrow_", "maskT")
    kb.barrier(); kb.free_to(m4b)
    if stages <= 4:
        return finish(nc, kb, outT)

    wuq = kb.sbuf("wuq", [128, 8, 2048], BF16)
    dma("pool", wuq[:], w_uq.rearrange("(k p) e -> p k e", p=128), [], ["wuq"])
    dma("sp", cqT[:], cqT_d, ["cqT_d"], ["cqT"])
    pr5 = PRing([pb4[:], pb5[:], pb6[:]], ["pb4", "pb5", "pb6"])
    for h in range(16):
        for half in range(2):
            ps, pk = pr5.get()
            for k in range(8):
                mm(ps[:, :], wuq[:, k, h * 128:(h + 1) * 128], cqT[:, k, half * 512:(half + 1) * 512], k == 0, k == 7, ["wuq", "cqT"], [pk])
            cp("act" if half == 0 else "dve", qaT[:, h, half * 512:(half + 1) * 512], ps[:, :], [pk], ["qaT"])
    kb.barrier(); kb.free_to(mA)
    ckvT = kb.sbuf("ckvT", [128, 4, 2048], BF16); ckv_tm = kb.sbuf("ckv_tm", [128, 16, 512], BF16)
    dma("sp", ckvT[:], ckvT_d, ["ckvT_d"], ["ckvT"])
    dma("sp", ckv_tm[:], ckv_tm_d.rearrange("(t p) c -> p t c", p=128), ["ckv_tm_d"], ["ckv_tm"])
    wukT = kb.sbuf("wukT", [128, 16, 512], BF16); wuv = kb.sbuf("wuv", [128, 4, 2048], BF16)
    dma("pool", wukT[:], w_ukT, [], ["wukT"])
    dma("pool", wuv[:], w_uv.rearrange("(k p) e -> p k e", p=128), [], ["wuv"])
    tb_sb = kb.sbuf("tb_sb", [32, 16], F32); oh_sb = kb.sbuf("oh_sb", [32, 512], F32); t15_sb = kb.sbuf("t15_sb", [16, 1], F32)
    dma("sp", tb_sb[:], t5b, [], ["tb_sb"]); dma("sp", oh_sb[:], oh_rel, [], ["oh_sb"]); dma("sp", t15_sb[:], t15, [], ["t15_sb"])
    amul(t15_sb[:], t15_sb[:], -1.0, ["t15_sb"], ["t15_sb"])
    mm(pb4[0:16, :], tb_sb[:], oh_sb[:], True, True, ["tb_sb", "oh_sb"], ["pb4"])
    ef_sb = kb.sbuf("ef_sb", [16, 512], F32)
    act(ef_sb[:], pb4[0:16, :], AF.Exp, ["pb4", "t15_sb"], ["ef_sb"], bias=t15_sb[:, 0:1], scale=1.0)
    dma("sp", ef_d, ef_sb[:], ["ef_sb"], ["ef_d"])
    EBp = kb.sbuf("EBp", [128, 3, 16, 128], BF16)
    mE = kb.mark()
    hk32 = kb.sbuf("hk32", [128, 16, 128], F32); hkb = kb.sbuf("hkb", [128, 16, 128], BF16)
    for n in range(3):
        src_ap = bass.AP(ef_d.tensor, n * 128, [[1, 128], [512, 16], [1, 128]])
        dma("sp", hk32[:], src_ap, ["ef_d"], ["hk32"])
        cp("dve", hkb[:], hk32[:], ["hk32"], ["hkb"])
        for h4 in range(4):
            ps, pk = pr5.get()
            for i in range(4):
                mm(ps[:, i * 128:(i + 1) * 128], hkb[:, h4 * 4 + i, :], anti[:], True, True, ["hkb", "anti"], [pk])
            cp("act", EBp[:, n, h4 * 4:(h4 + 1) * 4, :], ps[:, :].rearrange("p (h t) -> p h t", h=4), [pk], ["EBp"])
    kb.barrier(); kb.free_to(mE)
    qlat = Ring(kb, "qlat", 1, [128, 4, 16, 128], BF16)
    MBn = Ring(kb, "MBn", 1, [128, 3, 16, 128], BF16)
    Er = Ring(kb, "Er", 3, [128, 512], F32)
    PTr = Ring(kb, "PTr", 3, [128, 512], BF16)
    rz = Ring(kb, "rz", 2, [128, 512], F32)
    olat = Ring(kb, "olat", 1, [128, 4, 16, 128], BF16)
    yaT = Ring(kb, "yaT", 2, [128, 16, 128], BF16)
    prL = PRing([pb5[:], pb6[:]], ["pb5", "pb6"])
    for j in range(8):
        ql, qlk = qlat.get()
        for cc in range(4):
            for h4 in range(4):
                ps, pk = prL.get()
                for i in range(4):
                    h = h4 * 4 + i
                    mm(ps[:, i * 128:(i + 1) * 128], wukT[:, h, cc * 128:(cc + 1) * 128], qaT[:, h, j * 128:(j + 1) * 128], True, True, ["wukT", "qaT"], [pk])
                amul(ql[:, cc, h4 * 4:(h4 + 1) * 4, :], ps[:, :].rearrange("p (h t) -> p h t", h=4), 128 ** -0.5, [pk], [qlk])
        nk = 2 * j + 2
        near = [kt for kt in (2 * j - 1, 2 * j, 2 * j + 1) if kt >= 0]
        mb, mbk = MBn.get()
        for kt in near:
            n = kt - (2 * j - 1)
            tt("pool", mb[:, n, :, :], EBp[:, n, :, :], maskT[:, moff[j] + kt, :].unsqueeze(1).to_broadcast([128, 16, 128]), ALU.mult, ["EBp", "maskT"], [mbk])
        ol, olk = olat.get()
        pend5 = None
        for hg in range(4):
            for kt in range(nk):
                ps, pk = prL.get(); E, Ek = Er.get(); PT, PTk = PTr.get()
                for cc in range(4):
                    mm(ps[:, :], ckvT[:, cc, kt * 128:(kt + 1) * 128], ql[:, cc, hg * 4:(hg + 1) * 4, :], cc == 0, cc == 3, ["ckvT", qlk], [pk])
                act(E[:], ps[:, :], AF.Exp, [pk], [Ek])
                if kt in near:
                    n = kt - (2 * j - 1)
                    tt("dve", PT[:].rearrange("p (h t) -> p h t", h=4), E[:].rearrange("p (h t) -> p h t", h=4), mb[:, n, hg * 4:(hg + 1) * 4, :], ALU.mult, [Ek, mbk], [PTk])
                else:
                    tt("dve", PT[:].rearrange("p (h t) -> p h t", h=4), E[:].rearrange("p (h t) -> p h t", h=4),
                       maskT[:, moff[j] + kt, :].unsqueeze(1).to_broadcast([128, 4, 128]), ALU.mult, [Ek, "maskT"], [PTk])
                if pend5 is not None:
                    pend5()

                def tail5(kt=kt, PT=PT, PTk=PTk, nk=nk):
                    for cc in range(4):
                        mm(big[cc], ckv_tm[:, kt, cc * 128:(cc + 1) * 128], PT[:], kt == 0, kt == nk - 1, ["ckv_tm", PTk], [bigk[cc]])
                    mm(pb4[:, :], ones[:], PT[:], kt == 0, kt == nk - 1, ["ones", PTk], ["pb4"])
                pend5 = tail5
            pend5(); pend5 = None
            r_, rk_ = rz.get()
            recip(r_[:], pb4[:, :], ["pb4"], [rk_])
            for cc in range(4):
                tt("dve", ol[:, cc, hg * 4:(hg + 1) * 4, :], big[cc].rearrange("p (h t) -> p h t", h=4), r_[:].rearrange("p (h t) -> p h t", h=4), ALU.mult, [bigk[cc], rk_], [olk])
        ya, yak = yaT.get()
        for h4 in range(4):
            ps, pk = prL.get()
            for i in range(4):
                h = h4 * 4 + i
                for cc in range(4):
                    mm(ps[:, i * 128:(i + 1) * 128], wuv[:, cc, h * 128:(h + 1) * 128], ol[:, cc, h, :], cc == 0, cc == 3, ["wuv", olk], [pk])
            cp("act", ya[:, h4 * 4:(h4 + 1) * 4, :], ps[:, :].rearrange("p (h t) -> p h t", h=4), [pk], [yak])
        dma("sp", yaT_d[:, :, j * 128:(j + 1) * 128], ya[:], [yak], ["yaT_d"])
    kb.barrier(); kb.free_to(m4)
    if stages <= 5:
        return finish(nc, kb, outT)

    def gemm_fm(actT, akey, nk, w_view, col0, wring_, consumer):
        wt, wk = wring_.get()
        dma("pool", wt[:, 0:nk, :], w_view[:, :, col0:col0 + 512], [], [wk])
        for cc in range(4):
            for half in range(2):
                ps, pk = prF.get()
                for k in range(nk):
                    mm(ps[:, :], wt[:, k, cc * 128:(cc + 1) * 128], actT[:, k, half * 512:(half + 1) * 512], k == 0, k == nk - 1, [wk, akey], [pk])
                consumer(col0 // 128 + cc, half, ps, pk)

    prF = PRing(big + [pb4[:], pb5[:], pb6[:]], bigk + ["pb4", "pb5", "pb6"])
    m6 = kb.mark()
    actA = kb.sbuf("actA", [128, 32, 1024], BF16)
    wrF = Ring(kb, "wrF", 2, [128, 32, 512], BF16)
    bg = kb.sbuf("bg", [128, 64], F32)
    dma("sp", bg[:], b_gate, [], ["bg"])
    dma("sp", actA[:], hq_d, ["hq_d"], ["actA"])
    o512 = Ring(kb, "o512", 3, [128, 512], BF16)
    w_gate_v = w_gate.rearrange("(k p) e -> p k e", p=128)

    def c_gate(gc, half, ps, pk):
        o, ok = o512.get()
        act(o[:], ps[:, :], AF.Sigmoid, [pk, "bg"], [ok], bias=bg[:, gc:gc + 1], scale=1.0)
        dma("sp", gate_d[:, gc, half * 512:(half + 1) * 512], o[:], [ok], ["gate_d"])

    for g in range(16):
        gemm_fm(actA, "actA", 32, w_gate_v, g * 512, wrF, c_gate)
    kb.barrier(); kb.free_to(m6)

    m6 = kb.mark()
    yaS = kb.sbuf("yaS", [128, 16, 1024], BF16); yrS = kb.sbuf("yrS", [128, 16, 1024], BF16)
    dma("sp", yaS[:], yaT_d, ["yaT_d"], ["yaS"]); dma("sp", yrS[:], yrT_d, ["yrT_d"], ["yrS"])
    wa = Ring(kb, "wa", 2, [128, 16, 512], BF16); wrr = Ring(kb, "wrr", 2, [128, 16, 512], BF16)
    gar = Ring(kb, "gar", 2, [128, 512], BF16); grr = Ring(kb, "grr", 2, [128, 512], BF16)
    f512 = Ring(kb, "f512", 2, [128, 512], F32); f512b = Ring(kb, "f512b", 2, [128, 512], F32)
    o512 = Ring(kb, "o512", 3, [128, 512], BF16)
    w_upa_v = w_up[0:2048, :].rearrange("(k p) e -> p k e", p=128)
    w_upr_v = w_up[2048:4096, :].rearrange("(k p) e -> p k e", p=128)
    for g in range(8):
        wat, wak = wa.get(); wrt, wrk = wrr.get()
        dma("pool", wat[:], w_upa_v[:, :, g * 512:(g + 1) * 512], [], [wak])
        dma("pool", wrt[:], w_upr_v[:, :, g * 512:(g + 1) * 512], [], [wrk])
        for cc in range(4):
            dc = g * 4 + cc
            for half in range(2):
                psa, pka = prF.get(); psr, pkr = prF.get()
                for k in range(16):
                    mm(psa[:, :], wat[:, k, cc * 128:(cc + 1) * 128], yaS[:, k, half * 512:(half + 1) * 512], k == 0, k == 15, [wak, "yaS"], [pka])
                for k in range(16):
                    mm(psr[:, :], wrt[:, k, cc * 128:(cc + 1) * 128], yrS[:, k, half * 512:(half + 1) * 512], k == 0, k == 15, [wrk, "yrS"], [pkr])
                ga, gak = gar.get(); gr, grk = grr.get(); t1, t1k = f512.get(); t2, t2k = f512b.get(); o, ok = o512.get()
                dma("sp", ga[:], gate_d[:, dc, half * 512:(half + 1) * 512], ["gate_d"], [gak])
                dma("sp", gr[:], gate_d[:, 32 + dc, half * 512:(half + 1) * 512], ["gate_d"], [grk])
                tt("dve", t1[:], psa[:, :], ga[:], ALU.mult, [pka, gak], [t1k])
                tt("dve", t2[:], psr[:, :], gr[:], ALU.mult, [pkr, grk], [t2k])
                tt("pool", o[:], t1[:], t2[:], ALU.add, [t1k, t2k], [ok])
                dma("sp", mrg_d[:, dc, half * 512:(half + 1) * 512], o[:], [ok], ["mrg_d"])
    kb.barrier(); kb.free_to(m6)

    m6 = kb.mark()
    actA = kb.sbuf("actA", [128, 32, 1024], BF16)
    wrF = Ring(kb, "wrF", 2, [128, 32, 512], BF16)
    dma("sp", actA[:], mrg_d, ["mrg_d"], ["actA"])
    xr5 = Ring(kb, "xr5", 3, [128, 512], F32); x1r = Ring(kb, "x1r", 3, [128, 512], F32)
    w_out_v = w_out.rearrange("(k p) e -> p k e", p=128)

    def c_out(dc, half, ps, pk):
        xt, xk_ = xr5.get(); x1, x1k = x1r.get()
        dma("sp", xt[:], xq_v[:, dc, half * 512:(half + 1) * 512], [], [xk_])
        stt("dve", x1[:], ps[:, :], modT[:, 64 + dc:65 + dc], xt[:], ALU.mult, ALU.add, [pk, xk_, "modT"], [x1k])
        dma("sp", x1T_d[:, dc, half * 512:(half + 1) * 512], x1[:], [x1k], ["x1T_d"])

    for g in range(8):
        gemm_fm(actA, "actA", 32, w_out_v, g * 512, wrF, c_out)
    kb.barrier(); kb.free_to(m6)
    phase_norm(x1T_d, 1024, A2[:], modT[:, 96:128], h2T_d, "h2T_d")
    if stages <= 6:
        return finish(nc, kb, outT)

    pst_d = dscr("pst_d", [8, 128, 2, 8, 128], F32); dl_d = dscr("dl_d", [8, 128, 8], F32)
    m7 = kb.mark()
    qTs = kb.sbuf("qTs", [128, 16, 1024], BF16)
    kT2 = kb.sbuf("kT2", [128, 2, 128], BF16)
    dma("pool", kT2[:], keysT.rearrange("s d k -> d s k"), [], ["kT2"])
    m7g = kb.mark()
    actA = kb.sbuf("actA", [128, 32, 1024], BF16)
    wrF = Ring(kb, "wrF", 2, [128, 32, 512], BF16)
    dma("sp", actA[:], h2T_d, ["h2T_d"], ["actA"])
    w_pq_v = w_pq.rearrange("(k p) e -> p k e", p=128)

    def c_pq(qc, half, ps, pk):
        cp("act" if half == 0 else "dve", qTs[:, qc, half * 512:(half + 1) * 512], ps[:, :], [pk], ["qTs"])

    for g in range(4):
        gemm_fm(actA, "actA", 32, w_pq_v, g * 512, wrF, c_pq)
    kb.barrier(); kb.free_to(m7g)
    s_sb = Ring(kb, "s_sb", 2, [128, 16, 128], F32)
    tmpk = Ring(kb, "tmpk", 2, [128, 128], F32)
    v16 = Ring(kb, "v16", 2, [128, 16, 16], F32)
    cand = Ring(kb, "cand", 2, [128, 8, 256], F32); cand2 = Ring(kb, "cand2", 2, [128, 256], F32)
    top16 = Ring(kb, "top16", 2, [128, 8, 16], F32)
    e16 = Ring(kb, "e16", 2, [128, 8, 16], F32)
    sm8 = Ring(kb, "sm8", 2, [128, 4, 8], F32)
    pstS = Ring(kb, "pstS", 2, [128, 2, 8, 128], F32)
    for tti in range(8):
        ss, ssk = s_sb.get()
        for qc in range(16):
            mm(pbig[:, qc * 128:(qc + 1) * 128], qTs[:, qc, tti * 128:(tti + 1) * 128], kT2[:, qc % 2, :], True, True, ["qTs", "kT2"], [bigk[qc // 4]])
        cp("act", ss[:], pbig[:, :].rearrange("p (q k) -> p q k", q=16), bigk, [ssk])
        v, vk_ = v16.get()
        for qc in range(16):
            tm, tmk = tmpk.get()
            vmax(v[:, qc, 0:8], ss[:, qc, :], [ssk], [vk_])
            mrep(tm[:], v[:, qc, 0:8], ss[:, qc, :], [ssk, vk_], [tmk])
            vmax(v[:, qc, 8:16], tm[:], [tmk], [vk_])
        cd, cdk = cand.get(); tp, tpk = top16.get()
        vv = v[:].rearrange("p (h s) a -> p h s a", s=2)
        tt("dve", cd[:].rearrange("p h (a b) -> p h a b", a=16), vv[:, :, 0, :].unsqueeze(3).to_broadcast([128, 8, 16, 16]),
           vv[:, :, 1, :].unsqueeze(2).to_broadcast([128, 8, 16, 16]), ALU.add, [vk_], [cdk])
        for h in range(8):
            c2, c2k = cand2.get()
            vmax(tp[:, h, 0:8], cd[:, h, :], [cdk], [tpk])
            mrep(c2[:], tp[:, h, 0:8], cd[:, h, :], [cdk, tpk], [c2k])
            vmax(tp[:, h, 8:16], c2[:], [c2k], [tpk])
        ee, eek = e16.get(); s8, s8k = sm8.get(); po, pok = pstS.get()
        tt("dve", ee[:], tp[:], tp[:, :, 0:1].to_broadcast([128, 8, 16]), ALU.subtract, [tpk], [eek])
        act(ee[:], ee[:], AF.Exp, [eek], [eek])
        rsum(s8[:, 0, :], ee[:], [eek], [s8k])
        act(s8[:, 1, :], s8[:, 0, :], AF.Ln, [s8k], [s8k])
        tt("dve", s8[:, 2, :], s8[:, 1, :], tp[:, :, 0], ALU.add, [s8k, tpk], [s8k])
        tt("dve", s8[:, 3, :], tp[:, :, 15], s8[:, 2, :], ALU.subtract, [s8k, tpk], [s8k])
        ssv = ss[:].rearrange("p (h s) k -> p h s k", s=2)
        tt("dve", po[:, 0, :, :], ssv[:, :, 0, :], s8[:, 2, :].unsqueeze(2).to_broadcast([128, 8, 128]), ALU.subtract, [ssk, s8k], [pok])
        ts("dve", s8[:, 3, :], s8[:, 3, :], -1.0e-5, ALU.add, [s8k], [s8k])
        cp("pool", po[:, 1, :, :], ssv[:, :, 1, :], [ssk], [pok])
        dma("sp", pst_d[tti], po[:], [pok], ["pst_d"])
        dma("sp", dl_d[tti], s8[:, 3, :], [s8k], ["dl_d"])
    kb.barrier(); kb.free_to(m7)

    m7 = kb.mark()
    actA = kb.sbuf("actA", [128, 32, 1024], BF16)
    dma("sp", actA[:], h2T_d, ["h2T_d"], ["actA"])
    pstA = kb.sbuf("pstA", [128, 8, 2, 8, 128], F32); dlA = kb.sbuf("dlA", [128, 8, 8], F32)
    for tti in range(8):
        dma("sp", pstA[:, tti], pst_d[tti], ["pst_d"], ["pstA"])
        dma("sp", dlA[:, tti, :], dl_d[tti], ["dl_d"], ["dlA"])
    uTr = Ring(kb, "uTr", 2, [128, 32, 256], BF16)
    Sr = Ring(kb, "Sr", 2, [128, 8, 2, 128], F32); Er2 = Ring(kb, "Er2", 2, [128, 8, 2, 128], BF16); G8r = Ring(kb, "G8r", 3, [128, 8, 2, 128], BF16)
    geTr = Ring(kb, "geTr", 2, [128, 2, 512], BF16); cTr2 = Ring(kb, "cTr2", 2, [128, 2, 512], BF16)
    uT_v = uT.rearrange("(k p) e -> p k e", p=128)
    prAct = PRing([(big[0], big[1]), (big[2], big[3])], [(bigk[0], bigk[1]), (bigk[2], bigk[3])])
    prG = PRing([(pb4[:], pb5[:]), (pb6[:], pb7[:])], [("pb4", "pb5"), ("pb6", "pb7")])
    pend7 = None
    for eg in range(64):
        ut, utk = uTr.get()
        dma("pool", ut[:], uT_v[:, :, eg * 256:(eg + 1) * 256], [], [utk])
        for tq in range(2):
            aps, aks = prAct.get(); gps, gks = prG.get()
            geT, geTk = geTr.get(); cT_, cTk = cTr2.get()
            for t4 in range(4):
                tti = tq * 4 + t4
                i_ = t4 // 2
                for k in range((t4 % 2) * 16, (t4 % 2) * 16 + 16):
                    mm(aps[i_], ut[:, k, i_ * 128:(i_ + 1) * 128], actA[:, k, tq * 512:(tq + 1) * 512], k == 0, k == 31, ["actA", utk], [aks[i_]])
                S_, Sk = Sr.get(); E_, Ek = Er2.get(); G8, G8k = G8r.get()
                tt("pool", S_[:], pstA[:, tti, 1, :, :].unsqueeze(2).to_broadcast([128, 8, 2, 128]),
                   pstA[:, tti, 0, :, eg * 2:(eg + 1) * 2].unsqueeze(3).to_broadcast([128, 8, 2, 128]), ALU.add, ["pstA"], [Sk])
                act(E_[:], S_[:], AF.Exp, [Sk], [Ek])
                for h in range(8):
                    stt("dve", G8[:, h, :, :], S_[:, h, :, :], dlA[:, tti, h:h + 1], E_[:, h, :, :], ALU.is_ge, ALU.mult, [Sk, Ek, "dlA"], [f"{G8k}h{h}"])
                for i in range(2):
                    for h in range(8):
                        mm(gps[i][:, t4 * 128:(t4 + 1) * 128], G8[:, h, i, :], ident[:], h == 0, h == 7, [f"{G8k}h{h}", "ident"], [gks[i]])
                if t4 == 1 and pend7 is not None:
                    pend7(); pend7 = None
            for i in range(2):
                act(geT[:, i, :], aps[i], AF.Gelu, [aks[i]], [geTk + str(i)])

            def tail7(cT_=cT_, cTk=cTk, gps=gps, gks=gks, geT=geT, geTk=geTk, eg=eg, tq=tq):
                for i in range(2):
                    tt("dve", cT_[:, i, :], gps[i], geT[:, i, :], ALU.mult, [gks[i], geTk + str(i)], [cTk])
                dma("sp", coefT_d[eg * 256:(eg + 1) * 256, tq * 512:(tq + 1) * 512].rearrange("(i e) t -> e i t", e=128), cT_[:], [cTk], ["coefT_d"])
            pend7 = tail7
    pend7()
    kb.barrier(); kb.free_to(m7)

    m7 = kb.mark()
    vtr = Ring(kb, "vtr", 10, [128, 512], BF16); cTr = Ring(kb, "cTr", 10, [128, 1024], BF16)
    xr5 = Ring(kb, "xr5", 3, [128, 512], F32); x1r = Ring(kb, "x1r", 3, [128, 512], F32)
    banks = big + [pb4[:], pb5[:], pb6[:], pb7[:]]
    bkeys = bigk + ["pb4", "pb5", "pb6", "pb7"]
    for ds in range(8):
        for et in range(128):
            vt, vk_ = vtr.get(); ct, ctk = cTr.get()
            dma("pool", vt[:], v_exp[et * 128:(et + 1) * 128, ds * 512:(ds + 1) * 512], [], [vk_])
            dma("sp", ct[:], coefT_d[et * 128:(et + 1) * 128, :], ["coefT_d"], [ctk])
            for cc in range(4):
                for half in range(2):
                    bi = cc * 2 + half
                    wkeys = [bkeys[bi]]
                    mm(banks[bi], vt[:, cc * 128:(cc + 1) * 128], ct[:, half * 512:(half + 1) * 512], et == 0, et == 127, [vk_, ctk], wkeys)
        for cc in range(4):
            dc = ds * 4 + cc
            for half in range(2):
                bi = cc * 2 + half
                xt, xk_ = xr5.get(); x2, x2k = x1r.get()
                dma("sp", xt[:], x1T_d[:, dc, half * 512:(half + 1) * 512], ["x1T_d"], [xk_])
                stt("dve", x2[:], banks[bi], modT[:, 160 + dc:161 + dc], xt[:], ALU.mult, ALU.add, [bkeys[bi], xk_, "modT"], [x2k])
                dma("sp", x2T_d[:, dc, half * 512:(half + 1) * 512], x2[:], [x2k], ["x2T_d"])
    kb.barrier(); kb.free_to(m7)

    gfin = kb.sbuf("gfin", [128, 32], F32)
    dma("sp", gfin[:], g_final, [], ["gfin"])
    phase_norm(x2T_d, 1024, gfin[:], None, outT.rearrange("(k p) t -> p k t", p=128), "outT")
    return finish(nc, kb, outT)


def finish(nc, kb, outT):
    kb.barrier()
    kb.emit()
    kb.close()
    return nc


def _t5_bucket_np(rel):
    n = np.abs(rel)
    lr = np.log(np.maximum(n, 1).astype(np.float32) / np.float32(8)) / np.float32(math.log(128 / 8))
    large = np.minimum(8 + (lr * np.float32(8)).astype(np.int32), 15)
    return np.where(rel > 0, 16, 0) + np.where(n < 8, n, large)


_CONST_CACHE = {}


def _consts(p):
    if p in _CONST_CACHE:
        return _CONST_CACHE[p]
    f32 = np.float32
    own = np.concatenate([np.arange((2 * j + p) * 128, (2 * j + p + 1) * 128) for j in range(8)])
    inv = (1.0 / (f32(10000.0) ** np.linspace(0.0, 1.0, 64, dtype=f32))).astype(f32)
    pos = np.arange(2048, dtype=f32)
    ang = pos[:, None] * inv[None, :]
    cos, sin = np.cos(ang).astype(f32), np.sin(ang).astype(f32)
    ks = f32(128 ** -0.5)
    cos2 = np.concatenate([cos, cos], 1); sin2 = np.concatenate([-sin, sin], 1)
    logg = np.array(LOGG, dtype=np.float64)
    s_in = np.arange(2048) % 256
    kdec = np.exp((255 - s_in)[:, None] * logg[None, :]).astype(f32)
    qdec = np.exp(((own % 256) + 1)[:, None] * logg[None, :]).astype(f32)
    sg = (np.arange(2)[:, None] * 128 + np.arange(128)[None, :])
    tg = 128 * p + np.arange(128)
    diff = tg[None, None, :] - sg[:, :, None]
    m = np.where(diff[:, :, None, :] >= 0, np.exp(np.maximum(diff, 0)[:, :, None, :] * logg[None, None, :, None]), 0.0)
    mret = np.ascontiguousarray(np.transpose(m, (1, 0, 2, 3))).astype(f32)
    tq = np.arange(128); s2 = np.arange(256)
    adm01 = ((s2[None, :] // 64) <= (2 * p + tq[:, None] // 64)).astype(f32)
    negb = ((adm01 - 1.0) * 1.0e30).astype(f32)
    mm_ = np.arange(512)
    rel = mm_ - 127 - (1 + p) * 128
    bk = _t5_bucket_np(rel)
    oh = (bk[None, :] == np.arange(32)[:, None]).astype(f32)
    oh[:, 511] = 0.0
    c = dict(own=own, cosk=(cos2 * ks).astype(f32), sink=(sin2 * ks).astype(f32), kdec_tab=kdec,
             cosq=np.ascontiguousarray(cos2[own]), sinq=np.ascontiguousarray(sin2[own]), qdec_tab=qdec,
             mret=mret, adm01=adm01, negb=negb, oh_rel=oh,
             ident=np.eye(128, dtype=f32), anti=np.ascontiguousarray(np.eye(128, dtype=f32)[::-1]))
    _CONST_CACHE[p] = c
    return c


def _shared(inp):
    f = lambda a: np.ascontiguousarray(a, dtype=np.float32)
    ch = lambda v: f(np.asarray(v).reshape(-1, 128).T)
    sh = dict(
        w_ada=f(inp["w_ada"][0]), b_ada=f(inp["b_ada"][0].reshape(1, -1)),
        g_mix=ch(inp["g_mix"][0]), g_ffn=ch(inp["g_ffn"][0]), g_final=ch(inp["g_final"]),
        w_in=f(inp["w_in"][0]), g_cq=f(inp["g_cq"][0]), g_ckv=f(inp["g_ckv"][0]), g_ki=f(inp["g_ki"][0]), b_ki=f(inp["b_ki"][0]),
        g_ret=f(inp["g_ret"][0].reshape(-1)),
        w_uq=f(inp["w_uq"][0].reshape(1024, 2048)), w_ukT=f(np.transpose(inp["w_uk"][0], (2, 1, 0))),
        w_uv=f(inp["w_uv"][0].reshape(512, 2048)), w_qi=f(inp["w_qi"][0].reshape(1024, 8192)),
        t5b=f(inp["t5_bias"]), t15=f(inp["t5_bias"][15].reshape(16, 1)),
        w_up=f(inp["w_up"][0]), w_gate=f(inp["w_gate"][0]), b_gate=ch(inp["b_gate"][0]), w_out=f(inp["w_out"][0]),
        w_pq=f(inp["w_pq"][0]), keysT=f(np.transpose(inp["sub_keys"][0], (0, 2, 1))),
        uT=f(inp["u_exp"][0].T), v_exp=f(inp["v_exp"][0]),
    )
    return sh


def make_in_maps(inp, cores=None, names=None):
    sh = _shared(inp)
    x = np.asarray(inp["x"], dtype=np.float32); c = np.asarray(inp["c"], dtype=np.float32)
    maps = []
    for core in (range(8) if cores is None else cores):
        b, p = core // 2, core % 2
        cs = _consts(p)
        m = dict(sh)
        m["_core"] = core
        m["xkT"] = np.ascontiguousarray(x[b].T)
        m["xqT"] = np.ascontiguousarray(x[b][cs["own"]].T)
        m["cT"] = np.ascontiguousarray(c[b].reshape(32, 128).T)
        for k in ("cosk", "sink", "kdec_tab", "cosq", "sinq", "qdec_tab", "mret", "adm01", "negb", "oh_rel", "ident", "anti"):
            m[k] = cs[k]
        m.pop("_core")
        if names is not None:
            m = {k: v for k, v in m.items() if k in names}
        maps.append(m)
    return maps


def kernel(**inputs):
    inp = {k: np.asarray(v) for k, v in inputs.items()}
    nc = build()
    maps = make_in_maps(inp, names=set(_USED["names"]))
    res = run_bass_kernel_spmd(nc, maps, core_ids=list(range(8)))
    out = np.zeros((4, 2048, 4096), dtype=np.float32)
    for core in range(8):
        b, p = core // 2, core % 2
        own = _consts(p)["own"]
        out[b, own, :] = np.asarray(res.results[core]["outT"]).T
    return out
```
